# Optimizing a Trainium2 kernel written in Bass

```python
import math
import jax, jax.numpy as jnp
from jax import lax
import numpy as np


D_MODEL = 1024
BATCH = 4
SEQ = 4096
DEPTH = 1

HEAD_DIM = 64
N_HEADS_DSA = 8
N_HEADS_MOBA = 8
N_IDX_HEADS = 4
IDX_DIM = 64
DSA_TOPK = 256
MOBA_BLOCK = 256
MOBA_TOPK = 3
N_BUCKETS = 32
MAX_DISTANCE = 128
D_FF = 2816
Q_BLOCK = 128
MOBA_Q_CHUNK = 32
EPS = 1e-6

W_DSA = N_HEADS_DSA * HEAD_DIM
W_MOBA = N_HEADS_MOBA * HEAD_DIM
W_IDX_Q = N_IDX_HEADS * IDX_DIM
SPLIT_SIZES = (W_DSA, W_DSA, W_DSA, W_MOBA, W_MOBA, W_MOBA, W_IDX_Q, IDX_DIM, N_IDX_HEADS, 2 * D_MODEL)
SPLIT_POINTS = tuple(int(v) for v in np.cumsum(SPLIT_SIZES)[:-1])
D_IN = int(sum(SPLIT_SIZES))

kernel_name = 'hybrid_dsa_moba_gated_macaron'


def rmsnorm(x, g):
    xf = x.astype(jnp.float32)
    y = xf * lax.rsqrt(jnp.mean(xf * xf, axis=-1, keepdims=True) + EPS)
    return (y * g.astype(jnp.float32)).astype(x.dtype)


def swiglu(x, w_gu, w_down):
    gate, up = jnp.split(x @ w_gu, 2, axis=-1)
    return (jax.nn.silu(gate) * up) @ w_down


def t5_bucket(dist):
    max_exact = N_BUCKETS // 2
    d = jnp.maximum(dist, 0)
    df = jnp.maximum(d, 1).astype(jnp.float32)
    large = max_exact + (jnp.log(df / max_exact) / math.log(MAX_DISTANCE / max_exact)
                         * (N_BUCKETS - max_exact)).astype(jnp.int32)
    large = jnp.minimum(large, N_BUCKETS - 1)
    return jnp.where(d < max_exact, d, large)


def dsa_attention(q, k, v, q_idx, k_idx, w_idx, bias_tab):
    B, S, H, Dh = q.shape
    topk = min(DSA_TOPK, S // 4)
    scale = Dh ** -0.5
    key_pos = jnp.arange(S)
    b_ix = jnp.arange(B)[:, None, None]
    k_idx_f = k_idx.astype(jnp.float32)

    def block(i):
        t0 = i * Q_BLOCK
        qb = lax.dynamic_slice_in_dim(q, t0, Q_BLOCK, axis=1)
        qib = lax.dynamic_slice_in_dim(q_idx, t0, Q_BLOCK, axis=1).astype(jnp.float32)
        wib = lax.dynamic_slice_in_dim(w_idx, t0, Q_BLOCK, axis=1).astype(jnp.float32)
        q_pos = t0 + jnp.arange(Q_BLOCK)
        dots = jnp.einsum('bqhd,bsd->bqhs', qib, k_idx_f) * (IDX_DIM ** -0.5)
        scores = jnp.einsum('bqh,bqhs->bqs', wib, jax.nn.relu(dots))
        causal = key_pos[None, :] <= q_pos[:, None]
        scores = jnp.where(causal[None], scores, -jnp.inf)
        _, idx = lax.top_k(scores, topk)
        k_sel = k[b_ix, idx]
        v_sel = v[b_ix, idx]
        logits = jnp.einsum('bqhd,bqkhd->bqhk', qb, k_sel).astype(jnp.float32) * scale
        dist = q_pos[None, :, None] - idx
        bias = bias_tab[t5_bucket(dist)].astype(jnp.float32)
        logits = logits + jnp.transpose(bias, (0, 1, 3, 2))
        logits = jnp.where((dist >= 0)[:, :, None, :], logits, -jnp.inf)
        p = jax.nn.softmax(logits, axis=-1)
        return jnp.einsum('bqhk,bqkhd->bqhd', p.astype(v.dtype), v_sel)

    outs = lax.map(block, jnp.arange(S // Q_BLOCK))
    return jnp.transpose(outs, (1, 0, 2, 3, 4)).reshape(B, S, H * Dh)


def moba_attention(q, k, v, bias_tab):
    B, S, H, Dh = q.shape
    scale = Dh ** -0.5
    nb = -(-S // MOBA_BLOCK)
    pad = nb * MOBA_BLOCK - S
    kp = jnp.pad(k, ((0, 0), (0, pad), (0, 0), (0, 0))).reshape(B, nb, MOBA_BLOCK, H, Dh)
    vp = jnp.pad(v, ((0, 0), (0, pad), (0, 0), (0, 0))).reshape(B, nb, MOBA_BLOCK, H, Dh)
    counts = jnp.minimum(S - jnp.arange(nb) * MOBA_BLOCK, MOBA_BLOCK).astype(jnp.float32)
    k_mean = kp.astype(jnp.float32).sum(axis=2) / counts[None, :, None, None]
    kbT = jnp.transpose(kp, (0, 3, 1, 2, 4))
    vbT = jnp.transpose(vp, (0, 3, 1, 2, 4))
    n_sel = min(MOBA_TOPK, nb - 1)
    C = MOBA_Q_CHUNK
    b_ix = jnp.arange(B)[:, None, None, None]
    h_ix = jnp.arange(H)[None, None, :, None]
    h_ix5 = jnp.arange(H)[None, None, :, None, None]
    blk_ids = jnp.arange(nb)
    offs = jnp.arange(MOBA_BLOCK)

    def chunk(i):
        t0 = i * C
        qc = lax.dynamic_slice_in_dim(q, t0, C, axis=1)
        q_pos = t0 + jnp.arange(C)
        own = q_pos // MOBA_BLOCK
        own_b = jnp.broadcast_to(own[None, :, None, None], (B, C, H, 1))
        if n_sel > 0:
            gate = jnp.einsum('bqhd,bnhd->bqhn', qc.astype(jnp.float32), k_mean)
            past = blk_ids[None, :] < own[:, None]
            gate = jnp.where(past[None, :, None, :], gate, -jnp.inf)
            _, sel = lax.top_k(gate, n_sel)
            blocks = jnp.concatenate([sel, own_b], axis=-1)
            blk_valid = jnp.concatenate([sel < own[None, :, None, None],
                                         jnp.ones((B, C, H, 1), bool)], axis=-1)
        else:
            blocks = own_b
            blk_valid = jnp.ones((B, C, H, 1), bool)
        k_g = kbT[b_ix, h_ix, blocks]
        v_g = vbT[b_ix, h_ix, blocks]
        logits = jnp.einsum('bqhd,bqhjld->bqhjl', qc, k_g).astype(jnp.float32) * scale
        key_pos = blocks[..., None] * MOBA_BLOCK + offs
        dist = q_pos[None, :, None, None, None] - key_pos
        logits = logits + bias_tab[t5_bucket(dist), h_ix5].astype(jnp.float32)
        valid = (dist >= 0) & blk_valid[..., None]
        logits = jnp.where(valid, logits, -jnp.inf)
        J = blocks.shape[-1]
        p = jax.nn.softmax(logits.reshape(B, C, H, J * MOBA_BLOCK), axis=-1)
        p = p.reshape(B, C, H, J, MOBA_BLOCK).astype(v.dtype)
        return jnp.einsum('bqhjl,bqhjld->bqhd', p, v_g)

    outs = lax.map(chunk, jnp.arange(S // C))
    return jnp.transpose(outs, (1, 0, 2, 3, 4)).reshape(B, S, H * Dh)


def setup_inputs(seed: int = 0) -> dict:
    key = jax.random.key(seed)
    ks = jax.random.split(key, 20)
    f32 = jnp.float32
    L = DEPTH

    def dense(k, shape):
        return jax.random.normal(k, shape, f32) * (shape[-2] ** -0.5)

    def gain(k, shape):
        return 1.0 + 0.02 * jax.random.normal(k, shape, f32)

    return {
        'x': jax.random.normal(ks[0], (BATCH, SEQ, D_MODEL), f32),
        'ffn1_norm': gain(ks[1], (L, D_MODEL)),
        'ffn1_w_gu': dense(ks[2], (L, D_MODEL, 2 * D_FF)),
        'ffn1_w_down': dense(ks[3], (L, D_FF, D_MODEL)),
        'mix_norm': gain(ks[4], (L, D_MODEL)),
        'w_in': dense(ks[5], (L, D_MODEL, D_IN)),
        'b_gate': 0.1 * jax.random.normal(ks[6], (L, 2 * D_MODEL), f32),
        'q_norm_dsa': gain(ks[7], (L, HEAD_DIM)),
        'k_norm_dsa': gain(ks[8], (L, HEAD_DIM)),
        'q_norm_moba': gain(ks[9], (L, HEAD_DIM)),
        'k_norm_moba': gain(ks[10], (L, HEAD_DIM)),
        'w_branch_dsa': dense(ks[11], (L, W_DSA, D_MODEL)),
        'w_branch_moba': dense(ks[12], (L, W_MOBA, D_MODEL)),
        'w_out': dense(ks[13], (L, D_MODEL, D_MODEL)),
        'ffn2_norm': gain(ks[14], (L, D_MODEL)),
        'ffn2_w_gu': dense(ks[15], (L, D_MODEL, 2 * D_FF)),
        'ffn2_w_down': dense(ks[16], (L, D_FF, D_MODEL)),
        'rel_bias': 0.1 * jax.random.normal(ks[17], (N_BUCKETS, N_HEADS_DSA + N_HEADS_MOBA), f32),
    }


def reference(x, ffn1_norm, ffn1_w_gu, ffn1_w_down, mix_norm, w_in, b_gate,
              q_norm_dsa, k_norm_dsa, q_norm_moba, k_norm_moba,
              w_branch_dsa, w_branch_moba, w_out,
              ffn2_norm, ffn2_w_gu, ffn2_w_down, rel_bias):
    B, S, _ = x.shape
    bias_dsa = rel_bias[:, :N_HEADS_DSA]
    bias_moba = rel_bias[:, N_HEADS_DSA:]
    h = x
    for l in range(DEPTH):
        h = h + 0.5 * swiglu(rmsnorm(h, ffn1_norm[l]), ffn1_w_gu[l], ffn1_w_down[l])
        u = rmsnorm(h, mix_norm[l])
        qa, ka, va, qb, kb, vb, qi, ki, wi, g = jnp.split(u @ w_in[l], SPLIT_POINTS, axis=-1)
        qa = rmsnorm(qa.reshape(B, S, N_HEADS_DSA, HEAD_DIM), q_norm_dsa[l])
        ka = rmsnorm(ka.reshape(B, S, N_HEADS_DSA, HEAD_DIM), k_norm_dsa[l])
        va = va.reshape(B, S, N_HEADS_DSA, HEAD_DIM)
        qb = rmsnorm(qb.reshape(B, S, N_HEADS_MOBA, HEAD_DIM), q_norm_moba[l])
        kb = rmsnorm(kb.reshape(B, S, N_HEADS_MOBA, HEAD_DIM), k_norm_moba[l])
        vb = vb.reshape(B, S, N_HEADS_MOBA, HEAD_DIM)
        qi = qi.reshape(B, S, N_IDX_HEADS, IDX_DIM)
        wi = wi * (N_IDX_HEADS ** -0.5)
        out_a = dsa_attention(qa, ka, va, qi, ki, wi, bias_dsa)
        out_b = moba_attention(qb, kb, vb, bias_moba)
        gates = jax.nn.sigmoid((g + b_gate[l]).astype(jnp.float32)).astype(h.dtype)
        g_a, g_b = jnp.split(gates, 2, axis=-1)
        mixed = g_a * (out_a @ w_branch_dsa[l]) + g_b * (out_b @ w_branch_moba[l])
        h = h + mixed @ w_out[l]
        h = h + 0.5 * swiglu(rmsnorm(h, ffn2_norm[l]), ffn2_w_gu[l], ffn2_w_down[l])
    return h
```

```python
import math
from contextlib import ExitStack

import numpy as np
import concourse.bass as bass
import concourse.mybir as mybir
from concourse.bass_utils import run_bass_kernel_spmd

F32 = mybir.dt.float32
BF16 = mybir.dt.bfloat16
AF = mybir.ActivationFunctionType
ALU = mybir.AluOpType
AX = mybir.AxisListType

S = 4096
D = 1024
DFF = 2816
NFC = 22
SL = 512
BIG = 30000.0
EPS = 1e-6
NSTEP = 14
NDS = 20
DEBUG = False
import os
KSTOP = float(os.environ.get('KSTOP', '1000'))


class StopBuild(Exception):
    pass


def chk(n):
    if KSTOP <= n:
        raise StopBuild()


class Buf:
    __slots__ = ("w", "rs")

    def __init__(self):
        self.w = None
        self.rs = {}


class Tr:
    def __init__(self, nc, st):
        self.nc = nc
        self.E = {}
        for name, h in [("pe", nc.tensor), ("act", nc.scalar), ("dve", nc.vector),
                        ("pool", nc.gpsimd), ("sp", nc.sync)]:
            sem = st.enter_context(nc.semaphore("s_" + name))
            self.E[name] = dict(h=h, sem=sem, cnt=0, waited={})
        self.ds = {}
        for q, nq in (("pool", 4), ("sp", NDS)):
            sems = [st.enter_context(nc.semaphore("d%s%d" % (q, i))) for i in range(nq)]
            self.ds[q] = dict(sems=sems, val=[0] * nq, nxt=0, n=nq)

    def wait(self, eng, ev):
        if ev is None:
            return
        sem, val, key = ev
        e = self.E[eng]
        if key == "pe" and eng == "pe":
            return
        if key in self.E:
            assert self.E[key]["cnt"] >= val, "wait on unmarked event %s" % key
        if e["waited"].get(key, 0) >= val:
            return
        e["h"].wait_ge(sem, val)
        e["waited"][key] = val

    def _deps(self, eng, reads, writes):
        for b in reads:
            self.wait(eng, b.w)
        for b in writes:
            self.wait(eng, b.w)
            for k, r in b.rs.items():
                if k == eng:
                    continue
                self.wait(eng, r)

    def _record(self, ev, reads, writes):
        for b in writes:
            b.w = ev
            b.rs = {}
        for b in reads:
            b.rs[ev[2]] = ev

    def op(self, eng, fn, reads=(), writes=(), mark=True):
        self._deps(eng, reads, writes)
        e = self.E[eng]
        inst = fn(e["h"])
        if mark:
            inst.then_inc(e["sem"], 1)
            e["cnt"] += 1
            ev = (e["sem"], e["cnt"], eng)
        else:
            ev = (e["sem"], e["cnt"] + 1, eng)
        self._record(ev, reads, writes)
        return ev

    def dma(self, q, out, in_, reads=(), writes=()):
        self._deps(q, reads, writes)
        d = self.ds[q]
        i = d["nxt"]
        d["nxt"] = (i + 1) % d["n"]
        key = ("d", q, i)
        if d["val"][i] > 0:
            self.wait(q, (d["sems"][i], d["val"][i], key))
        self.E[q]["h"].dma_start(out=out, in_=in_).then_inc(d["sems"][i], 16)
        d["val"][i] += 16
        ev = (d["sems"][i], d["val"][i], key)
        self._record(ev, reads, writes)
        return ev

    def barrier(self):
        evs = []
        for k, e in self.E.items():
            if e["cnt"] > 0:
                evs.append((e["sem"], e["cnt"], k))
        for q, d in self.ds.items():
            for i in range(d["n"]):
                if d["val"][i] > 0:
                    evs.append((d["sems"][i], d["val"][i], ("d", q, i)))
        for k in self.E:
            for ev in evs:
                if ev[2] == k:
                    continue
                self.wait(k, ev)


def build_program(debug=False):
    nc = bass.Bass("TRN2", target_bir_lowering=False)

    def din(name, shape, dt=F32):
        return nc.dram_tensor(name, shape, dt, kind="ExternalInput").ap()

    x_d = din("x", [S, D])
    w1gu_d = din("w1gu", [D, 2 * DFF])
    w1d_d = din("w1d", [DFF, D])
    win_d = din("win", [D, 5444])
    wba_d = din("wba", [512, D])
    wbb_d = din("wbb", [512, D])
    wo_d = din("wo", [D, D])
    w2gu_d = din("w2gu", [D, 2 * DFF])
    w2d_d = din("w2d", [DFF, D])
    gains_d = din("gains", [128, 24])
    bg_d = din("bg", [128, 16])
    gqk_d = din("gqk", [128, 4])
    dtab_d = din("dtab", [128, 16, 384])
    cb_d = din("cb", [128, 16])
    cmask_d = din("cmask", [128, 384])
    mtab_d = din("mtab", [128, 896])
    pbv_d = din("pbv", [128, 1])
    idab_d = din("idab", [128, 3, 128])
    esel_d = din("esel", [128, 16, 128])
    gmask_d = din("gmask", [128, 2, 8, 4])
    ownhot_d = din("ownhot", [128, 2, 8, 4])
    c32_d = din("c32", [128, 3, 128])
    pow2_d = din("pow2", [128, NSTEP])
    y_d = nc.dram_tensor("y", [4 * SL, D], F32, kind="ExternalOutput").ap()
    Kc_d = nc.dram_tensor("Kc", [8, 128, S], BF16, kind="Internal").ap()
    Vc_d = nc.dram_tensor("Vc", [16, 128, 32, 66], BF16, kind="Internal").ap()
    kic_d = nc.dram_tensor("kic", [64, S], BF16, kind="Internal").ap()
    dbg = {}

    with ExitStack() as st:
        T = Tr(nc, st)

        def sb(name, shape, dt):
            return st.enter_context(nc.sbuf_tensor("sb_" + name, shape, dt))

        def ps(name, shape, dt):
            return st.enter_context(nc.psum_tensor("ps_" + name, shape, dt))

        R1 = sb("R1", [128, 57344], mybir.dt.uint8)

        def r1view(off, shape, dt):
            n = int(np.prod(shape[1:]))
            esz = 2 if dt == BF16 else 4
            v = R1[:, off:off + n * esz].bitcast(dt)
            if len(shape) == 3:
                v = v.rearrange("p (a b) -> p a b", a=shape[1])
            return v
        actT = r1view(0, [128, NFC, SL], BF16)
        xtok = r1view(22528, [128, 4, D], F32)
        wdb = [r1view(38912, [128, NFC, 128], BF16), r1view(44544, [128, NFC, 128], BF16)]
        sqb = [r1view(50176, [128, SL], F32), r1view(52224, [128, SL], F32)]
        maskT = r1view(0, [128, 32, SL], BF16)
        Isc = r1view(32768, [128, S], F32)
        msk = r1view(49152, [128, S], BF16)
        B_R1 = Buf()

        xT = sb("xT", [128, 8, SL], F32)
        xnT = sb("xnT", [128, 8, SL], BF16)
        wgub = [sb("wgu%d" % i, [128, 8, 2, 128], BF16) for i in range(3)]
        winb = [sb("winb%d" % i, [128, 8, 128], BF16) for i in range(3)]
        wvb = sb("wvb", [128, 8, 256], BF16)
        wkib = sb("wkib", [128, 8, 64], BF16)
        wwib = sb("wwib", [128, 8, 4], BF16)
        scr = [sb("scr%d" % i, [128, SL], F32) for i in range(4)]
        rstd = sb("rstd", [128, SL], F32)
        rstd2 = sb("rstd2", [128, SL], F32)
        mhalf = sb("mhalf", [128, SL], F32)
        qaT = sb("qaT", [128, 4, SL], BF16)
        qbT = sb("qbT", [128, 4, SL], BF16)
        qiT = sb("qiT", [128, 2, SL], BF16)
        wS = sb("wS", [128, 4, 4], F32)
        kiT2 = sb("kiT2", [128, S], BF16)
        kbuf = sb("kbuf", [128, S], BF16)
        vbuf = sb("vbuf", [128, 32, 66], BF16)
        pbuf = [sb("pbuf%d" % i, [128, SL], BF16) for i in range(3)]
        ebuf = [sb("ebuf%d" % i, [128, SL], BF16) for i in range(2)]
        AT = sb("AT", [128, 8, SL], BF16)
        mixT = sb("mixT", [128, 8, SL], BF16)
        Dt = sb("Dt", [128, 16, 384], BF16)
        esel = sb("esel", [128, 16, 128], BF16)
        idab = sb("idab", [128, 3, 128], BF16)
        c32 = sb("c32", [128, 3, 128], F32)
        gains = sb("gains", [128, 24], F32)
        bg = sb("bg", [128, 16], F32)
        gqk = sb("gqk", [128, 4], F32)
        gq8 = sb("gq8", [128, 2], F32)
        cb = sb("cb", [128, 32], F32)
        mtab = sb("mtab", [128, 896], BF16)
        pbv = sb("pbv", [128, 1], F32)
        gmask = sb("gmask", [128, 2, 8, 4], F32)
        ownhot = sb("ownhot", [128, 2, 8, 4], F32)
        pow2 = sb("pow2", [128, NSTEP], F32)
        kst = [sb("kst%d" % i, [128, SL], BF16) for i in range(2)]
        vst = sb("vst", [128, 4, 8, 66], BF16)
        kist = sb("kist", [64, SL], BF16)
        kms = sb("kms", [128, 4, 16], F32)
        kmb = sb("kmb", [128, 4, 16], BF16)
        G = sb("G", [128, 8, 16], F32)
        sel = sb("sel", [128, 8, 16], F32)
        m8 = sb("m8", [128, 8, 8], F32)
        thr = sb("thr", [128, 8], F32)
        negm = sb("negm", [128, 8, 32], BF16)
        negmT = sb("negmT", [128, 8, SL], BF16)
        small = sb("small", [128, 8], F32)
        steps = sb("steps", [128, NSTEP], F32)
        rd = rstd
        dtmp = scr[0][:, 0:384]
        cmask = scr[1][:, 0:384]

        ident_b = idab[:, 0, :]
        identA = idab[:, 1, :]
        identB = idab[:, 2, :]
        ident32 = c32[:, 0, :]
        ones32 = c32[:, 1, :]
        bones32 = c32[:, 2, :]

        pbank = [ps("pb%d" % i, [128, SL], F32) for i in range(4)]
        B_pbank = [Buf() for _ in range(4)]
        pobank = [ps("po%d" % i, [128, SL], F32) for i in range(2)]
        B_po = [Buf(), Buf()]
        pmisc = ps("pmisc", [128, SL], F32)
        B_misc = Buf()
        psb = ps("psb", [128, 8, 128], BF16)
        B_psb = Buf()
        pctr = [0]

        def nextbank():
            i = pctr[0] % 4
            pctr[0] += 1
            return pbank[i], B_pbank[i]

        B_xtok = Buf()
        B_xT = [Buf() for _ in range(8)]
        B_xnT = [Buf() for _ in range(8)]
        B_actT = [Buf() for _ in range(NFC)]
        B_wgu = [[Buf(), Buf()] for _ in range(3)]
        B_wd = [Buf(), Buf()]
        B_win = [Buf() for _ in range(3)]
        B_wv, B_wki, B_wwi = Buf(), Buf(), Buf()
        B_sq = [Buf(), Buf()]
        B_scr = [Buf() for _ in range(4)]
        B_rstd, B_rstd2 = Buf(), Buf()
        B_const = Buf()
        B_qaT = [Buf() for _ in range(4)]
        B_qbT = [Buf() for _ in range(4)]
        B_qiT = [Buf(), Buf()]
        B_wS = Buf()
        B_kiT2, B_kbuf, B_vbuf = Buf(), Buf(), Buf()
        B_pbuf = [Buf() for _ in range(3)]
        B_ebuf = [Buf(), Buf()]
        B_AT = [Buf() for _ in range(8)]
        B_mixT = [Buf() for _ in range(8)]
        B_kst = [Buf(), Buf()]
        B_vst, B_kist = Buf(), Buf()
        B_km = Buf()
        B_G, B_sel, B_m8, B_thr, B_negm, B_negmT = Buf(), Buf(), Buf(), Buf(), Buf(), Buf()
        B_small, B_steps, B_rd = Buf(), Buf(), Buf()
        B_Isc, B_msk = Buf(), Buf()
        B_maskT = [Buf() for _ in range(4)]
        B_Kc = [[Buf() for _ in range(8)] for _ in range(8)]
        B_Vc = [[[Buf() for _ in range(4)] for _ in range(8)] for _ in range(2)]
        B_kic = [Buf() for _ in range(8)]
        B_y = Buf()
        sctr = [0]

        def nextscr():
            i = sctr[0] % 4
            sctr[0] += 1
            return scr[i], B_scr[i]

        T.dma("pool", idab[:], idab_d, writes=[B_const])
        T.dma("pool", esel[:], esel_d, writes=[B_const])
        T.dma("pool", mtab[:], mtab_d, writes=[B_const])
        for dst, src in [(c32, c32_d), (gains, gains_d), (bg, bg_d), (gqk, gqk_d), (pbv, pbv_d),
                         (gmask, gmask_d), (ownhot, ownhot_d), (pow2, pow2_d)]:
            T.dma("sp", dst[:], src, writes=[B_const])
        T.dma("sp", cmask, cmask_d, writes=[B_scr[1]])
        T.dma("sp", cb[:, 0:16], cb_d, writes=[B_const])
        T.op("dve", lambda e: e.memset(mhalf[:], -0.5), writes=[B_const])
        T.op("dve", lambda e: e.memset(vst[:], 1.0), writes=[B_vst])
        T.op("dve", lambda e: e.memset(negm[:], 0.0), writes=[B_negm])
        T.op("dve", lambda e: e.memset(negmT[:], 0.0), writes=[B_negmT])
        T.op("dve", lambda e: e.tensor_scalar(out=gq8[:], in0=gqk[:, 0:2], scalar1=0.125, scalar2=None, op0=ALU.mult),
             reads=[B_const], writes=[B_const])
        T.op("dve", lambda e: e.tensor_scalar(out=cb[:, 16:32], in0=cb[:, 0:16], scalar1=pbv[:, 0:1], scalar2=None, op0=ALU.add),
             reads=[B_const], writes=[B_const])
        for h in range(16):
            T.dma("sp", dtmp, dtab_d[:, h, :], writes=[B_scr[0]])
            T.op("dve", lambda e, h=h: e.scalar_tensor_tensor(out=Dt[:, h, :], in0=dtmp, scalar=cb[:, h:h + 1], in1=cmask,
                                                             op0=ALU.subtract, op1=ALU.add),
                 reads=[B_scr[0], B_scr[1], B_const], writes=[B_const])

        def mmgroup(out_ap, obuf, items, extra_reads=()):
            n = len(items)
            for i, (l, r, rb) in enumerate(items):
                T.op("pe", lambda e, l=l, r=r, i=i: e.matmul(out_ap, lhsT=l, rhs=r, start=(i == 0), stop=(i == n - 1)),
                     reads=list(rb) + list(extra_reads), writes=[obuf], mark=(i == n - 1))

        win_v = win_d.rearrange("(kc p) n -> p kc n", p=128)

        def rmsnorm(gcol0):
            for kc in range(8):
                sq, bsq = sqb[kc % 2], B_sq[kc % 2]
                T.op("act", lambda e, kc=kc, sq=sq: e.activation(out=sq, in_=xT[:, kc, :], func=AF.Square),
                     reads=[B_xT[kc]], writes=[bsq])
                T.op("pe", lambda e, kc=kc, sq=sq: e.matmul(pmisc[:], lhsT=ones32, rhs=sq, start=(kc == 0), stop=(kc == 7)),
                     reads=[bsq, B_const], writes=[B_misc], mark=True)
            T.op("dve", lambda e: e.tensor_scalar(out=rstd[:], in0=pmisc[:], scalar1=1.0 / D, scalar2=EPS, op0=ALU.mult, op1=ALU.add),
                 reads=[B_misc], writes=[B_rstd])
            T.op("pool", lambda e: e.tensor_tensor(out=rstd2[:], in0=rstd[:], in1=mhalf[:], op=ALU.pow),
                 reads=[B_rstd, B_const], writes=[B_rstd2])
            for kc in range(8):
                T.op("dve", lambda e, kc=kc: e.scalar_tensor_tensor(out=xnT[:, kc, :], in0=xT[:, kc, :],
                                                                   scalar=gains[:, gcol0 + kc:gcol0 + kc + 1], in1=rstd2[:],
                                                                   op0=ALU.mult, op1=ALU.mult),
                     reads=[B_xT[kc], B_rstd2, B_const], writes=[B_xnT[kc]])

        def ffn(wgu_d, wd_d):
            wgu_v = wgu_d.rearrange("(kc p) (two f) -> p kc two f", p=128, two=2)
            wd_v = wd_d.rearrange("(c p) n -> p c n", p=128)

            def load_gu(c):
                T.dma("pool", wgub[c % 3][:, :, 0, :], wgu_v[:, :, 0, c * 128:(c + 1) * 128], writes=[B_wgu[c % 3][0]])
                T.dma("pool", wgub[c % 3][:, :, 1, :], wgu_v[:, :, 1, c * 128:(c + 1) * 128], writes=[B_wgu[c % 3][1]])

            def load_d(cc):
                T.dma("pool", wdb[cc % 2], wd_v[:, :, cc * 128:(cc + 1) * 128], writes=[B_wd[cc % 2]])
            load_gu(0)
            load_gu(1)
            for c in range(NFC):
                if c + 2 < NFC:
                    load_gu(c + 2)
                w = wgub[c % 3]
                gb_, bgb = nextbank()
                ub_, bub = nextbank()
                mmgroup(gb_[:], bgb, [(w[:, kc, 0, :], xnT[:, kc, :], [B_wgu[c % 3][0], B_xnT[kc]]) for kc in range(8)])
                mmgroup(ub_[:], bub, [(w[:, kc, 1, :], xnT[:, kc, :], [B_wgu[c % 3][1], B_xnT[kc]]) for kc in range(8)])
                sg, bsg = nextscr()
                T.op("act", lambda e, sg=sg, gb_=gb_: e.activation(out=sg[:], in_=gb_[:], func=AF.Silu), reads=[bgb], writes=[bsg])
                T.op("dve", lambda e, sg=sg, ub_=ub_, c=c: e.tensor_tensor(out=actT[:, c, :], in0=ub_[:], in1=sg[:], op=ALU.mult),
                     reads=[bub, bsg], writes=[B_actT[c]])
                if c == NFC - 3:
                    load_d(0)
                if c == NFC - 2:
                    load_d(1)
            for cc in range(8):
                w = wdb[cc % 2]
                bk, bbk = nextbank()
                mmgroup(bk[:], bbk, [(w[:, c, :], actT[:, c, :], [B_wd[cc % 2], B_actT[c]]) for c in range(NFC)])
                T.op("dve", lambda e, bk=bk, cc=cc: e.scalar_tensor_tensor(out=xT[:, cc, :], in0=bk[:], scalar=0.5, in1=xT[:, cc, :],
                                                                          op0=ALU.mult, op1=ALU.add),
                     reads=[bbk, B_xT[cc]], writes=[B_xT[cc]])
                if cc + 2 < 8:
                    load_d(cc + 2)

        wctr = [0]

        def load_win(col0):
            i = wctr[0] % 3
            wctr[0] += 1
            T.dma("pool", winb[i][:], win_v[:, :, col0:col0 + 128], writes=[B_win[i]])
            return winb[i], B_win[i]

        def proj_pair_normed(col0, gain_ap, out_ap, obufs):
            w, bw = load_win(col0)
            bk, bbk = nextbank()
            mmgroup(bk[:], bbk, [(w[:, kc, :], xnT[:, kc, :], [bw, B_xnT[kc]]) for kc in range(8)])
            raw, braw = nextscr()
            sq, bsq = nextscr()
            T.op("act", lambda e: e.activation(out=raw[:], in_=bk[:], func=AF.Copy), reads=[bbk], writes=[braw])
            T.op("act", lambda e: e.activation(out=sq[:], in_=bk[:], func=AF.Square), reads=[bbk], writes=[bsq])
            T.op("pe", lambda e: e.matmul(pmisc[:], lhsT=bones32, rhs=sq[:], start=True, stop=True),
                 reads=[bsq, B_const], writes=[B_misc])
            T.op("dve", lambda e: e.tensor_scalar(out=rstd[:], in0=pmisc[:], scalar1=1.0 / 64, scalar2=EPS, op0=ALU.mult, op1=ALU.add),
                 reads=[B_misc], writes=[B_rstd])
            T.op("pool", lambda e: e.tensor_tensor(out=rstd2[:], in0=rstd[:], in1=mhalf[:], op=ALU.pow),
                 reads=[B_rstd, B_const], writes=[B_rstd2])
            T.op("dve", lambda e: e.scalar_tensor_tensor(out=out_ap, in0=raw[:], scalar=gain_ap, in1=rstd2[:], op0=ALU.mult, op1=ALU.mult),
                 reads=[braw, B_rstd2, B_const], writes=obufs)

        def phase1(slot, own, m):
            T.dma("sp", xtok, x_d[slot * SL:(slot + 1) * SL, :].rearrange("(tt p) f -> p tt f", p=128), writes=[B_xtok])
            for kc in range(8):
                bk, bbk = nextbank()
                for tt in range(4):
                    T.op("pe", lambda e, bk=bk, tt=tt, kc=kc: e.transpose(out=bk[:, tt * 128:(tt + 1) * 128],
                                                                         in_=xtok[:, tt, kc * 128:(kc + 1) * 128], identity=ident32),
                         reads=[B_xtok, B_const], writes=[bbk], mark=(tt == 3))
                if kc % 2 == 0:
                    T.op("act", lambda e, bk=bk, kc=kc: e.activation(out=xT[:, kc, :], in_=bk[:], func=AF.Copy), reads=[bbk], writes=[B_xT[kc]])
                else:
                    T.op("dve", lambda e, bk=bk, kc=kc: e.tensor_copy(out=xT[:, kc, :], in_=bk[:]), reads=[bbk], writes=[B_xT[kc]])
            chk(1)
            rmsnorm(0)
            chk(2)
            ffn(w1gu_d, w1d_d)
            chk(3)
            rmsnorm(8)
            for grp, col_base, gcol in ((0, 512, 2), (1, 2048, 3)):
                for pr in range(4):
                    ks, bks = kst[pr % 2], B_kst[pr % 2]
                    proj_pair_normed(col_base + pr * 128, gqk[:, gcol:gcol + 1], ks[:], [bks])
                    if grp == 1:
                        T.op("dve", lambda e, ks=ks, pr=pr: e.tensor_reduce(out=kms[:, pr, 2 * slot:2 * slot + 2],
                                                                           in_=ks[:].rearrange("p (a b) -> p a b", a=2),
                                                                           axis=AX.X, op=ALU.add),
                             reads=[bks], writes=[B_km])
                        T.op("dve", lambda e, pr=pr: e.tensor_scalar(out=kmb[:, pr, 2 * slot:2 * slot + 2], in0=kms[:, pr, 2 * slot:2 * slot + 2],
                                                                    scalar1=1.0 / 256, scalar2=None, op0=ALU.mult),
                             reads=[B_km], writes=[B_km])
                    T.dma("sp", Kc_d[grp * 4 + pr, :, slot * SL:(slot + 1) * SL], ks[:], reads=[bks], writes=[B_Kc[grp * 4 + pr][slot]])
            for grp, col_base in ((0, 1024), (1, 2560)):
                for half in range(2):
                    T.dma("pool", wvb[:], win_v[:, :, col_base + half * 256:col_base + (half + 1) * 256], writes=[B_wv])
                    for tt in range(4):
                        bk, bbk = nextbank()
                        mmgroup(bk[:, 0:256], bbk, [(xnT[:, kc, tt * 128:(tt + 1) * 128], wvb[:, kc, :], [B_wv, B_xnT[kc]]) for kc in range(8)])
                        T.op("act", lambda e, bk=bk, tt=tt, half=half: e.activation(
                            out=vst[:, tt, half * 4:(half + 1) * 4, 0:64],
                            in_=bk[:, 0:256].rearrange("p (h d) -> p h d", h=4), func=AF.Copy),
                            reads=[bbk], writes=[B_vst])
                for tt in range(4):
                    T.dma("sp", Vc_d[grp * 8:(grp + 1) * 8].rearrange("h p t d -> p t h d")[:, slot * 4 + tt, :, :], vst[:, tt, :, :],
                          reads=[B_vst], writes=[B_Vc[grp][slot][tt]])
            T.dma("pool", wkib[:], win_v[:, :, 3328:3392], writes=[B_wki])
            bk, bbk = nextbank()
            mmgroup(bk[0:64, :], bbk, [(wkib[:, kc, :], xnT[:, kc, :], [B_wki, B_xnT[kc]]) for kc in range(8)])
            T.op("act", lambda e: e.activation(out=kist[:], in_=bk[0:64, :], func=AF.Copy), reads=[bbk], writes=[B_kist])
            T.dma("sp", kic_d[:, slot * SL:(slot + 1) * SL], kist[:], reads=[B_kist], writes=[B_kic[slot]])
            if not own:
                return
            for pr in range(4):
                proj_pair_normed(pr * 128, gq8[:, 0:1], qaT[:, pr, :], [B_qaT[pr]])
            for pr in range(4):
                proj_pair_normed(1536 + pr * 128, gq8[:, 1:2], qbT[:, pr, :], [B_qbT[pr]])
            for pr in range(2):
                w, bw = load_win(3072 + pr * 128)
                bk, bbk = nextbank()
                mmgroup(bk[:], bbk, [(w[:, kc, :], xnT[:, kc, :], [bw, B_xnT[kc]]) for kc in range(8)])
                T.op("act", lambda e, bk=bk, pr=pr: e.activation(out=qiT[:, pr, :], in_=bk[:], func=AF.Copy), reads=[bbk], writes=[B_qiT[pr]])
            T.dma("pool", wwib[:], win_v[:, :, 3392:3396], writes=[B_wwi])
            for tt in range(4):
                bk, bbk = nextbank()
                mmgroup(bk[:, 0:4], bbk, [(xnT[:, kc, tt * 128:(tt + 1) * 128], wwib[:, kc, :], [B_wwi, B_xnT[kc]]) for kc in range(8)])
                T.op("dve", lambda e, bk=bk, tt=tt: e.tensor_scalar(out=wS[:, tt, :], in0=bk[:, 0:4], scalar1=0.0625, scalar2=None, op0=ALU.mult),
                     reads=[bbk], writes=[B_wS])

        def attention(m):
            nsl = 2 * m + 2
            nk = nsl * SL
            nkt = nsl * 4
            nb = nsl * 2
            k0 = 1024 * m
            for hf in range(2):
                T.dma("sp", kiT2[hf * 64:(hf + 1) * 64, 0:nk], kic_d[:, 0:nk], reads=B_kic[0:nsl], writes=[B_kiT2])
            for r in range(4):
                for j in range(nsl):
                    banks = [nextbank() for _ in range(4)]
                    for h in range(4):
                        p0 = 64 * (h % 2)
                        bk, bbk = banks[h]
                        mmgroup(bk[:], bbk, [(qiT[p0:p0 + 64, h // 2, r * 128:(r + 1) * 128], kiT2[p0:p0 + 64, j * SL:(j + 1) * SL],
                                              [B_qiT[h // 2], B_kiT2])])
                    for h in range(4):
                        bk, bbk = banks[h]
                        rl, brl = nextscr()
                        T.op("act", lambda e, bk=bk, rl=rl: e.activation(out=rl[:], in_=bk[:], func=AF.Relu), reads=[bbk], writes=[brl])
                        dst = Isc[:, j * SL:(j + 1) * SL]
                        if h == 0:
                            T.op("dve", lambda e, rl=rl, dst=dst: e.tensor_scalar(out=dst, in0=rl[:], scalar1=wS[:, r, 0:1], scalar2=None, op0=ALU.mult),
                                 reads=[brl, B_wS], writes=[B_Isc])
                        else:
                            T.op("dve", lambda e, rl=rl, dst=dst, h=h: e.scalar_tensor_tensor(out=dst, in0=rl[:], scalar=wS[:, r, h:h + 1], in1=dst,
                                                                                            op0=ALU.mult, op1=ALU.add),
                                 reads=[brl, B_wS, B_Isc], writes=[B_Isc])
                chk(6 if m == 0 else (12.31 if r == 0 else 12.39))
                Bv, lo, mid, cnt, tt_ = (small[:, i:i + 1] for i in range(5))
                T.op("dve", lambda e: e.tensor_reduce(out=Bv, in_=Isc[:, 0:nk], axis=AX.X, op=ALU.max, apply_absolute_value=True),
                     reads=[B_Isc], writes=[B_small])
                if m == 1 and r == 0:
                    chk(12.311)
                T.op("dve", lambda e: e.tensor_scalar(out=lo, in0=Bv, scalar1=1.0, scalar2=-1.0, op0=ALU.add, op1=ALU.mult),
                     reads=[B_small], writes=[B_small])
                T.op("dve", lambda e: e.tensor_scalar(out=steps[:], in0=pow2[:], scalar1=lo, scalar2=-2.0, op0=ALU.mult, op1=ALU.mult),
                     reads=[B_small, B_const], writes=[B_steps])
                T.op("dve", lambda e: e.tensor_tensor(out=Isc[:, k0:k0 + SL], in0=Isc[:, k0:k0 + SL],
                                                      in1=mtab[:, 384 - 128 * r:896 - 128 * r], op=ALU.add),
                     reads=[B_Isc, B_const], writes=[B_Isc])
                T.op("dve", lambda e: e.tensor_scalar(out=Isc[:, k0 + SL:k0 + 2 * SL], in0=Isc[:, k0 + SL:k0 + 2 * SL], scalar1=pbv[:, 0:1],
                                                      scalar2=None, op0=ALU.add),
                     reads=[B_Isc, B_const], writes=[B_Isc])
                if m == 1 and r == 0:
                    chk(12.312)
                for s_ in range(NSTEP):
                    if m == 1 and r == 0 and s_ == 1:
                        chk(12.313)
                    T.op("dve", lambda e, s_=s_: e.tensor_tensor(out=mid, in0=lo, in1=steps[:, s_:s_ + 1], op=ALU.add),
                         reads=[B_small, B_steps], writes=[B_small])
                    T.op("dve", lambda e: e.tensor_scalar(out=msk[:, 0:nk], in0=Isc[:, 0:nk], scalar1=mid, scalar2=0.0, op0=ALU.is_ge, op1=ALU.add,
                                                          accum_out=cnt),
                         reads=[B_Isc, B_small], writes=[B_msk, B_small])
                    T.op("dve", lambda e, s_=s_: e.tensor_scalar(out=tt_, in0=cnt, scalar1=256.0, scalar2=steps[:, s_:s_ + 1], op0=ALU.is_ge, op1=ALU.mult),
                         reads=[B_small, B_steps], writes=[B_small])
                    T.op("dve", lambda e: e.tensor_tensor(out=lo, in0=lo, in1=tt_, op=ALU.add), reads=[B_small], writes=[B_small])
                T.op("dve", lambda e: e.tensor_scalar(out=msk[:, 0:nk], in0=Isc[:, 0:nk], scalar1=lo, scalar2=None, op0=ALU.is_ge),
                     reads=[B_Isc, B_small], writes=[B_msk])
                if m == 1 and r == 0:
                    chk(12.314)
                for g0 in range(0, nkt, 8):
                    if m == 1 and r == 0 and g0 == 8:
                        chk(12.315)
                    for i in range(8):
                        kt = g0 + i
                        T.op("pe", lambda e, i=i, kt=kt: e.transpose(out=psb[:, i, :], in_=msk[:, kt * 128:(kt + 1) * 128], identity=ident_b),
                             reads=[B_msk, B_const], writes=[B_psb], mark=(i == 7))
                    T.op("act", lambda e, g0=g0: e.activation(out=maskT[:, g0:g0 + 8, r * 128:(r + 1) * 128], in_=psb[:], func=AF.Copy),
                         reads=[B_psb], writes=[B_maskT[r]])
                chk(7 if m == 0 else (12.32 if r == 0 else 12.39))
                half = r // 2
                T.op("dve", lambda e: e.memset(G[:], -BIG), writes=[B_G])
                gbo, bgbo = nextbank()
                for par, (gbk, bgbk) in enumerate(((pmisc, B_misc), (gbo, bgbo))):
                    p0 = 64 * par
                    for pr_ in range(4):
                        h = 2 * pr_ + par
                        T.op("pe", lambda e, h=h, p0=p0, pr_=pr_, gbk=gbk: e.matmul(gbk[:, h * 16:h * 16 + nb], lhsT=qbT[p0:p0 + 64, pr_, r * 128:(r + 1) * 128],
                                                                                  rhs=kmb[p0:p0 + 64, pr_, 0:nb], start=True, stop=True),
                             reads=[B_qbT[pr_], B_km], writes=[bgbk], mark=(pr_ == 3))
                    T.op("dve", lambda e, par=par, gbk=gbk: e.tensor_copy(
                        out=G[:].rearrange("p (hp two) b -> p hp two b", two=2)[:, :, par, 0:nb],
                        in_=gbk[:, 0:128].rearrange("p (hp two b) -> p hp two b", two=2, b=16)[:, :, par, 0:nb]),
                        reads=[bgbk], writes=[B_G])
                T.op("dve", lambda e: e.tensor_tensor(out=G[:, :, 4 * m:4 * m + 4], in0=G[:, :, 4 * m:4 * m + 4], in1=gmask[:, half, :, :], op=ALU.add),
                     reads=[B_G, B_const], writes=[B_G])
                for h in range(8):
                    T.op("dve", lambda e, h=h: e.max(out=m8[:, h, :], in_=G[:, h, :]), reads=[B_G], writes=[B_m8])
                T.op("dve", lambda e: e.tensor_scalar(out=thr[:], in0=m8[:, :, 2], scalar1=-BIG / 2, scalar2=None, op0=ALU.max),
                     reads=[B_m8], writes=[B_thr])
                for h in range(8):
                    T.op("dve", lambda e, h=h: e.tensor_scalar(out=sel[:, h, :], in0=G[:, h, :], scalar1=thr[:, h:h + 1], scalar2=None, op0=ALU.is_ge),
                         reads=[B_G, B_thr], writes=[B_sel])
                T.op("dve", lambda e: e.tensor_tensor(out=sel[:, :, 4 * m:4 * m + 4], in0=sel[:, :, 4 * m:4 * m + 4], in1=ownhot[:, half, :, :], op=ALU.add),
                     reads=[B_sel, B_const], writes=[B_sel])
                T.op("dve", lambda e: e.tensor_scalar(out=negm[:, :, 0:16], in0=sel[:], scalar1=-1.0, scalar2=BIG, op0=ALU.add, op1=ALU.mult),
                     reads=[B_sel], writes=[B_negm])
                for h in range(8):
                    T.op("pe", lambda e, h=h: e.transpose(out=psb[0:32, h, :], in_=negm[:, h, :], identity=ident_b),
                         reads=[B_negm, B_const], writes=[B_psb], mark=(h == 7))
                T.op("act", lambda e: e.activation(out=negmT[0:32, :, r * 128:(r + 1) * 128], in_=psb[0:32, :, :], func=AF.Copy),
                     reads=[B_psb], writes=[B_negmT])
                if m == 1 and r == 0:
                    chk(12.33)

            chk(8 if m == 0 else 12.4)
            pbi = [0]
            for hg in range(16):
                moba = hg >= 8
                h = hg % 8
                pr = h // 2
                p0 = 64 * (h % 2)
                grp = 1 if moba else 0
                if h % 2 == 0:
                    T.dma("sp", kbuf[:, 0:nk], Kc_d[grp * 4 + pr, :, 0:nk], reads=B_Kc[grp * 4 + pr][0:nsl], writes=[B_kbuf])
                T.dma("sp", vbuf[:, 0:nkt, :], Vc_d[hg, :, 0:nkt, :], reads=[b for sl_ in B_Vc[grp][0:nsl] for b in sl_], writes=[B_vbuf])
                qT = qbT if moba else qaT
                bq = (B_qbT if moba else B_qaT)[pr]
                po, bpo = pobank[hg % 2], B_po[hg % 2]
                g, jj = h // 3, h % 3
                for kt in range(nkt):
                    j = kt - 8 * m
                    q0 = 128 * j if 0 <= j < 4 else 0
                    sbk, bsbk = nextbank()
                    items = [(sbk[:, q0:SL], kbuf[p0:p0 + 64, kt * 128:(kt + 1) * 128], qT[p0:p0 + 64, pr, q0:SL], [B_kbuf, bq])]
                    if 0 <= j < 4:
                        W = min(384, SL - q0)
                        items.append((sbk[:, q0:q0 + W], ident_b, Dt[:, hg, 0:W], [B_const]))
                    if m >= 1 and kt == 8 * m - 1:
                        items.append((sbk[:, 0:256], identA, Dt[:, hg, 128:384], [B_const]))
                    if kt == 8 * m + 7:
                        items.append((sbk[:, 0:256], identB, Dt[:, hg, 128:384], [B_const]))
                    if moba:
                        items.append((sbk[:, q0:SL], esel[:, kt // 2, :], negmT[:, h, q0:SL], [B_const, B_negmT]))
                    n = len(items)
                    for i, (o, l, rr, rb) in enumerate(items):
                        T.op("pe", lambda e, o=o, l=l, rr=rr, i=i, n=n: e.matmul(o, lhsT=l, rhs=rr, start=(i == 0), stop=(i == n - 1)),
                             reads=rb, writes=[bsbk], mark=(i == n - 1))
                    if hg == 0 and kt == 0:
                        chk(8.31)
                    pb_, bpb = pbuf[pbi[0] % 3], B_pbuf[pbi[0] % 3]
                    pbi[0] += 1
                    col = hg + (16 if kt >= 8 * m + 4 else 0)
                    if moba:
                        eb_, beb = pb_, bpb
                    else:
                        eb_, beb = ebuf[pbi[0] % 2], B_ebuf[pbi[0] % 2]
                    T.op("act", lambda e, eb_=eb_, sbk=sbk, q0=q0, col=col: e.activation(out=eb_[:, q0:SL], in_=sbk[:, q0:SL], func=AF.Exp,
                                                                                        bias=cb[:, col:col + 1], scale=1.0),
                         reads=[bsbk, B_const], writes=[beb])
                    if hg == 0 and kt == 0:
                        chk(8.32)
                    if not moba:
                        T.op("dve", lambda e, pb_=pb_, eb_=eb_, q0=q0, kt=kt: e.scalar_tensor_tensor(out=pb_[:, q0:SL], in0=eb_[:, q0:SL], scalar=1.0,
                                                                                                    in1=maskT[:, kt, q0:SL], op0=ALU.mult, op1=ALU.mult),
                             reads=[beb] + B_maskT, writes=[bpb])
                    if hg == 0 and kt == 0:
                        chk(8.33)
                    T.op("pe", lambda e, po=po, pb_=pb_, q0=q0, kt=kt: e.matmul(po[0:65, q0:SL], lhsT=vbuf[:, kt, 0:65], rhs=pb_[:, q0:SL],
                                                                               start=(kt == 0), stop=(kt == nkt - 1)),
                         reads=[B_vbuf, bpb], writes=[bpo], mark=True)
                    if hg == 0:
                        chk(8.34 + 0.005 * kt)
                chk(8.4 if hg == 0 else (8.7 if hg == 8 else 8.99))
                T.op("dve", lambda e, po=po: e.reciprocal(out=rd[64:65, :], in_=po[64:65, :]), reads=[bpo], writes=[B_rd])
                if hg == 0:
                    chk(8.41)
                T.op("pe", lambda e: e.matmul(pmisc[:], lhsT=ones32[64:65, :], rhs=rd[64:65, :], start=True, stop=True),
                     reads=[B_rd, B_const], writes=[B_misc])
                if hg == 0:
                    chk(8.42)
                tmpo, btmpo = nextscr()
                T.op("act", lambda e, po=po, tmpo=tmpo, p0=p0: e.activation(out=tmpo[p0:p0 + 64, :], in_=po[0:64, :], func=AF.Copy),
                     reads=[bpo], writes=[btmpo])
                ap_ = pr + (4 if moba else 0)
                if hg == 0:
                    chk(8.43)
                T.op("dve", lambda e, tmpo=tmpo, p0=p0, ap_=ap_: e.tensor_tensor(out=AT[p0:p0 + 64, ap_, :], in0=pmisc[p0:p0 + 64, :],
                                                                                 in1=tmpo[p0:p0 + 64, :], op=ALU.mult),
                     reads=[btmpo, B_misc], writes=[B_AT[ap_]])
                chk(8.5 if hg == 0 else (8.6 if hg == 7 else (8.8 if hg == 8 else 8.99)))

        def mix(m):
            wba_v = wba_d.rearrange("(kc p) n -> p kc n", p=128)
            wbb_v = wbb_d.rearrange("(kc p) n -> p kc n", p=128)
            wo_v = wo_d.rearrange("(kc p) n -> p kc n", p=128)
            for cc in range(8):
                wa, bwa = winb[wctr[0] % 3], B_win[wctr[0] % 3]
                wctr[0] += 1
                T.dma("pool", wa[:, 0:4, :], wba_v[:, :, cc * 128:(cc + 1) * 128], writes=[bwa])
                T.dma("pool", wa[:, 4:8, :], wbb_v[:, :, cc * 128:(cc + 1) * 128], writes=[bwa])
                wga, bwga = load_win(3396 + cc * 128)
                wgb, bwgb = load_win(3396 + 1024 + cc * 128)
                bA, bbA = nextbank()
                bB, bbB = nextbank()
                bGa, bbGa = nextbank()
                bGb, bbGb = nextbank()
                mmgroup(bA[:], bbA, [(wa[:, kc, :], AT[:, kc, :], [bwa, B_AT[kc]]) for kc in range(4)])
                mmgroup(bB[:], bbB, [(wa[:, 4 + kc, :], AT[:, 4 + kc, :], [bwa, B_AT[4 + kc]]) for kc in range(4)])
                mmgroup(bGa[:], bbGa, [(wga[:, kc, :], xnT[:, kc, :], [bwga, B_xnT[kc]]) for kc in range(8)])
                mmgroup(bGb[:], bbGb, [(wgb[:, kc, :], xnT[:, kc, :], [bwgb, B_xnT[kc]]) for kc in range(8)])
                ga, bga = nextscr()
                gb2, bgb2 = nextscr()
                T.op("act", lambda e, ga=ga, bGa=bGa, cc=cc: e.activation(out=ga[:], in_=bGa[:], func=AF.Sigmoid, bias=bg[:, cc:cc + 1], scale=1.0),
                     reads=[bbGa, B_const], writes=[bga])
                T.op("act", lambda e, gb2=gb2, bGb=bGb, cc=cc: e.activation(out=gb2[:], in_=bGb[:], func=AF.Sigmoid, bias=bg[:, 8 + cc:9 + cc], scale=1.0),
                     reads=[bbGb, B_const], writes=[bgb2])
                T.op("dve", lambda e, ga=ga, bA=bA: e.tensor_tensor(out=ga[:], in0=bA[:], in1=ga[:], op=ALU.mult), reads=[bbA, bga], writes=[bga])
                T.op("dve", lambda e, gb2=gb2, bB=bB: e.tensor_tensor(out=gb2[:], in0=bB[:], in1=gb2[:], op=ALU.mult), reads=[bbB, bgb2], writes=[bgb2])
                T.op("dve", lambda e, ga=ga, gb2=gb2, cc=cc: e.tensor_tensor(out=mixT[:, cc, :], in0=ga[:], in1=gb2[:], op=ALU.add),
                     reads=[bga, bgb2], writes=[B_mixT[cc]])
            for cc in range(8):
                w, bw = winb[wctr[0] % 3], B_win[wctr[0] % 3]
                wctr[0] += 1
                T.dma("pool", w[:], wo_v[:, :, cc * 128:(cc + 1) * 128], writes=[bw])
                bk, bbk = nextbank()
                mmgroup(bk[:], bbk, [(w[:, kc, :], mixT[:, kc, :], [bw, B_mixT[kc]]) for kc in range(8)])
                T.op("dve", lambda e, bk=bk, cc=cc: e.tensor_tensor(out=xT[:, cc, :], in0=bk[:], in1=xT[:, cc, :], op=ALU.add),
                     reads=[bbk, B_xT[cc]], writes=[B_xT[cc]])

        def output(m):
            for tt in range(4):
                for half in range(2):
                    bk, bbk = nextbank()
                    for i in range(4):
                        kc = half * 4 + i
                        T.op("pe", lambda e, bk=bk, i=i, kc=kc, tt=tt: e.transpose(out=bk[:, i * 128:(i + 1) * 128],
                                                                                  in_=xT[:, kc, tt * 128:(tt + 1) * 128], identity=ident32),
                             reads=[B_xT[kc], B_const], writes=[bbk], mark=(i == 3))
                    if half == 0:
                        T.op("act", lambda e, bk=bk, tt=tt: e.activation(out=xtok[:, tt, 0:512], in_=bk[:], func=AF.Copy), reads=[bbk], writes=[B_xtok])
                    else:
                        T.op("dve", lambda e, bk=bk, tt=tt: e.tensor_copy(out=xtok[:, tt, 512:1024], in_=bk[:]), reads=[bbk], writes=[B_xtok])
            T.dma("sp", y_d[m * SL:(m + 1) * SL, :].rearrange("(tt p) f -> p tt f", p=128), xtok, reads=[B_xtok], writes=[B_y])

        try:
            chk(0)
            for m in range(4):
                phase1(2 * m + 1, False, m)
                chk(4 if m == 0 else 12.2)
                phase1(2 * m, True, m)
                chk(5 if m == 0 else 12.3)
                T.barrier()
                attention(m)
                chk(9 if m == 0 else 12.5)
                T.barrier()
                mix(m)
                chk(10 if m == 0 else 12.6)
                rmsnorm(16)
                ffn(w2gu_d, w2d_d)
                chk(11)
                output(m)
                chk(12 + m)
        except StopBuild:
            pass
        T.barrier()
    return nc


def _t5_bucket(d):
    d = np.maximum(d, 0)
    df = np.maximum(d, 1).astype(np.float64)
    large = 16 + (np.log(df / 16.0) / math.log(128 / 16) * 16).astype(np.int64)
    large = np.minimum(large, 31)
    return np.where(d < 16, d, large)


def _consts(p, rel_bias):
    c = {}
    s = np.arange(128)[:, None]
    cc = np.arange(384)[None, :]
    bucket = _t5_bucket(cc - s)
    c["dtab"] = np.ascontiguousarray(np.transpose(rel_bias[bucket], (0, 2, 1))).astype(np.float32)
    c["cb"] = np.ascontiguousarray(np.broadcast_to(rel_bias[31][None, :], (128, 16))).astype(np.float32)
    c["cmask"] = np.where(cc >= s, 0.0, -BIG).astype(np.float32)
    c2 = np.arange(896)[None, :]
    c["mtab"] = np.where(c2 <= s + 384, 0.0, -BIG).astype(np.float32)
    pb = 0.0 if p == 1 else -BIG
    c["pbv"] = np.full((128, 1), pb, np.float32)
    eye = np.eye(128, dtype=np.float32)
    c["idab"] = np.ascontiguousarray(np.stack([eye, eye * (1.0 if p == 0 else 0.0), eye * (1.0 if p == 1 else 0.0)], axis=1))
    pp = np.arange(128)
    es = np.zeros((128, 16, 128), np.float32)
    for b in range(16):
        es[pp == b, b, :] = 1.0
    c["esel"] = es
    gm = np.zeros((2, 4), np.float32)
    oh = np.zeros((2, 4), np.float32)
    gm[0] = [-BIG, -BIG, pb, pb]
    oh[0] = [1, 0, 0, 0]
    gm[1] = [0.0, -BIG, pb, pb]
    oh[1] = [0, 1, 0, 0]
    c["gmask"] = np.ascontiguousarray(np.broadcast_to(gm[None, :, None, :], (128, 2, 8, 4))).astype(np.float32)
    c["ownhot"] = np.ascontiguousarray(np.broadcast_to(oh[None, :, None, :], (128, 2, 8, 4))).astype(np.float32)
    bo = np.zeros((128, 128), np.float32)
    bo[0:64, 0:64] = 1.0
    bo[64:128, 64:128] = 1.0
    c["c32"] = np.ascontiguousarray(np.stack([eye, np.ones((128, 128), np.float32), bo], axis=1))
    c["pow2"] = np.ascontiguousarray(np.broadcast_to((0.5 ** (np.arange(NSTEP) + 1))[None, :], (128, NSTEP))).astype(np.float32)
    return c


_NC_CACHE = {}


def make_in_maps(**inp):
    f = lambda k: np.asarray(inp[k], dtype=np.float32)
    x = f("x")
    fm = lambda v: np.ascontiguousarray(v.reshape(8, 128).T)
    gains = np.concatenate([fm(f("ffn1_norm")[0]), fm(f("mix_norm")[0]), fm(f("ffn2_norm")[0])], axis=1).astype(np.float32)
    bgv = np.ascontiguousarray(f("b_gate")[0].reshape(16, 128).T)
    gqk = np.stack([np.tile(f("q_norm_dsa")[0], 2), np.tile(f("q_norm_moba")[0], 2),
                    np.tile(f("k_norm_dsa")[0], 2), np.tile(f("k_norm_moba")[0], 2)], axis=1).astype(np.float32)
    rel_bias = f("rel_bias")
    shared = {
        "w1gu": np.ascontiguousarray(f("ffn1_w_gu")[0]), "w1d": np.ascontiguousarray(f("ffn1_w_down")[0]),
        "win": np.ascontiguousarray(f("w_in")[0]), "wba": np.ascontiguousarray(f("w_branch_dsa")[0]),
        "wbb": np.ascontiguousarray(f("w_branch_moba")[0]), "wo": np.ascontiguousarray(f("w_out")[0]),
        "w2gu": np.ascontiguousarray(f("ffn2_w_gu")[0]), "w2d": np.ascontiguousarray(f("ffn2_w_down")[0]),
        "gains": np.ascontiguousarray(gains), "bg": bgv, "gqk": np.ascontiguousarray(gqk),
    }
    cons = [_consts(0, rel_bias), _consts(1, rel_bias)]
    in_maps = []
    for core in range(8):
        b, p = core // 2, core % 2
        xb = x[b].reshape(8, SL, D)
        order = list(range(8)) if p == 0 else [1, 0, 3, 2, 5, 4, 7, 6]
        d = dict(shared)
        d.update(cons[p])
        d["x"] = np.ascontiguousarray(xb[order].reshape(S, D))
        in_maps.append(d)
    return in_maps


def kernel(**inp):
    in_maps = make_in_maps(**inp)
    if "nc" not in _NC_CACHE:
        _NC_CACHE["nc"] = build_program()
    res = run_bass_kernel_spmd(_NC_CACHE["nc"], in_maps, core_ids=list(range(8)))
    out = np.empty((4, S, D), np.float32)
    for core in range(8):
        b, p = core // 2, core % 2
        y = np.asarray(res.results[core]["y"]).reshape(4, SL, D)
        for m in range(4):
            out[b, (2 * m + p) * SL:(2 * m + p + 1) * SL] = y[m]
    return out
```

```python
import math
from contextlib import ExitStack

import numpy as np
import concourse.bass as bass
import concourse.mybir as mybir
from concourse.bass_utils import run_bass_kernel_spmd

F32 = mybir.dt.float32
BF16 = mybir.dt.bfloat16
AF = mybir.ActivationFunctionType
ALU = mybir.AluOpType
AX = mybir.AxisListType

S = 4096
D = 1024
DFF = 2816
NFC = 22
SL = 512
BIG = 30000.0
EPS = 1e-6
NSTEP = 14
NDS = 20
DEBUG = False
import os
KSTOP = float(os.environ.get('KSTOP', '1000'))


class StopBuild(Exception):
    pass


def chk(n):
    if KSTOP <= n:
        raise StopBuild()


class Buf:
    __slots__ = ("w", "rs")

    def __init__(self):
        self.w = None
        self.rs = {}


class Tr:
    def __init__(self, nc, st):
        self.nc = nc
        self.E = {}
        for name, h in [("pe", nc.tensor), ("act", nc.scalar), ("dve", nc.vector),
                        ("pool", nc.gpsimd), ("sp", nc.sync)]:
            sem = st.enter_context(nc.semaphore("s_" + name))
            self.E[name] = dict(h=h, sem=sem, cnt=0, waited={})
        self.ds = {}
        for q, nq in (("pool", 4), ("sp", NDS)):
            sems = [st.enter_context(nc.semaphore("d%s%d" % (q, i))) for i in range(nq)]
            self.ds[q] = dict(sems=sems, val=[0] * nq, nxt=0, n=nq)

    def wait(self, eng, ev):
        if ev is None:
            return
        sem, val, key = ev
        e = self.E[eng]
        if key == "pe" and eng == "pe":
            return
        if key in self.E:
            assert self.E[key]["cnt"] >= val, "wait on unmarked event %s" % key
        if e["waited"].get(key, 0) >= val:
            return
        e["h"].wait_ge(sem, val)
        e["waited"][key] = val

    def _deps(self, eng, reads, writes):
        for b in reads:
            self.wait(eng, b.w)
        for b in writes:
            self.wait(eng, b.w)
            for k, r in b.rs.items():
                if k == eng:
                    continue
                self.wait(eng, r)

    def _record(self, ev, reads, writes):
        for b in writes:
            b.w = ev
            b.rs = {}
        for b in reads:
            b.rs[ev[2]] = ev

    def op(self, eng, fn, reads=(), writes=(), mark=True):
        self._deps(eng, reads, writes)
        e = self.E[eng]
        inst = fn(e["h"])
        if mark:
            inst.then_inc(e["sem"], 1)
            e["cnt"] += 1
            ev = (e["sem"], e["cnt"], eng)
        else:
            ev = (e["sem"], e["cnt"] + 1, eng)
        self._record(ev, reads, writes)
        return ev

    def dma(self, q, out, in_, reads=(), writes=()):
        self._deps(q, reads, writes)
        d = self.ds[q]
        i = d["nxt"]
        d["nxt"] = (i + 1) % d["n"]
        key = ("d", q, i)
        if d["val"][i] > 0:
            self.wait(q, (d["sems"][i], d["val"][i], key))
        self.E[q]["h"].dma_start(out=out, in_=in_).then_inc(d["sems"][i], 16)
        d["val"][i] += 16
        ev = (d["sems"][i], d["val"][i], key)
        self._record(ev, reads, writes)
        return ev

    def barrier(self):
        evs = []
        for k, e in self.E.items():
            if e["cnt"] > 0:
                evs.append((e["sem"], e["cnt"], k))
        for q, d in self.ds.items():
            for i in range(d["n"]):
                if d["val"][i] > 0:
                    evs.append((d["sems"][i], d["val"][i], ("d", q, i)))
        for k in self.E:
            for ev in evs:
                if ev[2] == k:
                    continue
                self.wait(k, ev)


def build_program(debug=False):
    nc = bass.Bass("TRN2", target_bir_lowering=False)

    def din(name, shape, dt=F32):
        return nc.dram_tensor(name, shape, dt, kind="ExternalInput").ap()

    x_d = din("x", [S, D])
    w1gu_d = din("w1gu", [D, 2 * DFF])
    w1d_d = din("w1d", [DFF, D])
    win_d = din("win", [D, 5444])
    wba_d = din("wba", [512, D])
    wbb_d = din("wbb", [512, D])
    wo_d = din("wo", [D, D])
    w2gu_d = din("w2gu", [D, 2 * DFF])
    w2d_d = din("w2d", [DFF, D])
    gains_d = din("gains", [128, 24])
    bg_d = din("bg", [128, 16])
    gqk_d = din("gqk", [128, 4])
    dtab_d = din("dtab", [128, 16, 384])
    cb_d = din("cb", [128, 16])
    cmask_d = din("cmask", [128, 384])
    mtab_d = din("mtab", [128, 896])
    pbv_d = din("pbv", [128, 1])
    idab_d = din("idab", [128, 3, 128])
    esel_d = din("esel", [128, 16, 128])
    gmask_d = din("gmask", [128, 2, 8, 4])
    ownhot_d = din("ownhot", [128, 2, 8, 4])
    c32_d = din("c32", [128, 3, 128])
    pow2_d = din("pow2", [128, NSTEP])
    y_d = nc.dram_tensor("y", [4 * SL, D], F32, kind="ExternalOutput").ap()
    Kc_d = nc.dram_tensor("Kc", [8, 128, S], BF16, kind="Internal").ap()
    Vc_d = nc.dram_tensor("Vc", [16, 128, 32, 66], BF16, kind="Internal").ap()
    kic_d = nc.dram_tensor("kic", [64, S], BF16, kind="Internal").ap()
    wb_shapes = {"w1gu": (D, 2 * DFF), "w1d": (DFF, D), "win": (D, 5444), "wba": (512, D), "wbb": (512, D),
                 "wo": (D, D), "w2gu": (D, 2 * DFF), "w2d": (DFF, D)}
    wsrc = {"w1gu": w1gu_d, "w1d": w1d_d, "win": win_d, "wba": wba_d, "wbb": wbb_d, "wo": wo_d, "w2gu": w2gu_d, "w2d": w2d_d}
    wbf = {k: nc.dram_tensor(k + "_bf", list(v), BF16, kind="Internal").ap() for k, v in wb_shapes.items()}

    with ExitStack() as st:
        T = Tr(nc, st)

        def sb(name, shape, dt):
            return st.enter_context(nc.sbuf_tensor("sb_" + name, shape, dt))

        def ps(name, shape, dt):
            return st.enter_context(nc.psum_tensor("ps_" + name, shape, dt))

        R1 = sb("R1", [128, 57344], mybir.dt.uint8)

        def r1view(off, shape, dt):
            n = int(np.prod(shape[1:]))
            esz = 2 if dt == BF16 else 4
            v = R1[:, off:off + n * esz].bitcast(dt)
            if len(shape) == 3:
                v = v.rearrange("p (a b) -> p a b", a=shape[1])
            return v
        actT = r1view(0, [128, NFC, SL], BF16)
        xtok = r1view(22528, [128, 4, D], F32)
        wdb = [r1view(38912, [128, NFC, 128], BF16), r1view(44544, [128, NFC, 128], BF16)]
        sqb = [r1view(50176, [128, SL], F32), r1view(52224, [128, SL], F32)]
        maskT = r1view(0, [128, 32, SL], BF16)
        Isc = r1view(32768, [128, S], F32)
        msk = r1view(49152, [128, S], BF16)
        B_R1 = Buf()

        xT = sb("xT", [128, 8, SL], F32)
        xnT = sb("xnT", [128, 8, SL], BF16)
        wgub = [sb("wgu%d" % i, [128, 8, 2, 128], BF16) for i in range(3)]
        winb = [sb("winb%d" % i, [128, 8, 128], BF16) for i in range(3)]
        wvb = sb("wvb", [128, 8, 256], BF16)
        wkib = sb("wkib", [128, 8, 64], BF16)
        wwib = sb("wwib", [128, 8, 4], BF16)
        scr = [sb("scr%d" % i, [128, SL], F32) for i in range(4)]
        rstd = sb("rstd", [128, SL], F32)
        rstd2 = sb("rstd2", [128, SL], F32)
        mhalf = sb("mhalf", [128, SL], F32)
        qaT = sb("qaT", [128, 4, SL], BF16)
        qbT = sb("qbT", [128, 4, SL], BF16)
        qiT = sb("qiT", [128, 2, SL], BF16)
        wS = sb("wS", [128, 4, 4], F32)
        kiT2 = sb("kiT2", [128, S], BF16)
        kbuf = sb("kbuf", [128, S], BF16)
        vbuf = sb("vbuf", [128, 32, 66], BF16)
        pbuf = [sb("pbuf%d" % i, [128, SL], BF16) for i in range(3)]
        ebuf = [sb("ebuf%d" % i, [128, SL], BF16) for i in range(2)]
        AT = sb("AT", [128, 8, SL], BF16)
        mixT = sb("mixT", [128, 8, SL], BF16)
        Dt = sb("Dt", [128, 16, 384], BF16)
        esel = sb("esel", [128, 16, 128], BF16)
        idab = sb("idab", [128, 3, 128], BF16)
        c32 = sb("c32", [128, 3, 128], F32)
        gains = sb("gains", [128, 24], F32)
        bg = sb("bg", [128, 16], F32)
        gqk = sb("gqk", [128, 4], F32)
        gq8 = sb("gq8", [128, 2], F32)
        cb = sb("cb", [128, 32], F32)
        mtab = sb("mtab", [128, 896], BF16)
        pbv = sb("pbv", [128, 1], F32)
        gmask = sb("gmask", [128, 2, 8, 4], F32)
        ownhot = sb("ownhot", [128, 2, 8, 4], F32)
        pow2 = sb("pow2", [128, NSTEP], F32)
        kst = [sb("kst%d" % i, [128, SL], BF16) for i in range(2)]
        vst = sb("vst", [128, 4, 8, 66], BF16)
        kist = sb("kist", [64, SL], BF16)
        kms = sb("kms", [128, 4, 16], F32)
        kmb = sb("kmb", [128, 4, 16], BF16)
        G = sb("G", [128, 8, 16], F32)
        sel = sb("sel", [128, 8, 16], F32)
        m8 = sb("m8", [128, 8, 8], F32)
        thr = sb("thr", [128, 8], F32)
        negm = sb("negm", [128, 8, 32], BF16)
        negmT = sb("negmT", [128, 8, SL], BF16)
        small = sb("small", [128, 8], F32)
        steps = sb("steps", [128, NSTEP], F32)
        rd = rstd
        dtmp = scr[0][:, 0:384]
        cmask = scr[1][:, 0:384]

        ident_b = idab[:, 0, :]
        identA = idab[:, 1, :]
        identB = idab[:, 2, :]
        ident32 = c32[:, 0, :]
        ones32 = c32[:, 1, :]
        bones32 = c32[:, 2, :]

        pbank = [ps("pb%d" % i, [128, SL], F32) for i in range(4)]
        B_pbank = [Buf() for _ in range(4)]
        pobank = [ps("po%d" % i, [128, SL], F32) for i in range(2)]
        B_po = [Buf(), Buf()]
        pmisc = ps("pmisc", [128, SL], F32)
        B_misc = Buf()
        psb = ps("psb", [128, 8, 128], BF16)
        B_psb = Buf()
        pctr = [0]

        def nextbank():
            i = pctr[0] % 4
            pctr[0] += 1
            return pbank[i], B_pbank[i]

        B_xtok = Buf()
        B_xT = [Buf() for _ in range(8)]
        B_xnT = [Buf() for _ in range(8)]
        B_actT = [Buf() for _ in range(NFC)]
        B_wgu = [[Buf(), Buf()] for _ in range(3)]
        B_wd = [Buf(), Buf()]
        B_win = [Buf() for _ in range(3)]
        B_wv, B_wki, B_wwi = Buf(), Buf(), Buf()
        B_sq = [Buf(), Buf()]
        B_scr = [Buf() for _ in range(4)]
        B_rstd, B_rstd2 = Buf(), Buf()
        B_const = Buf()
        B_qaT = [Buf() for _ in range(4)]
        B_qbT = [Buf() for _ in range(4)]
        B_qiT = [Buf(), Buf()]
        B_wS = Buf()
        B_kiT2, B_kbuf, B_vbuf = Buf(), Buf(), Buf()
        B_pbuf = [Buf() for _ in range(3)]
        B_ebuf = [Buf(), Buf()]
        B_AT = [Buf() for _ in range(8)]
        B_mixT = [Buf() for _ in range(8)]
        B_kst = [Buf(), Buf()]
        B_vst, B_kist = Buf(), Buf()
        B_km = Buf()
        B_G, B_sel, B_m8, B_thr, B_negm, B_negmT = Buf(), Buf(), Buf(), Buf(), Buf(), Buf()
        B_small, B_steps, B_rd = Buf(), Buf(), Buf()
        B_Isc, B_msk = Buf(), Buf()
        B_maskT = [Buf() for _ in range(4)]
        B_Kc = [[Buf() for _ in range(8)] for _ in range(8)]
        B_Vc = [[[Buf() for _ in range(4)] for _ in range(8)] for _ in range(2)]
        B_kic = [Buf() for _ in range(8)]
        B_y = Buf()
        sctr = [0]

        def nextscr():
            i = sctr[0] % 4
            sctr[0] += 1
            return scr[i], B_scr[i]

        stg = [r1view(i * 11264, [128, 5632], BF16) for i in range(4)]
        B_stg = [Buf() for _ in range(4)]
        B_wbf = Buf()
        si = 0
        for k in ("w1gu", "w1d", "win", "wba", "wbb", "wo", "w2gu", "w2d"):
            R_, C_ = wb_shapes[k]
            for rb in range(R_ // 128):
                i = si % 4
                si += 1
                T.dma("pool", stg[i][:, 0:C_], wsrc[k][rb * 128:(rb + 1) * 128, :], writes=[B_stg[i]])
                T.dma("sp", wbf[k][rb * 128:(rb + 1) * 128, :], stg[i][:, 0:C_], reads=[B_stg[i]], writes=[B_wbf])
        T.dma("pool", idab[:], idab_d, writes=[B_const])
        T.dma("pool", esel[:], esel_d, writes=[B_const])
        T.dma("pool", mtab[:], mtab_d, writes=[B_const])
        for dst, src in [(c32, c32_d), (gains, gains_d), (bg, bg_d), (gqk, gqk_d), (pbv, pbv_d),
                         (gmask, gmask_d), (ownhot, ownhot_d), (pow2, pow2_d)]:
            T.dma("sp", dst[:], src, writes=[B_const])
        T.dma("sp", cmask, cmask_d, writes=[B_scr[1]])
        T.dma("sp", cb[:, 0:16], cb_d, writes=[B_const])
        T.op("dve", lambda e: e.memset(mhalf[:], -0.5), writes=[B_const])
        T.op("dve", lambda e: e.memset(vst[:], 1.0), writes=[B_vst])
        T.op("dve", lambda e: e.memset(negm[:], 0.0), writes=[B_negm])
        T.op("dve", lambda e: e.memset(negmT[:], 0.0), writes=[B_negmT])
        T.op("dve", lambda e: e.tensor_scalar(out=gq8[:], in0=gqk[:, 0:2], scalar1=0.125, scalar2=None, op0=ALU.mult),
             reads=[B_const], writes=[B_const])
        T.op("dve", lambda e: e.tensor_scalar(out=cb[:, 16:32], in0=cb[:, 0:16], scalar1=pbv[:, 0:1], scalar2=None, op0=ALU.add),
             reads=[B_const], writes=[B_const])
        for h in range(16):
            T.dma("sp", dtmp, dtab_d[:, h, :], writes=[B_scr[0]])
            T.op("dve", lambda e, h=h: e.scalar_tensor_tensor(out=Dt[:, h, :], in0=dtmp, scalar=cb[:, h:h + 1], in1=cmask,
                                                             op0=ALU.subtract, op1=ALU.add),
                 reads=[B_scr[0], B_scr[1], B_const], writes=[B_const])

        def mmgroup(out_ap, obuf, items, extra_reads=()):
            n = len(items)
            for i, (l, r, rb) in enumerate(items):
                T.op("pe", lambda e, l=l, r=r, i=i: e.matmul(out_ap, lhsT=l, rhs=r, start=(i == 0), stop=(i == n - 1)),
                     reads=list(rb) + list(extra_reads), writes=[obuf], mark=(i == n - 1))

        win_v = wbf["win"].rearrange("(kc p) n -> p kc n", p=128)

        def rmsnorm(gcol0):
            for kc in range(8):
                sq, bsq = sqb[kc % 2], B_sq[kc % 2]
                T.op("act", lambda e, kc=kc, sq=sq: e.activation(out=sq, in_=xT[:, kc, :], func=AF.Square),
                     reads=[B_xT[kc]], writes=[bsq])
                T.op("pe", lambda e, kc=kc, sq=sq: e.matmul(pmisc[:], lhsT=ones32, rhs=sq, start=(kc == 0), stop=(kc == 7)),
                     reads=[bsq, B_const], writes=[B_misc], mark=True)
            T.op("dve", lambda e: e.tensor_scalar(out=rstd[:], in0=pmisc[:], scalar1=1.0 / D, scalar2=EPS, op0=ALU.mult, op1=ALU.add),
                 reads=[B_misc], writes=[B_rstd])
            T.op("pool", lambda e: e.tensor_tensor(out=rstd2[:], in0=rstd[:], in1=mhalf[:], op=ALU.pow),
                 reads=[B_rstd, B_const], writes=[B_rstd2])
            for kc in range(8):
                T.op("dve", lambda e, kc=kc: e.scalar_tensor_tensor(out=xnT[:, kc, :], in0=xT[:, kc, :],
                                                                   scalar=gains[:, gcol0 + kc:gcol0 + kc + 1], in1=rstd2[:],
                                                                   op0=ALU.mult, op1=ALU.mult),
                     reads=[B_xT[kc], B_rstd2, B_const], writes=[B_xnT[kc]])

        def ffn(wgu_d, wd_d):
            wgu_v = wgu_d.rearrange("(kc p) (two f) -> p kc two f", p=128, two=2)
            wd_v = wd_d.rearrange("(c p) n -> p c n", p=128)

            def load_gu(c):
                T.dma("sp", wgub[c % 3][:, :, 0, :], wgu_v[:, :, 0, c * 128:(c + 1) * 128], writes=[B_wgu[c % 3][0]])
                T.dma("sp", wgub[c % 3][:, :, 1, :], wgu_v[:, :, 1, c * 128:(c + 1) * 128], writes=[B_wgu[c % 3][1]])

            def load_d(cc):
                T.dma("sp", wdb[cc % 2], wd_v[:, :, cc * 128:(cc + 1) * 128], writes=[B_wd[cc % 2]])
            load_gu(0)
            load_gu(1)
            for c in range(NFC):
                if c + 2 < NFC:
                    load_gu(c + 2)
                w = wgub[c % 3]
                gb_, bgb = nextbank()
                ub_, bub = nextbank()
                mmgroup(gb_[:], bgb, [(w[:, kc, 0, :], xnT[:, kc, :], [B_wgu[c % 3][0], B_xnT[kc]]) for kc in range(8)])
                mmgroup(ub_[:], bub, [(w[:, kc, 1, :], xnT[:, kc, :], [B_wgu[c % 3][1], B_xnT[kc]]) for kc in range(8)])
                sg, bsg = nextscr()
                T.op("act", lambda e, sg=sg, gb_=gb_: e.activation(out=sg[:], in_=gb_[:], func=AF.Silu), reads=[bgb], writes=[bsg])
                T.op("dve", lambda e, sg=sg, ub_=ub_, c=c: e.tensor_tensor(out=actT[:, c, :], in0=ub_[:], in1=sg[:], op=ALU.mult),
                     reads=[bub, bsg], writes=[B_actT[c]])
                if c == NFC - 3:
                    load_d(0)
                if c == NFC - 2:
                    load_d(1)
            for cc in range(8):
                w = wdb[cc % 2]
                bk, bbk = nextbank()
                mmgroup(bk[:], bbk, [(w[:, c, :], actT[:, c, :], [B_wd[cc % 2], B_actT[c]]) for c in range(NFC)])
                T.op("dve", lambda e, bk=bk, cc=cc: e.scalar_tensor_tensor(out=xT[:, cc, :], in0=bk[:], scalar=0.5, in1=xT[:, cc, :],
                                                                          op0=ALU.mult, op1=ALU.add),
                     reads=[bbk, B_xT[cc]], writes=[B_xT[cc]])
                if cc + 2 < 8:
                    load_d(cc + 2)

        wctr = [0]

        def load_win(col0):
            i = wctr[0] % 3
            wctr[0] += 1
            T.dma("sp", winb[i][:], win_v[:, :, col0:col0 + 128], writes=[B_win[i]])
            return winb[i], B_win[i]

        def proj_pair_normed(col0, gain_ap, out_ap, obufs):
            w, bw = load_win(col0)
            bk, bbk = nextbank()
            mmgroup(bk[:], bbk, [(w[:, kc, :], xnT[:, kc, :], [bw, B_xnT[kc]]) for kc in range(8)])
            raw, braw = nextscr()
            sq, bsq = nextscr()
            T.op("act", lambda e: e.activation(out=raw[:], in_=bk[:], func=AF.Copy), reads=[bbk], writes=[braw])
            T.op("act", lambda e: e.activation(out=sq[:], in_=bk[:], func=AF.Square), reads=[bbk], writes=[bsq])
            T.op("pe", lambda e: e.matmul(pmisc[:], lhsT=bones32, rhs=sq[:], start=True, stop=True),
                 reads=[bsq, B_const], writes=[B_misc])
            T.op("dve", lambda e: e.tensor_scalar(out=rstd[:], in0=pmisc[:], scalar1=1.0 / 64, scalar2=EPS, op0=ALU.mult, op1=ALU.add),
                 reads=[B_misc], writes=[B_rstd])
            T.op("pool", lambda e: e.tensor_tensor(out=rstd2[:], in0=rstd[:], in1=mhalf[:], op=ALU.pow),
                 reads=[B_rstd, B_const], writes=[B_rstd2])
            T.op("dve", lambda e: e.scalar_tensor_tensor(out=out_ap, in0=raw[:], scalar=gain_ap, in1=rstd2[:], op0=ALU.mult, op1=ALU.mult),
                 reads=[braw, B_rstd2, B_const], writes=obufs)

        def phase1(slot, own, m):
            T.dma("sp", xtok, x_d[slot * SL:(slot + 1) * SL, :].rearrange("(tt p) f -> p tt f", p=128), writes=[B_xtok])
            for kc in range(8):
                bk, bbk = nextbank()
                for tt in range(4):
                    T.op("pe", lambda e, bk=bk, tt=tt, kc=kc: e.transpose(out=bk[:, tt * 128:(tt + 1) * 128],
                                                                         in_=xtok[:, tt, kc * 128:(kc + 1) * 128], identity=ident32),
                         reads=[B_xtok, B_const], writes=[bbk], mark=(tt == 3))
                if kc % 2 == 0:
                    T.op("act", lambda e, bk=bk, kc=kc: e.activation(out=xT[:, kc, :], in_=bk[:], func=AF.Copy), reads=[bbk], writes=[B_xT[kc]])
                else:
                    T.op("dve", lambda e, bk=bk, kc=kc: e.tensor_copy(out=xT[:, kc, :], in_=bk[:]), reads=[bbk], writes=[B_xT[kc]])
            chk(1)
            rmsnorm(0)
            chk(2)
            ffn(wbf["w1gu"], wbf["w1d"])
            chk(3)
            rmsnorm(8)
            for grp, col_base, gcol in ((0, 512, 2), (1, 2048, 3)):
                for pr in range(4):
                    ks, bks = kst[pr % 2], B_kst[pr % 2]
                    proj_pair_normed(col_base + pr * 128, gqk[:, gcol:gcol + 1], ks[:], [bks])
                    if grp == 1:
                        T.op("dve", lambda e, ks=ks, pr=pr: e.tensor_reduce(out=kms[:, pr, 2 * slot:2 * slot + 2],
                                                                           in_=ks[:].rearrange("p (a b) -> p a b", a=2),
                                                                           axis=AX.X, op=ALU.add),
                             reads=[bks], writes=[B_km])
                        T.op("dve", lambda e, pr=pr: e.tensor_scalar(out=kmb[:, pr, 2 * slot:2 * slot + 2], in0=kms[:, pr, 2 * slot:2 * slot + 2],
                                                                    scalar1=1.0 / 256, scalar2=None, op0=ALU.mult),
                             reads=[B_km], writes=[B_km])
                    T.dma("sp", Kc_d[grp * 4 + pr, :, slot * SL:(slot + 1) * SL], ks[:], reads=[bks], writes=[B_Kc[grp * 4 + pr][slot]])
            for grp, col_base in ((0, 1024), (1, 2560)):
                for half in range(2):
                    T.dma("sp", wvb[:], win_v[:, :, col_base + half * 256:col_base + (half + 1) * 256], writes=[B_wv])
                    for tt in range(4):
                        bk, bbk = nextbank()
                        mmgroup(bk[:, 0:256], bbk, [(xnT[:, kc, tt * 128:(tt + 1) * 128], wvb[:, kc, :], [B_wv, B_xnT[kc]]) for kc in range(8)])
                        T.op("act", lambda e, bk=bk, tt=tt, half=half: e.activation(
                            out=vst[:, tt, half * 4:(half + 1) * 4, 0:64],
                            in_=bk[:, 0:256].rearrange("p (h d) -> p h d", h=4), func=AF.Copy),
                            reads=[bbk], writes=[B_vst])
                for tt in range(4):
                    T.dma("sp", Vc_d[grp * 8:(grp + 1) * 8].rearrange("h p t d -> p t h d")[:, slot * 4 + tt, :, :], vst[:, tt, :, :],
                          reads=[B_vst], writes=[B_Vc[grp][slot][tt]])
            T.dma("sp", wkib[:], win_v[:, :, 3328:3392], writes=[B_wki])
            bk, bbk = nextbank()
            mmgroup(bk[0:64, :], bbk, [(wkib[:, kc, :], xnT[:, kc, :], [B_wki, B_xnT[kc]]) for kc in range(8)])
            T.op("act", lambda e: e.activation(out=kist[:], in_=bk[0:64, :], func=AF.Copy), reads=[bbk], writes=[B_kist])
            T.dma("sp", kic_d[:, slot * SL:(slot + 1) * SL], kist[:], reads=[B_kist], writes=[B_kic[slot]])
            if not own:
                return
            for pr in range(4):
                proj_pair_normed(pr * 128, gq8[:, 0:1], qaT[:, pr, :], [B_qaT[pr]])
            for pr in range(4):
                proj_pair_normed(1536 + pr * 128, gq8[:, 1:2], qbT[:, pr, :], [B_qbT[pr]])
            for pr in range(2):
                w, bw = load_win(3072 + pr * 128)
                bk, bbk = nextbank()
                mmgroup(bk[:], bbk, [(w[:, kc, :], xnT[:, kc, :], [bw, B_xnT[kc]]) for kc in range(8)])
                T.op("act", lambda e, bk=bk, pr=pr: e.activation(out=qiT[:, pr, :], in_=bk[:], func=AF.Copy), reads=[bbk], writes=[B_qiT[pr]])
            T.dma("sp", wwib[:], win_v[:, :, 3392:3396], writes=[B_wwi])
            for tt in range(4):
                bk, bbk = nextbank()
                mmgroup(bk[:, 0:4], bbk, [(xnT[:, kc, tt * 128:(tt + 1) * 128], wwib[:, kc, :], [B_wwi, B_xnT[kc]]) for kc in range(8)])
                T.op("dve", lambda e, bk=bk, tt=tt: e.tensor_scalar(out=wS[:, tt, :], in0=bk[:, 0:4], scalar1=0.0625, scalar2=None, op0=ALU.mult),
                     reads=[bbk], writes=[B_wS])

        def attention(m):
            nsl = 2 * m + 2
            nk = nsl * SL
            nkt = nsl * 4
            nb = nsl * 2
            k0 = 1024 * m
            for hf in range(2):
                T.dma("sp", kiT2[hf * 64:(hf + 1) * 64, 0:nk], kic_d[:, 0:nk], reads=B_kic[0:nsl], writes=[B_kiT2])
            for r in range(4):
                for j in range(nsl):
                    banks = [nextbank() for _ in range(4)]
                    for h in range(4):
                        p0 = 64 * (h % 2)
                        bk, bbk = banks[h]
                        mmgroup(bk[:], bbk, [(qiT[p0:p0 + 64, h // 2, r * 128:(r + 1) * 128], kiT2[p0:p0 + 64, j * SL:(j + 1) * SL],
                                              [B_qiT[h // 2], B_kiT2])])
                    for h in range(4):
                        bk, bbk = banks[h]
                        rl, brl = nextscr()
                        T.op("act", lambda e, bk=bk, rl=rl: e.activation(out=rl[:], in_=bk[:], func=AF.Relu), reads=[bbk], writes=[brl])
                        dst = Isc[:, j * SL:(j + 1) * SL]
                        if h == 0:
                            T.op("dve", lambda e, rl=rl, dst=dst: e.tensor_scalar(out=dst, in0=rl[:], scalar1=wS[:, r, 0:1], scalar2=None, op0=ALU.mult),
                                 reads=[brl, B_wS], writes=[B_Isc])
                        else:
                            T.op("dve", lambda e, rl=rl, dst=dst, h=h: e.scalar_tensor_tensor(out=dst, in0=rl[:], scalar=wS[:, r, h:h + 1], in1=dst,
                                                                                            op0=ALU.mult, op1=ALU.add),
                                 reads=[brl, B_wS, B_Isc], writes=[B_Isc])
                chk(6 if m == 0 else (12.31 if r == 0 else 12.39))
                Bv, lo, mid, cnt, tt_ = (small[:, i:i + 1] for i in range(5))
                T.op("dve", lambda e: e.tensor_reduce(out=Bv, in_=Isc[:, 0:nk], axis=AX.X, op=ALU.max, apply_absolute_value=True),
                     reads=[B_Isc], writes=[B_small])
                if m == 1 and r == 0:
                    chk(12.311)
                T.op("dve", lambda e: e.tensor_scalar(out=lo, in0=Bv, scalar1=1.0, scalar2=-1.0, op0=ALU.add, op1=ALU.mult),
                     reads=[B_small], writes=[B_small])
                T.op("dve", lambda e: e.tensor_scalar(out=steps[:], in0=pow2[:], scalar1=lo, scalar2=-2.0, op0=ALU.mult, op1=ALU.mult),
                     reads=[B_small, B_const], writes=[B_steps])
                T.op("dve", lambda e: e.tensor_tensor(out=Isc[:, k0:k0 + SL], in0=Isc[:, k0:k0 + SL],
                                                      in1=mtab[:, 384 - 128 * r:896 - 128 * r], op=ALU.add),
                     reads=[B_Isc, B_const], writes=[B_Isc])
                T.op("dve", lambda e: e.tensor_scalar(out=Isc[:, k0 + SL:k0 + 2 * SL], in0=Isc[:, k0 + SL:k0 + 2 * SL], scalar1=pbv[:, 0:1],
                                                      scalar2=None, op0=ALU.add),
                     reads=[B_Isc, B_const], writes=[B_Isc])
                if m == 1 and r == 0:
                    chk(12.312)
                for s_ in range(NSTEP):
                    if m == 1 and r == 0 and s_ == 1:
                        chk(12.313)
                    T.op("dve", lambda e, s_=s_: e.tensor_tensor(out=mid, in0=lo, in1=steps[:, s_:s_ + 1], op=ALU.add),
                         reads=[B_small, B_steps], writes=[B_small])
                    T.op("dve", lambda e: e.tensor_scalar(out=msk[:, 0:nk], in0=Isc[:, 0:nk], scalar1=mid, scalar2=0.0, op0=ALU.is_ge, op1=ALU.add,
                                                          accum_out=cnt),
                         reads=[B_Isc, B_small], writes=[B_msk, B_small])
                    T.op("dve", lambda e, s_=s_: e.tensor_scalar(out=tt_, in0=cnt, scalar1=256.0, scalar2=steps[:, s_:s_ + 1], op0=ALU.is_ge, op1=ALU.mult),
                         reads=[B_small, B_steps], writes=[B_small])
                    T.op("dve", lambda e: e.tensor_tensor(out=lo, in0=lo, in1=tt_, op=ALU.add), reads=[B_small], writes=[B_small])
                T.op("dve", lambda e: e.tensor_scalar(out=msk[:, 0:nk], in0=Isc[:, 0:nk], scalar1=lo, scalar2=None, op0=ALU.is_ge),
                     reads=[B_Isc, B_small], writes=[B_msk])
                if m == 1 and r == 0:
                    chk(12.314)
                for g0 in range(0, nkt, 8):
                    if m == 1 and r == 0 and g0 == 8:
                        chk(12.315)
                    for i in range(8):
                        kt = g0 + i
                        T.op("pe", lambda e, i=i, kt=kt: e.transpose(out=psb[:, i, :], in_=msk[:, kt * 128:(kt + 1) * 128], identity=ident_b),
                             reads=[B_msk, B_const], writes=[B_psb], mark=(i == 7))
                    T.op("act", lambda e, g0=g0: e.activation(out=maskT[:, g0:g0 + 8, r * 128:(r + 1) * 128], in_=psb[:], func=AF.Copy),
                         reads=[B_psb], writes=[B_maskT[r]])
                chk(7 if m == 0 else (12.32 if r == 0 else 12.39))
                half = r // 2
                T.op("dve", lambda e: e.memset(G[:], -BIG), writes=[B_G])
                gbo, bgbo = nextbank()
                for par, (gbk, bgbk) in enumerate(((pmisc, B_misc), (gbo, bgbo))):
                    p0 = 64 * par
                    for pr_ in range(4):
                        h = 2 * pr_ + par
                        T.op("pe", lambda e, h=h, p0=p0, pr_=pr_, gbk=gbk: e.matmul(gbk[:, h * 16:h * 16 + nb], lhsT=qbT[p0:p0 + 64, pr_, r * 128:(r + 1) * 128],
                                                                                  rhs=kmb[p0:p0 + 64, pr_, 0:nb], start=True, stop=True),
                             reads=[B_qbT[pr_], B_km], writes=[bgbk], mark=(pr_ == 3))
                    T.op("dve", lambda e, par=par, gbk=gbk: e.tensor_copy(
                        out=G[:].rearrange("p (hp two) b -> p hp two b", two=2)[:, :, par, 0:nb],
                        in_=gbk[:, 0:128].rearrange("p (hp two b) -> p hp two b", two=2, b=16)[:, :, par, 0:nb]),
                        reads=[bgbk], writes=[B_G])
                T.op("dve", lambda e: e.tensor_tensor(out=G[:, :, 4 * m:4 * m + 4], in0=G[:, :, 4 * m:4 * m + 4], in1=gmask[:, half, :, :], op=ALU.add),
                     reads=[B_G, B_const], writes=[B_G])
                for h in range(8):
                    T.op("dve", lambda e, h=h: e.max(out=m8[:, h, :], in_=G[:, h, :]), reads=[B_G], writes=[B_m8])
                T.op("dve", lambda e: e.tensor_scalar(out=thr[:], in0=m8[:, :, 2], scalar1=-BIG / 2, scalar2=None, op0=ALU.max),
                     reads=[B_m8], writes=[B_thr])
                for h in range(8):
                    T.op("dve", lambda e, h=h: e.tensor_scalar(out=sel[:, h, :], in0=G[:, h, :], scalar1=thr[:, h:h + 1], scalar2=None, op0=ALU.is_ge),
                         reads=[B_G, B_thr], writes=[B_sel])
                T.op("dve", lambda e: e.tensor_tensor(out=sel[:, :, 4 * m:4 * m + 4], in0=sel[:, :, 4 * m:4 * m + 4], in1=ownhot[:, half, :, :], op=ALU.add),
                     reads=[B_sel, B_const], writes=[B_sel])
                T.op("dve", lambda e: e.tensor_scalar(out=negm[:, :, 0:16], in0=sel[:], scalar1=-1.0, scalar2=BIG, op0=ALU.add, op1=ALU.mult),
                     reads=[B_sel], writes=[B_negm])
                for h in range(8):
                    T.op("pe", lambda e, h=h: e.transpose(out=psb[0:32, h, :], in_=negm[:, h, :], identity=ident_b),
                         reads=[B_negm, B_const], writes=[B_psb], mark=(h == 7))
                T.op("act", lambda e: e.activation(out=negmT[0:32, :, r * 128:(r + 1) * 128], in_=psb[0:32, :, :], func=AF.Copy),
                     reads=[B_psb], writes=[B_negmT])
                if m == 1 and r == 0:
                    chk(12.33)

            chk(8 if m == 0 else 12.4)
            pbi = [0]
            for hg in range(16):
                moba = hg >= 8
                h = hg % 8
                pr = h // 2
                p0 = 64 * (h % 2)
                grp = 1 if moba else 0
                if h % 2 == 0:
                    T.dma("sp", kbuf[:, 0:nk], Kc_d[grp * 4 + pr, :, 0:nk], reads=B_Kc[grp * 4 + pr][0:nsl], writes=[B_kbuf])
                T.dma("sp", vbuf[:, 0:nkt, :], Vc_d[hg, :, 0:nkt, :], reads=[b for sl_ in B_Vc[grp][0:nsl] for b in sl_], writes=[B_vbuf])
                qT = qbT if moba else qaT
                bq = (B_qbT if moba else B_qaT)[pr]
                po, bpo = pobank[hg % 2], B_po[hg % 2]
                g, jj = h // 3, h % 3
                for kt in range(nkt):
                    j = kt - 8 * m
                    q0 = 128 * j if 0 <= j < 4 else 0
                    sbk, bsbk = nextbank()
                    items = [(sbk[:, q0:SL], kbuf[p0:p0 + 64, kt * 128:(kt + 1) * 128], qT[p0:p0 + 64, pr, q0:SL], [B_kbuf, bq])]
                    if 0 <= j < 4:
                        W = min(384, SL - q0)
                        items.append((sbk[:, q0:q0 + W], ident_b, Dt[:, hg, 0:W], [B_const]))
                    if m >= 1 and kt == 8 * m - 1:
                        items.append((sbk[:, 0:256], identA, Dt[:, hg, 128:384], [B_const]))
                    if kt == 8 * m + 7:
                        items.append((sbk[:, 0:256], identB, Dt[:, hg, 128:384], [B_const]))
                    if moba:
                        items.append((sbk[:, q0:SL], esel[:, kt // 2, :], negmT[:, h, q0:SL], [B_const, B_negmT]))
                    n = len(items)
                    for i, (o, l, rr, rb) in enumerate(items):
                        T.op("pe", lambda e, o=o, l=l, rr=rr, i=i, n=n: e.matmul(o, lhsT=l, rhs=rr, start=(i == 0), stop=(i == n - 1)),
                             reads=rb, writes=[bsbk], mark=(i == n - 1))
                    if hg == 0 and kt == 0:
                        chk(8.31)
                    pb_, bpb = pbuf[pbi[0] % 3], B_pbuf[pbi[0] % 3]
                    pbi[0] += 1
                    col = hg + (16 if kt >= 8 * m + 4 else 0)
                    if moba:
                        eb_, beb = pb_, bpb
                    else:
                        eb_, beb = ebuf[pbi[0] % 2], B_ebuf[pbi[0] % 2]
                    T.op("act", lambda e, eb_=eb_, sbk=sbk, q0=q0, col=col: e.activation(out=eb_[:, q0:SL], in_=sbk[:, q0:SL], func=AF.Exp,
                                                                                        bias=cb[:, col:col + 1], scale=1.0),
                         reads=[bsbk, B_const], writes=[beb])
                    if hg == 0 and kt == 0:
                        chk(8.32)
                    if not moba:
                        T.op("dve", lambda e, pb_=pb_, eb_=eb_, q0=q0, kt=kt: e.scalar_tensor_tensor(out=pb_[:, q0:SL], in0=eb_[:, q0:SL], scalar=1.0,
                                                                                                    in1=maskT[:, kt, q0:SL], op0=ALU.mult, op1=ALU.mult),
                             reads=[beb] + B_maskT, writes=[bpb])
                    if hg == 0 and kt == 0:
                        chk(8.33)
                    T.op("pe", lambda e, po=po, pb_=pb_, q0=q0, kt=kt: e.matmul(po[0:65, q0:SL], lhsT=vbuf[:, kt, 0:65], rhs=pb_[:, q0:SL],
                                                                               start=(kt == 0), stop=(kt == nkt - 1)),
                         reads=[B_vbuf, bpb], writes=[bpo], mark=True)
                    if hg == 0:
                        chk(8.34 + 0.005 * kt)
                chk(8.4 if hg == 0 else (8.7 if hg == 8 else 8.99))
                T.op("dve", lambda e, po=po: e.reciprocal(out=rd[64:65, :], in_=po[64:65, :]), reads=[bpo], writes=[B_rd])
                if hg == 0:
                    chk(8.41)
                T.op("pe", lambda e: e.matmul(pmisc[:], lhsT=ones32[64:65, :], rhs=rd[64:65, :], start=True, stop=True),
                     reads=[B_rd, B_const], writes=[B_misc])
                if hg == 0:
                    chk(8.42)
                tmpo, btmpo = nextscr()
                T.op("act", lambda e, po=po, tmpo=tmpo, p0=p0: e.activation(out=tmpo[p0:p0 + 64, :], in_=po[0:64, :], func=AF.Copy),
                     reads=[bpo], writes=[btmpo])
                ap_ = pr + (4 if moba else 0)
                if hg == 0:
                    chk(8.43)
                T.op("dve", lambda e, tmpo=tmpo, p0=p0, ap_=ap_: e.tensor_tensor(out=AT[p0:p0 + 64, ap_, :], in0=pmisc[p0:p0 + 64, :],
                                                                                 in1=tmpo[p0:p0 + 64, :], op=ALU.mult),
                     reads=[btmpo, B_misc], writes=[B_AT[ap_]])
                chk(8.5 if hg == 0 else (8.6 if hg == 7 else (8.8 if hg == 8 else 8.99)))

        def mix(m):
            wba_v = wbf["wba"].rearrange("(kc p) n -> p kc n", p=128)
            wbb_v = wbf["wbb"].rearrange("(kc p) n -> p kc n", p=128)
            wo_v = wbf["wo"].rearrange("(kc p) n -> p kc n", p=128)
            for cc in range(8):
                wa, bwa = winb[wctr[0] % 3], B_win[wctr[0] % 3]
                wctr[0] += 1
                T.dma("sp", wa[:, 0:4, :], wba_v[:, :, cc * 128:(cc + 1) * 128], writes=[bwa])
                T.dma("sp", wa[:, 4:8, :], wbb_v[:, :, cc * 128:(cc + 1) * 128], writes=[bwa])
                wga, bwga = load_win(3396 + cc * 128)
                wgb, bwgb = load_win(3396 + 1024 + cc * 128)
                bA, bbA = nextbank()
                bB, bbB = nextbank()
                bGa, bbGa = nextbank()
                bGb, bbGb = nextbank()
                mmgroup(bA[:], bbA, [(wa[:, kc, :], AT[:, kc, :], [bwa, B_AT[kc]]) for kc in range(4)])
                mmgroup(bB[:], bbB, [(wa[:, 4 + kc, :], AT[:, 4 + kc, :], [bwa, B_AT[4 + kc]]) for kc in range(4)])
                mmgroup(bGa[:], bbGa, [(wga[:, kc, :], xnT[:, kc, :], [bwga, B_xnT[kc]]) for kc in range(8)])
                mmgroup(bGb[:], bbGb, [(wgb[:, kc, :], xnT[:, kc, :], [bwgb, B_xnT[kc]]) for kc in range(8)])
                ga, bga = nextscr()
                gb2, bgb2 = nextscr()
                T.op("act", lambda e, ga=ga, bGa=bGa, cc=cc: e.activation(out=ga[:], in_=bGa[:], func=AF.Sigmoid, bias=bg[:, cc:cc + 1], scale=1.0),
                     reads=[bbGa, B_const], writes=[bga])
                T.op("act", lambda e, gb2=gb2, bGb=bGb, cc=cc: e.activation(out=gb2[:], in_=bGb[:], func=AF.Sigmoid, bias=bg[:, 8 + cc:9 + cc], scale=1.0),
                     reads=[bbGb, B_const], writes=[bgb2])
                T.op("dve", lambda e, ga=ga, bA=bA: e.tensor_tensor(out=ga[:], in0=bA[:], in1=ga[:], op=ALU.mult), reads=[bbA, bga], writes=[bga])
                T.op("dve", lambda e, gb2=gb2, bB=bB: e.tensor_tensor(out=gb2[:], in0=bB[:], in1=gb2[:], op=ALU.mult), reads=[bbB, bgb2], writes=[bgb2])
                T.op("dve", lambda e, ga=ga, gb2=gb2, cc=cc: e.tensor_tensor(out=mixT[:, cc, :], in0=ga[:], in1=gb2[:], op=ALU.add),
                     reads=[bga, bgb2], writes=[B_mixT[cc]])
            for cc in range(8):
                w, bw = winb[wctr[0] % 3], B_win[wctr[0] % 3]
                wctr[0] += 1
                T.dma("sp", w[:], wo_v[:, :, cc * 128:(cc + 1) * 128], writes=[bw])
                bk, bbk = nextbank()
                mmgroup(bk[:], bbk, [(w[:, kc, :], mixT[:, kc, :], [bw, B_mixT[kc]]) for kc in range(8)])
                T.op("dve", lambda e, bk=bk, cc=cc: e.tensor_tensor(out=xT[:, cc, :], in0=bk[:], in1=xT[:, cc, :], op=ALU.add),
                     reads=[bbk, B_xT[cc]], writes=[B_xT[cc]])

        def output(m):
            for tt in range(4):
                for half in range(2):
                    bk, bbk = nextbank()
                    for i in range(4):
                        kc = half * 4 + i
                        T.op("pe", lambda e, bk=bk, i=i, kc=kc, tt=tt: e.transpose(out=bk[:, i * 128:(i + 1) * 128],
                                                                                  in_=xT[:, kc, tt * 128:(tt + 1) * 128], identity=ident32),
                             reads=[B_xT[kc], B_const], writes=[bbk], mark=(i == 3))
                    if half == 0:
                        T.op("act", lambda e, bk=bk, tt=tt: e.activation(out=xtok[:, tt, 0:512], in_=bk[:], func=AF.Copy), reads=[bbk], writes=[B_xtok])
                    else:
                        T.op("dve", lambda e, bk=bk, tt=tt: e.tensor_copy(out=xtok[:, tt, 512:1024], in_=bk[:]), reads=[bbk], writes=[B_xtok])
            T.dma("sp", y_d[m * SL:(m + 1) * SL, :].rearrange("(tt p) f -> p tt f", p=128), xtok, reads=[B_xtok], writes=[B_y])

        T.barrier()
        try:
            chk(0)
            for m in range(4):
                phase1(2 * m + 1, False, m)
                chk(4 if m == 0 else 12.2)
                phase1(2 * m, True, m)
                chk(5 if m == 0 else 12.3)
                T.barrier()
                attention(m)
                chk(9 if m == 0 else 12.5)
                T.barrier()
                mix(m)
                chk(10 if m == 0 else 12.6)
                rmsnorm(16)
                ffn(wbf["w2gu"], wbf["w2d"])
                chk(11)
                output(m)
                chk(12 + m)
        except StopBuild:
            pass
        T.barrier()
    return nc


def _t5_bucket(d):
    d = np.maximum(d, 0)
    df = np.maximum(d, 1).astype(np.float64)
    large = 16 + (np.log(df / 16.0) / math.log(128 / 16) * 16).astype(np.int64)
    large = np.minimum(large, 31)
    return np.where(d < 16, d, large)


def _consts(p, rel_bias):
    c = {}
    s = np.arange(128)[:, None]
    cc = np.arange(384)[None, :]
    bucket = _t5_bucket(cc - s)
    c["dtab"] = np.ascontiguousarray(np.transpose(rel_bias[bucket], (0, 2, 1))).astype(np.float32)
    c["cb"] = np.ascontiguousarray(np.broadcast_to(rel_bias[31][None, :], (128, 16))).astype(np.float32)
    c["cmask"] = np.where(cc >= s, 0.0, -BIG).astype(np.float32)
    c2 = np.arange(896)[None, :]
    c["mtab"] = np.where(c2 <= s + 384, 0.0, -BIG).astype(np.float32)
    pb = 0.0 if p == 1 else -BIG
    c["pbv"] = np.full((128, 1), pb, np.float32)
    eye = np.eye(128, dtype=np.float32)
    c["idab"] = np.ascontiguousarray(np.stack([eye, eye * (1.0 if p == 0 else 0.0), eye * (1.0 if p == 1 else 0.0)], axis=1))
    pp = np.arange(128)
    es = np.zeros((128, 16, 128), np.float32)
    for b in range(16):
        es[pp == b, b, :] = 1.0
    c["esel"] = es
    gm = np.zeros((2, 4), np.float32)
    oh = np.zeros((2, 4), np.float32)
    gm[0] = [-BIG, -BIG, pb, pb]
    oh[0] = [1, 0, 0, 0]
    gm[1] = [0.0, -BIG, pb, pb]
    oh[1] = [0, 1, 0, 0]
    c["gmask"] = np.ascontiguousarray(np.broadcast_to(gm[None, :, None, :], (128, 2, 8, 4))).astype(np.float32)
    c["ownhot"] = np.ascontiguousarray(np.broadcast_to(oh[None, :, None, :], (128, 2, 8, 4))).astype(np.float32)
    bo = np.zeros((128, 128), np.float32)
    bo[0:64, 0:64] = 1.0
    bo[64:128, 64:128] = 1.0
    c["c32"] = np.ascontiguousarray(np.stack([eye, np.ones((128, 128), np.float32), bo], axis=1))
    c["pow2"] = np.ascontiguousarray(np.broadcast_to((0.5 ** (np.arange(NSTEP) + 1))[None, :], (128, NSTEP))).astype(np.float32)
    return c


_NC_CACHE = {}


def make_in_maps(**inp):
    f = lambda k: np.asarray(inp[k], dtype=np.float32)
    x = f("x")
    fm = lambda v: np.ascontiguousarray(v.reshape(8, 128).T)
    gains = np.concatenate([fm(f("ffn1_norm")[0]), fm(f("mix_norm")[0]), fm(f("ffn2_norm")[0])], axis=1).astype(np.float32)
    bgv = np.ascontiguousarray(f("b_gate")[0].reshape(16, 128).T)
    gqk = np.stack([np.tile(f("q_norm_dsa")[0], 2), np.tile(f("q_norm_moba")[0], 2),
                    np.tile(f("k_norm_dsa")[0], 2), np.tile(f("k_norm_moba")[0], 2)], axis=1).astype(np.float32)
    rel_bias = f("rel_bias")
    shared = {
        "w1gu": np.ascontiguousarray(f("ffn1_w_gu")[0]), "w1d": np.ascontiguousarray(f("ffn1_w_down")[0]),
        "win": np.ascontiguousarray(f("w_in")[0]), "wba": np.ascontiguousarray(f("w_branch_dsa")[0]),
        "wbb": np.ascontiguousarray(f("w_branch_moba")[0]), "wo": np.ascontiguousarray(f("w_out")[0]),
        "w2gu": np.ascontiguousarray(f("ffn2_w_gu")[0]), "w2d": np.ascontiguousarray(f("ffn2_w_down")[0]),
        "gains": np.ascontiguousarray(gains), "bg": bgv, "gqk": np.ascontiguousarray(gqk),
    }
    cons = [_consts(0, rel_bias), _consts(1, rel_bias)]
    in_maps = []
    for core in range(8):
        b, p = core // 2, core % 2
        xb = x[b].reshape(8, SL, D)
        order = list(range(8)) if p == 0 else [1, 0, 3, 2, 5, 4, 7, 6]
        d = dict(shared)
        d.update(cons[p])
        d["x"] = np.ascontiguousarray(xb[order].reshape(S, D))
        in_maps.append(d)
    return in_maps


def kernel(**inp):
    in_maps = make_in_maps(**inp)
    if "nc" not in _NC_CACHE:
        _NC_CACHE["nc"] = build_program()
    res = run_bass_kernel_spmd(_NC_CACHE["nc"], in_maps, core_ids=list(range(8)))
    out = np.empty((4, S, D), np.float32)
    for core in range(8):
        b, p = core // 2, core % 2
        y = np.asarray(res.results[core]["y"]).reshape(4, SL, D)
        for m in range(4):
            out[b, (2 * m + p) * SL:(2 * m + p + 1) * SL] = y[m]
    return out
```

```python
import math
from contextlib import ExitStack

import numpy as np
import concourse.bass as bass
import concourse.mybir as mybir
from concourse.bass_utils import run_bass_kernel_spmd

F32 = mybir.dt.float32
BF16 = mybir.dt.bfloat16
AF = mybir.ActivationFunctionType
ALU = mybir.AluOpType
AX = mybir.AxisListType

S = 4096
D = 1024
DFF = 2816
NFC = 22
SL = 512
BIG = 30000.0
EPS = 1e-6
NSTEP = 14
NDS = 20
DEBUG = False
import os
KSTOP = float(os.environ.get('KSTOP', '1000'))


class StopBuild(Exception):
    pass


def chk(n):
    if KSTOP <= n:
        raise StopBuild()


class Buf:
    __slots__ = ("w", "rs")

    def __init__(self):
        self.w = None
        self.rs = {}


class Tr:
    def __init__(self, nc, st):
        self.nc = nc
        self.E = {}
        for name, h in [("pe", nc.tensor), ("act", nc.scalar), ("dve", nc.vector),
                        ("pool", nc.gpsimd), ("sp", nc.sync)]:
            sem = st.enter_context(nc.semaphore("s_" + name))
            self.E[name] = dict(h=h, sem=sem, cnt=0, waited={})
        self.ds = {}
        for q, nq in (("pool", 4), ("sp", NDS)):
            sems = [st.enter_context(nc.semaphore("d%s%d" % (q, i))) for i in range(nq)]
            self.ds[q] = dict(sems=sems, val=[0] * nq, nxt=0, n=nq)

    def wait(self, eng, ev):
        if ev is None:
            return
        sem, val, key = ev
        e = self.E[eng]
        if key == "pe" and eng == "pe":
            return
        if key in self.E:
            assert self.E[key]["cnt"] >= val, "wait on unmarked event %s" % key
        if e["waited"].get(key, 0) >= val:
            return
        e["h"].wait_ge(sem, val)
        e["waited"][key] = val

    def _deps(self, eng, reads, writes):
        for b in reads:
            self.wait(eng, b.w)
        for b in writes:
            self.wait(eng, b.w)
            for k, r in b.rs.items():
                self.wait(eng, r)

    def _record(self, ev, reads, writes):
        for b in writes:
            b.w = ev
            b.rs = {}
        for b in reads:
            b.rs[ev[2]] = ev

    def op(self, eng, fn, reads=(), writes=(), mark=True):
        self._deps(eng, reads, writes)
        e = self.E[eng]
        inst = fn(e["h"])
        if mark:
            inst.then_inc(e["sem"], 1)
            e["cnt"] += 1
            ev = (e["sem"], e["cnt"], eng)
        else:
            ev = (e["sem"], e["cnt"] + 1, eng)
        self._record(ev, reads, writes)
        return ev

    def dma(self, q, out, in_, reads=(), writes=()):
        self._deps(q, reads, writes)
        d = self.ds[q]
        i = d["nxt"]
        d["nxt"] = (i + 1) % d["n"]
        key = ("d", q, i)
        if d["val"][i] > 0:
            self.wait(q, (d["sems"][i], d["val"][i], key))
        self.E[q]["h"].dma_start(out=out, in_=in_).then_inc(d["sems"][i], 16)
        d["val"][i] += 16
        ev = (d["sems"][i], d["val"][i], key)
        self._record(ev, reads, writes)
        return ev

    def barrier(self):
        evs = []
        for k, e in self.E.items():
            if e["cnt"] > 0:
                evs.append((e["sem"], e["cnt"], k))
        for q, d in self.ds.items():
            for i in range(d["n"]):
                if d["val"][i] > 0:
                    evs.append((d["sems"][i], d["val"][i], ("d", q, i)))
        for k in self.E:
            for ev in evs:
                if ev[2] == k:
                    continue
                self.wait(k, ev)


def build_program(debug=False):
    nc = bass.Bass("TRN2", target_bir_lowering=False)

    def din(name, shape, dt=F32):
        return nc.dram_tensor(name, shape, dt, kind="ExternalInput").ap()

    x_d = din("x", [S, D])
    w1gu_d = din("w1gu", [D, 2 * DFF])
    w1d_d = din("w1d", [DFF, D])
    win_d = din("win", [D, 5444])
    wba_d = din("wba", [512, D])
    wbb_d = din("wbb", [512, D])
    wo_d = din("wo", [D, D])
    w2gu_d = din("w2gu", [D, 2 * DFF])
    w2d_d = din("w2d", [DFF, D])
    gains_d = din("gains", [128, 24])
    bg_d = din("bg", [128, 16])
    gqk_d = din("gqk", [128, 4])
    dtab_d = din("dtab", [128, 16, 384])
    cb_d = din("cb", [128, 16])
    cmask_d = din("cmask", [128, 384])
    mtab_d = din("mtab", [128, 896])
    pbv_d = din("pbv", [128, 1])
    idab_d = din("idab", [128, 3, 128])
    esel_d = din("esel", [128, 16, 128])
    gmask_d = din("gmask", [128, 2, 8, 4])
    ownhot_d = din("ownhot", [128, 2, 8, 4])
    c32_d = din("c32", [128, 3, 128])
    pow2_d = din("pow2", [128, NSTEP])
    y_d = nc.dram_tensor("y", [4 * SL, D], F32, kind="ExternalOutput").ap()
    Kc_d = nc.dram_tensor("Kc", [8, 128, S], BF16, kind="Internal").ap()
    Vc_d = nc.dram_tensor("Vc", [16, 128, 32, 66], BF16, kind="Internal").ap()
    kic_d = nc.dram_tensor("kic", [64, S], BF16, kind="Internal").ap()
    wb_shapes = {"w1gu": (D, 2 * DFF), "w1d": (DFF, D), "win": (D, 5444), "wba": (512, D), "wbb": (512, D),
                 "wo": (D, D), "w2gu": (D, 2 * DFF), "w2d": (DFF, D)}
    wsrc = {"w1gu": w1gu_d, "w1d": w1d_d, "win": win_d, "wba": wba_d, "wbb": wbb_d, "wo": wo_d, "w2gu": w2gu_d, "w2d": w2d_d}
    wbf = {k: nc.dram_tensor(k + "_bf", list(v), BF16, kind="Internal").ap() for k, v in wb_shapes.items()}

    with ExitStack() as st:
        T = Tr(nc, st)

        def sb(name, shape, dt):
            return st.enter_context(nc.sbuf_tensor("sb_" + name, shape, dt))

        def ps(name, shape, dt):
            return st.enter_context(nc.psum_tensor("ps_" + name, shape, dt))

        R1 = sb("R1", [128, 57344], mybir.dt.uint8)

        def r1view(off, shape, dt):
            n = int(np.prod(shape[1:]))
            esz = 2 if dt == BF16 else 4
            v = R1[:, off:off + n * esz].bitcast(dt)
            if len(shape) == 3:
                v = v.rearrange("p (a b) -> p a b", a=shape[1])
            return v
        actT = r1view(0, [128, NFC, SL], BF16)
        xtok = r1view(22528, [128, 4, D], F32)
        wdb = [r1view(38912, [128, NFC, 128], BF16), r1view(44544, [128, NFC, 128], BF16)]
        sqb = [r1view(50176, [128, SL], F32), r1view(52224, [128, SL], F32)]
        maskT = r1view(0, [128, 32, SL], BF16)
        Isc = r1view(32768, [128, S], F32)
        msk = r1view(49152, [128, S], BF16)
        B_R1 = Buf()

        xT = sb("xT", [128, 8, SL], F32)
        xnT = sb("xnT", [128, 8, SL], BF16)
        wgub = [sb("wgu%d" % i, [128, 8, 2, 128], BF16) for i in range(3)]
        winb = [sb("winb%d" % i, [128, 8, 128], BF16) for i in range(3)]
        wvb = sb("wvb", [128, 8, 256], BF16)
        wkib = sb("wkib", [128, 8, 64], BF16)
        wwib = sb("wwib", [128, 8, 4], BF16)
        scr = [sb("scr%d" % i, [128, SL], F32) for i in range(4)]
        rstd = sb("rstd", [128, SL], F32)
        rstd2 = sb("rstd2", [128, SL], F32)
        epst = sb("epst", [128, 1], F32)
        qaT = sb("qaT", [128, 4, SL], BF16)
        qbT = sb("qbT", [128, 4, SL], BF16)
        qiT = sb("qiT", [128, 2, SL], BF16)
        wS = sb("wS", [128, 4, 4], F32)
        kiT2 = sb("kiT2", [128, S], BF16)
        kbuf = sb("kbuf", [128, S], BF16)
        vbuf = sb("vbuf", [128, 32, 66], BF16)
        pbuf = [sb("pbuf%d" % i, [128, SL], BF16) for i in range(3)]
        ebuf = [sb("ebuf%d" % i, [128, SL], BF16) for i in range(2)]
        AT = sb("AT", [128, 8, SL], BF16)
        mixT = sb("mixT", [128, 8, SL], BF16)
        Dt = sb("Dt", [128, 16, 384], BF16)
        esel = sb("esel", [128, 16, 128], BF16)
        idab = sb("idab", [128, 3, 128], BF16)
        c32 = sb("c32", [128, 3, 128], F32)
        gains = sb("gains", [128, 24], F32)
        bg = sb("bg", [128, 16], F32)
        gqk = sb("gqk", [128, 4], F32)
        gq8 = sb("gq8", [128, 2], F32)
        cb = sb("cb", [128, 32], F32)
        mtab = sb("mtab", [128, 896], BF16)
        pbv = sb("pbv", [128, 1], F32)
        gmask = sb("gmask", [128, 2, 8, 4], F32)
        ownhot = sb("ownhot", [128, 2, 8, 4], F32)
        pow2 = sb("pow2", [128, NSTEP], F32)
        kst = [sb("kst%d" % i, [128, SL], BF16) for i in range(2)]
        vst = sb("vst", [128, 4, 8, 66], BF16)
        kist = sb("kist", [64, SL], BF16)
        kms = sb("kms", [128, 4, 16], F32)
        kmb = sb("kmb", [128, 4, 16], BF16)
        G = sb("G", [128, 8, 16], F32)
        sel = sb("sel", [128, 8, 16], F32)
        m8 = sb("m8", [128, 8, 8], F32)
        thr = sb("thr", [128, 8], F32)
        negm = sb("negm", [128, 8, 32], BF16)
        negmT = sb("negmT", [128, 8, SL], BF16)
        small = sb("small", [128, 8], F32)
        steps = sb("steps", [128, NSTEP], F32)
        rd = rstd
        dtmp = scr[0][:, 0:384]
        cmask = scr[1][:, 0:384]

        ident_b = idab[:, 0, :]
        identA = idab[:, 1, :]
        identB = idab[:, 2, :]
        ident32 = c32[:, 0, :]
        ones32 = c32[:, 1, :]
        bones32 = c32[:, 2, :]

        pbank = [ps("pb%d" % i, [128, SL], F32) for i in range(4)]
        B_pbank = [Buf() for _ in range(4)]
        pobank = [ps("po%d" % i, [128, SL], F32) for i in range(2)]
        B_po = [Buf(), Buf()]
        pmisc = ps("pmisc", [128, SL], F32)
        B_misc = Buf()
        psb = ps("psb", [128, 8, 128], BF16)
        B_psb = Buf()
        pctr = [0]

        def nextbank():
            i = pctr[0] % 4
            pctr[0] += 1
            return pbank[i], B_pbank[i]

        B_xtok = Buf()
        B_xT = [Buf() for _ in range(8)]
        B_xnT = [Buf() for _ in range(8)]
        B_actT = [Buf() for _ in range(NFC)]
        B_wgu = [[Buf(), Buf()] for _ in range(3)]
        B_wd = [Buf(), Buf()]
        B_win = [Buf() for _ in range(3)]
        B_wv, B_wki, B_wwi = Buf(), Buf(), Buf()
        B_sq = [Buf(), Buf()]
        B_scr = [Buf() for _ in range(4)]
        B_rstd, B_rstd2 = Buf(), Buf()
        B_const = Buf()
        B_qaT = [Buf() for _ in range(4)]
        B_qbT = [Buf() for _ in range(4)]
        B_qiT = [Buf(), Buf()]
        B_wS = Buf()
        B_kiT2, B_kbuf, B_vbuf = Buf(), Buf(), Buf()
        B_pbuf = [Buf() for _ in range(3)]
        B_ebuf = [Buf(), Buf()]
        B_AT = [Buf() for _ in range(8)]
        B_mixT = [Buf() for _ in range(8)]
        B_kst = [Buf(), Buf()]
        B_vst, B_kist = Buf(), Buf()
        B_km = Buf()
        B_G, B_sel, B_m8, B_thr, B_negm, B_negmT = Buf(), Buf(), Buf(), Buf(), Buf(), Buf()
        B_small, B_steps, B_rd = Buf(), Buf(), Buf()
        B_Isc, B_msk = Buf(), Buf()
        B_maskT = [Buf() for _ in range(4)]
        B_Kc = [[Buf() for _ in range(8)] for _ in range(8)]
        B_Vc = [[[Buf() for _ in range(4)] for _ in range(8)] for _ in range(2)]
        B_kic = [Buf() for _ in range(8)]
        B_y = Buf()
        sctr = [0]

        def nextscr():
            i = sctr[0] % 4
            sctr[0] += 1
            return scr[i], B_scr[i]

        stg = [r1view(i * 11264, [128, 5632], BF16) for i in range(4)]
        B_stg = [Buf() for _ in range(4)]
        B_wbf = Buf()
        si = 0
        for k in ("w1gu", "w1d", "win", "wba", "wbb", "wo", "w2gu", "w2d"):
            R_, C_ = wb_shapes[k]
            for rb in range(R_ // 128):
                i = si % 4
                si += 1
                T.dma("pool", stg[i][:, 0:C_], wsrc[k][rb * 128:(rb + 1) * 128, :], writes=[B_stg[i]])
                T.dma("sp", wbf[k][rb * 128:(rb + 1) * 128, :], stg[i][:, 0:C_], reads=[B_stg[i]], writes=[B_wbf])
        T.dma("pool", idab[:], idab_d, writes=[B_const])
        T.dma("pool", esel[:], esel_d, writes=[B_const])
        T.dma("pool", mtab[:], mtab_d, writes=[B_const])
        for dst, src in [(c32, c32_d), (gains, gains_d), (bg, bg_d), (gqk, gqk_d), (pbv, pbv_d),
                         (gmask, gmask_d), (ownhot, ownhot_d), (pow2, pow2_d)]:
            T.dma("sp", dst[:], src, writes=[B_const])
        T.dma("sp", cmask, cmask_d, writes=[B_scr[1]])
        T.dma("sp", cb[:, 0:16], cb_d, writes=[B_const])
        T.op("dve", lambda e: e.memset(epst[:], EPS), writes=[B_const])
        T.op("dve", lambda e: e.memset(vst[:], 1.0), writes=[B_vst])
        T.op("dve", lambda e: e.memset(negm[:], 0.0), writes=[B_negm])
        T.op("dve", lambda e: e.memset(negmT[:], 0.0), writes=[B_negmT])
        T.op("dve", lambda e: e.tensor_scalar(out=gq8[:], in0=gqk[:, 0:2], scalar1=0.125, scalar2=None, op0=ALU.mult),
             reads=[B_const], writes=[B_const])
        T.op("dve", lambda e: e.tensor_scalar(out=cb[:, 16:32], in0=cb[:, 0:16], scalar1=pbv[:, 0:1], scalar2=None, op0=ALU.add),
             reads=[B_const], writes=[B_const])
        for h in range(16):
            T.dma("sp", dtmp, dtab_d[:, h, :], writes=[B_scr[0]])
            T.op("dve", lambda e, h=h: e.scalar_tensor_tensor(out=Dt[:, h, :], in0=dtmp, scalar=cb[:, h:h + 1], in1=cmask,
                                                             op0=ALU.subtract, op1=ALU.add),
                 reads=[B_scr[0], B_scr[1], B_const], writes=[B_const])

        def mmgroup(out_ap, obuf, items, extra_reads=()):
            n = len(items)
            for i, (l, r, rb) in enumerate(items):
                T.op("pe", lambda e, l=l, r=r, i=i: e.matmul(out_ap, lhsT=l, rhs=r, start=(i == 0), stop=(i == n - 1)),
                     reads=list(rb) + list(extra_reads), writes=[obuf], mark=(i == n - 1))

        win_v = wbf["win"].rearrange("(kc p) n -> p kc n", p=128)

        def rmsnorm(gcol0):
            for kc in range(8):
                sq, bsq = sqb[kc % 2], B_sq[kc % 2]
                T.op("act", lambda e, kc=kc, sq=sq: e.activation(out=sq, in_=xT[:, kc, :], func=AF.Square),
                     reads=[B_xT[kc]], writes=[bsq])
                T.op("pe", lambda e, kc=kc, sq=sq: e.matmul(pmisc[:], lhsT=ones32, rhs=sq, start=(kc == 0), stop=(kc == 7)),
                     reads=[bsq, B_const], writes=[B_misc], mark=True)
            T.op("act", lambda e: e.activation(out=rstd[:], in_=pmisc[:], func=AF.Sqrt, bias=epst[:, 0:1], scale=1.0 / D),
                 reads=[B_misc, B_const], writes=[B_rstd])
            T.op("dve", lambda e: e.reciprocal(out=rstd2[:], in_=rstd[:]), reads=[B_rstd], writes=[B_rstd2])
            for kc in range(8):
                T.op("dve", lambda e, kc=kc: e.scalar_tensor_tensor(out=xnT[:, kc, :], in0=xT[:, kc, :],
                                                                   scalar=gains[:, gcol0 + kc:gcol0 + kc + 1], in1=rstd2[:],
                                                                   op0=ALU.mult, op1=ALU.mult),
                     reads=[B_xT[kc], B_rstd2, B_const], writes=[B_xnT[kc]])

        def ffn(wgu_d, wd_d):
            wgu_v = wgu_d.rearrange("(kc p) (two f) -> p kc two f", p=128, two=2)
            wd_v = wd_d.rearrange("(c p) n -> p c n", p=128)

            def load_gu(c):
                T.dma("sp", wgub[c % 3][:, :, 0, :], wgu_v[:, :, 0, c * 128:(c + 1) * 128], writes=[B_wgu[c % 3][0]])
                T.dma("sp", wgub[c % 3][:, :, 1, :], wgu_v[:, :, 1, c * 128:(c + 1) * 128], writes=[B_wgu[c % 3][1]])

            def load_d(cc):
                T.dma("sp", wdb[cc % 2], wd_v[:, :, cc * 128:(cc + 1) * 128], writes=[B_wd[cc % 2]])
            load_gu(0)
            load_gu(1)
            for c in range(NFC):
                if c + 2 < NFC:
                    load_gu(c + 2)
                w = wgub[c % 3]
                gb_, bgb = nextbank()
                ub_, bub = nextbank()
                mmgroup(gb_[:], bgb, [(w[:, kc, 0, :], xnT[:, kc, :], [B_wgu[c % 3][0], B_xnT[kc]]) for kc in range(8)])
                mmgroup(ub_[:], bub, [(w[:, kc, 1, :], xnT[:, kc, :], [B_wgu[c % 3][1], B_xnT[kc]]) for kc in range(8)])
                sg, bsg = nextscr()
                T.op("act", lambda e, sg=sg, gb_=gb_: e.activation(out=sg[:], in_=gb_[:], func=AF.Silu), reads=[bgb], writes=[bsg])
                T.op("dve", lambda e, sg=sg, ub_=ub_, c=c: e.tensor_tensor(out=actT[:, c, :], in0=ub_[:], in1=sg[:], op=ALU.mult),
                     reads=[bub, bsg], writes=[B_actT[c]])
                if c == NFC - 3:
                    load_d(0)
                if c == NFC - 2:
                    load_d(1)
            for cc in range(8):
                w = wdb[cc % 2]
                bk, bbk = nextbank()
                mmgroup(bk[:], bbk, [(w[:, c, :], actT[:, c, :], [B_wd[cc % 2], B_actT[c]]) for c in range(NFC)])
                T.op("dve", lambda e, bk=bk, cc=cc: e.scalar_tensor_tensor(out=xT[:, cc, :], in0=bk[:], scalar=0.5, in1=xT[:, cc, :],
                                                                          op0=ALU.mult, op1=ALU.add),
                     reads=[bbk, B_xT[cc]], writes=[B_xT[cc]])
                if cc + 2 < 8:
                    load_d(cc + 2)

        wctr = [0]

        def load_win(col0):
            i = wctr[0] % 3
            wctr[0] += 1
            T.dma("sp", winb[i][:], win_v[:, :, col0:col0 + 128], writes=[B_win[i]])
            return winb[i], B_win[i]

        def proj_pair_normed(col0, gain_ap, out_ap, obufs):
            w, bw = load_win(col0)
            bk, bbk = nextbank()
            mmgroup(bk[:], bbk, [(w[:, kc, :], xnT[:, kc, :], [bw, B_xnT[kc]]) for kc in range(8)])
            raw, braw = nextscr()
            sq, bsq = nextscr()
            T.op("act", lambda e: e.activation(out=raw[:], in_=bk[:], func=AF.Copy), reads=[bbk], writes=[braw])
            T.op("act", lambda e: e.activation(out=sq[:], in_=bk[:], func=AF.Square), reads=[bbk], writes=[bsq])
            T.op("pe", lambda e: e.matmul(pmisc[:], lhsT=bones32, rhs=sq[:], start=True, stop=True),
                 reads=[bsq, B_const], writes=[B_misc])
            T.op("act", lambda e: e.activation(out=rstd[:], in_=pmisc[:], func=AF.Sqrt, bias=epst[:, 0:1], scale=1.0 / 64),
                 reads=[B_misc, B_const], writes=[B_rstd])
            T.op("dve", lambda e: e.reciprocal(out=rstd2[:], in_=rstd[:]), reads=[B_rstd], writes=[B_rstd2])
            T.op("dve", lambda e: e.scalar_tensor_tensor(out=out_ap, in0=raw[:], scalar=gain_ap, in1=rstd2[:], op0=ALU.mult, op1=ALU.mult),
                 reads=[braw, B_rstd2, B_const], writes=obufs)

        def phase1(slot, own, m):
            T.dma("sp", xtok, x_d[slot * SL:(slot + 1) * SL, :].rearrange("(tt p) f -> p tt f", p=128), writes=[B_xtok])
            for kc in range(8):
                bk, bbk = nextbank()
                for tt in range(4):
                    T.op("pe", lambda e, bk=bk, tt=tt, kc=kc: e.transpose(out=bk[:, tt * 128:(tt + 1) * 128],
                                                                         in_=xtok[:, tt, kc * 128:(kc + 1) * 128], identity=ident32),
                         reads=[B_xtok, B_const], writes=[bbk], mark=(tt == 3))
                if kc % 2 == 0:
                    T.op("act", lambda e, bk=bk, kc=kc: e.activation(out=xT[:, kc, :], in_=bk[:], func=AF.Copy), reads=[bbk], writes=[B_xT[kc]])
                else:
                    T.op("dve", lambda e, bk=bk, kc=kc: e.tensor_copy(out=xT[:, kc, :], in_=bk[:]), reads=[bbk], writes=[B_xT[kc]])
            chk(1)
            rmsnorm(0)
            chk(2)
            ffn(wbf["w1gu"], wbf["w1d"])
            chk(3)
            rmsnorm(8)
            for grp, col_base, gcol in ((0, 512, 2), (1, 2048, 3)):
                for pr in range(4):
                    ks, bks = kst[pr % 2], B_kst[pr % 2]
                    proj_pair_normed(col_base + pr * 128, gqk[:, gcol:gcol + 1], ks[:], [bks])
                    if grp == 1:
                        T.op("dve", lambda e, ks=ks, pr=pr: e.tensor_reduce(out=kms[:, pr, 2 * slot:2 * slot + 2],
                                                                           in_=ks[:].rearrange("p (a b) -> p a b", a=2),
                                                                           axis=AX.X, op=ALU.add),
                             reads=[bks], writes=[B_km])
                        T.op("dve", lambda e, pr=pr: e.tensor_scalar(out=kmb[:, pr, 2 * slot:2 * slot + 2], in0=kms[:, pr, 2 * slot:2 * slot + 2],
                                                                    scalar1=1.0 / 256, scalar2=None, op0=ALU.mult),
                             reads=[B_km], writes=[B_km])
                    T.dma("sp", Kc_d[grp * 4 + pr, :, slot * SL:(slot + 1) * SL], ks[:], reads=[bks], writes=[B_Kc[grp * 4 + pr][slot]])
            for grp, col_base in ((0, 1024), (1, 2560)):
                for half in range(2):
                    T.dma("sp", wvb[:], win_v[:, :, col_base + half * 256:col_base + (half + 1) * 256], writes=[B_wv])
                    for tt in range(4):
                        bk, bbk = nextbank()
                        mmgroup(bk[:, 0:256], bbk, [(xnT[:, kc, tt * 128:(tt + 1) * 128], wvb[:, kc, :], [B_wv, B_xnT[kc]]) for kc in range(8)])
                        T.op("act", lambda e, bk=bk, tt=tt, half=half: e.activation(
                            out=vst[:, tt, half * 4:(half + 1) * 4, 0:64],
                            in_=bk[:, 0:256].rearrange("p (h d) -> p h d", h=4), func=AF.Copy),
                            reads=[bbk], writes=[B_vst])
                for tt in range(4):
                    T.dma("sp", Vc_d[grp * 8:(grp + 1) * 8].rearrange("h p t d -> p t h d")[:, slot * 4 + tt, :, :], vst[:, tt, :, :],
                          reads=[B_vst], writes=[B_Vc[grp][slot][tt]])
            T.dma("sp", wkib[:], win_v[:, :, 3328:3392], writes=[B_wki])
            bk, bbk = nextbank()
            mmgroup(bk[0:64, :], bbk, [(wkib[:, kc, :], xnT[:, kc, :], [B_wki, B_xnT[kc]]) for kc in range(8)])
            T.op("act", lambda e: e.activation(out=kist[:], in_=bk[0:64, :], func=AF.Copy), reads=[bbk], writes=[B_kist])
            T.dma("sp", kic_d[:, slot * SL:(slot + 1) * SL], kist[:], reads=[B_kist], writes=[B_kic[slot]])
            if not own:
                return
            for pr in range(4):
                proj_pair_normed(pr * 128, gq8[:, 0:1], qaT[:, pr, :], [B_qaT[pr]])
            for pr in range(4):
                proj_pair_normed(1536 + pr * 128, gq8[:, 1:2], qbT[:, pr, :], [B_qbT[pr]])
            for pr in range(2):
                w, bw = load_win(3072 + pr * 128)
                bk, bbk = nextbank()
                mmgroup(bk[:], bbk, [(w[:, kc, :], xnT[:, kc, :], [bw, B_xnT[kc]]) for kc in range(8)])
                T.op("act", lambda e, bk=bk, pr=pr: e.activation(out=qiT[:, pr, :], in_=bk[:], func=AF.Copy), reads=[bbk], writes=[B_qiT[pr]])
            T.dma("sp", wwib[:], win_v[:, :, 3392:3396], writes=[B_wwi])
            for tt in range(4):
                bk, bbk = nextbank()
                mmgroup(bk[:, 0:4], bbk, [(xnT[:, kc, tt * 128:(tt + 1) * 128], wwib[:, kc, :], [B_wwi, B_xnT[kc]]) for kc in range(8)])
                T.op("dve", lambda e, bk=bk, tt=tt: e.tensor_scalar(out=wS[:, tt, :], in0=bk[:, 0:4], scalar1=0.0625, scalar2=None, op0=ALU.mult),
                     reads=[bbk], writes=[B_wS])

        def attention(m):
            nsl = 2 * m + 2
            nk = nsl * SL
            nkt = nsl * 4
            nb = nsl * 2
            k0 = 1024 * m
            for hf in range(2):
                T.dma("sp", kiT2[hf * 64:(hf + 1) * 64, 0:nk], kic_d[:, 0:nk], reads=B_kic[0:nsl], writes=[B_kiT2])
            for r in range(4):
                for j in range(nsl):
                    banks = [nextbank() for _ in range(4)]
                    for h in range(4):
                        p0 = 64 * (h % 2)
                        bk, bbk = banks[h]
                        mmgroup(bk[:], bbk, [(qiT[p0:p0 + 64, h // 2, r * 128:(r + 1) * 128], kiT2[p0:p0 + 64, j * SL:(j + 1) * SL],
                                              [B_qiT[h // 2], B_kiT2])])
                    for h in range(4):
                        bk, bbk = banks[h]
                        rl, brl = nextscr()
                        T.op("act", lambda e, bk=bk, rl=rl: e.activation(out=rl[:], in_=bk[:], func=AF.Relu), reads=[bbk], writes=[brl])
                        dst = Isc[:, j * SL:(j + 1) * SL]
                        if h == 0:
                            T.op("dve", lambda e, rl=rl, dst=dst: e.tensor_scalar(out=dst, in0=rl[:], scalar1=wS[:, r, 0:1], scalar2=None, op0=ALU.mult),
                                 reads=[brl, B_wS], writes=[B_Isc])
                        else:
                            T.op("dve", lambda e, rl=rl, dst=dst, h=h: e.scalar_tensor_tensor(out=dst, in0=rl[:], scalar=wS[:, r, h:h + 1], in1=dst,
                                                                                            op0=ALU.mult, op1=ALU.add),
                                 reads=[brl, B_wS, B_Isc], writes=[B_Isc])
                chk(6 if m == 0 else (12.31 if r == 0 else 12.39))
                Bv, lo, mid, cnt, tt_ = (small[:, i:i + 1] for i in range(5))
                T.op("dve", lambda e: e.tensor_reduce(out=Bv, in_=Isc[:, 0:nk], axis=AX.X, op=ALU.max, apply_absolute_value=True),
                     reads=[B_Isc], writes=[B_small])
                if m == 1 and r == 0:
                    chk(12.311)
                T.op("dve", lambda e: e.tensor_scalar(out=lo, in0=Bv, scalar1=1.0, scalar2=-1.0, op0=ALU.add, op1=ALU.mult),
                     reads=[B_small], writes=[B_small])
                T.op("dve", lambda e: e.tensor_scalar(out=steps[:], in0=pow2[:], scalar1=lo, scalar2=-2.0, op0=ALU.mult, op1=ALU.mult),
                     reads=[B_small, B_const], writes=[B_steps])
                T.op("dve", lambda e: e.tensor_tensor(out=Isc[:, k0:k0 + SL], in0=Isc[:, k0:k0 + SL],
                                                      in1=mtab[:, 384 - 128 * r:896 - 128 * r], op=ALU.add),
                     reads=[B_Isc, B_const], writes=[B_Isc])
                T.op("dve", lambda e: e.tensor_scalar(out=Isc[:, k0 + SL:k0 + 2 * SL], in0=Isc[:, k0 + SL:k0 + 2 * SL], scalar1=pbv[:, 0:1],
                                                      scalar2=None, op0=ALU.add),
                     reads=[B_Isc, B_const], writes=[B_Isc])
                if m == 1 and r == 0:
                    chk(12.312)
                for s_ in range(NSTEP):
                    if m == 1 and r == 0 and s_ == 1:
                        chk(12.313)
                    T.op("dve", lambda e, s_=s_: e.tensor_tensor(out=mid, in0=lo, in1=steps[:, s_:s_ + 1], op=ALU.add),
                         reads=[B_small, B_steps], writes=[B_small])
                    T.op("dve", lambda e: e.tensor_scalar(out=msk[:, 0:nk], in0=Isc[:, 0:nk], scalar1=mid, scalar2=0.0, op0=ALU.is_ge, op1=ALU.add,
                                                          accum_out=cnt),
                         reads=[B_Isc, B_small], writes=[B_msk, B_small])
                    T.op("dve", lambda e, s_=s_: e.tensor_scalar(out=tt_, in0=cnt, scalar1=256.0, scalar2=steps[:, s_:s_ + 1], op0=ALU.is_ge, op1=ALU.mult),
                         reads=[B_small, B_steps], writes=[B_small])
                    T.op("dve", lambda e: e.tensor_tensor(out=lo, in0=lo, in1=tt_, op=ALU.add), reads=[B_small], writes=[B_small])
                T.op("dve", lambda e: e.tensor_scalar(out=msk[:, 0:nk], in0=Isc[:, 0:nk], scalar1=lo, scalar2=None, op0=ALU.is_ge),
                     reads=[B_Isc, B_small], writes=[B_msk])
                if m == 1 and r == 0:
                    chk(12.314)
                for g0 in range(0, nkt, 8):
                    if m == 1 and r == 0 and g0 == 8:
                        chk(12.315)
                    for i in range(8):
                        kt = g0 + i
                        T.op("pe", lambda e, i=i, kt=kt: e.transpose(out=psb[:, i, :], in_=msk[:, kt * 128:(kt + 1) * 128], identity=ident_b),
                             reads=[B_msk, B_const], writes=[B_psb], mark=(i == 7))
                    T.op("act", lambda e, g0=g0: e.activation(out=maskT[:, g0:g0 + 8, r * 128:(r + 1) * 128], in_=psb[:], func=AF.Copy),
                         reads=[B_psb], writes=[B_maskT[r]])
                chk(7 if m == 0 else (12.32 if r == 0 else 12.39))
                half = r // 2
                T.op("dve", lambda e: e.memset(G[:], -BIG), writes=[B_G])
                gbo, bgbo = nextbank()
                for par, (gbk, bgbk) in enumerate(((pmisc, B_misc), (gbo, bgbo))):
                    p0 = 64 * par
                    for pr_ in range(4):
                        h = 2 * pr_ + par
                        T.op("pe", lambda e, h=h, p0=p0, pr_=pr_, gbk=gbk: e.matmul(gbk[:, h * 16:h * 16 + nb], lhsT=qbT[p0:p0 + 64, pr_, r * 128:(r + 1) * 128],
                                                                                  rhs=kmb[p0:p0 + 64, pr_, 0:nb], start=True, stop=True),
                             reads=[B_qbT[pr_], B_km], writes=[bgbk], mark=(pr_ == 3))
                    T.op("dve", lambda e, par=par, gbk=gbk: e.tensor_copy(
                        out=G[:].rearrange("p (hp two) b -> p hp two b", two=2)[:, :, par, 0:nb],
                        in_=gbk[:, 0:128].rearrange("p (hp two b) -> p hp two b", two=2, b=16)[:, :, par, 0:nb]),
                        reads=[bgbk], writes=[B_G])
                T.op("dve", lambda e: e.tensor_tensor(out=G[:, :, 4 * m:4 * m + 4], in0=G[:, :, 4 * m:4 * m + 4], in1=gmask[:, half, :, :], op=ALU.add),
                     reads=[B_G, B_const], writes=[B_G])
                for h in range(8):
                    T.op("dve", lambda e, h=h: e.max(out=m8[:, h, :], in_=G[:, h, :]), reads=[B_G], writes=[B_m8])
                T.op("dve", lambda e: e.tensor_scalar(out=thr[:], in0=m8[:, :, 2], scalar1=-BIG / 2, scalar2=None, op0=ALU.max),
                     reads=[B_m8], writes=[B_thr])
                for h in range(8):
                    T.op("dve", lambda e, h=h: e.tensor_scalar(out=sel[:, h, :], in0=G[:, h, :], scalar1=thr[:, h:h + 1], scalar2=None, op0=ALU.is_ge),
                         reads=[B_G, B_thr], writes=[B_sel])
                T.op("dve", lambda e: e.tensor_tensor(out=sel[:, :, 4 * m:4 * m + 4], in0=sel[:, :, 4 * m:4 * m + 4], in1=ownhot[:, half, :, :], op=ALU.add),
                     reads=[B_sel, B_const], writes=[B_sel])
                T.op("dve", lambda e: e.tensor_scalar(out=negm[:, :, 0:16], in0=sel[:], scalar1=-1.0, scalar2=BIG, op0=ALU.add, op1=ALU.mult),
                     reads=[B_sel], writes=[B_negm])
                for h in range(8):
                    T.op("pe", lambda e, h=h: e.transpose(out=psb[0:32, h, :], in_=negm[:, h, :], identity=ident_b),
                         reads=[B_negm, B_const], writes=[B_psb], mark=(h == 7))
                T.op("act", lambda e: e.activation(out=negmT[0:32, :, r * 128:(r + 1) * 128], in_=psb[0:32, :, :], func=AF.Copy),
                     reads=[B_psb], writes=[B_negmT])
                if m == 1 and r == 0:
                    chk(12.33)

            chk(8 if m == 0 else 12.4)
            pbi = [0]
            for hg in range(16):
                moba = hg >= 8
                h = hg % 8
                pr = h // 2
                p0 = 64 * (h % 2)
                grp = 1 if moba else 0
                if h % 2 == 0:
                    T.dma("sp", kbuf[:, 0:nk], Kc_d[grp * 4 + pr, :, 0:nk], reads=B_Kc[grp * 4 + pr][0:nsl], writes=[B_kbuf])
                T.dma("sp", vbuf[:, 0:nkt, :], Vc_d[hg, :, 0:nkt, :], reads=[b for sl_ in B_Vc[grp][0:nsl] for b in sl_], writes=[B_vbuf])
                qT = qbT if moba else qaT
                bq = (B_qbT if moba else B_qaT)[pr]
                po, bpo = pobank[hg % 2], B_po[hg % 2]
                g, jj = h // 3, h % 3
                def stageA(kt):
                    j = kt - 8 * m
                    q0 = 128 * j if 0 <= j < 4 else 0
                    sbk, bsbk = nextbank()
                    items = [(sbk[:, q0:SL], kbuf[p0:p0 + 64, kt * 128:(kt + 1) * 128], qT[p0:p0 + 64, pr, q0:SL], [B_kbuf, bq])]
                    if 0 <= j < 4:
                        W = min(384, SL - q0)
                        items.append((sbk[:, q0:q0 + W], ident_b, Dt[:, hg, 0:W], [B_const]))
                    if m >= 1 and kt == 8 * m - 1:
                        items.append((sbk[:, 0:256], identA, Dt[:, hg, 128:384], [B_const]))
                    if kt == 8 * m + 7:
                        items.append((sbk[:, 0:256], identB, Dt[:, hg, 128:384], [B_const]))
                    if moba:
                        items.append((sbk[:, q0:SL], esel[:, kt // 2, :], negmT[:, h, q0:SL], [B_const, B_negmT]))
                    n = len(items)
                    for i, (o, l, rr, rb) in enumerate(items):
                        T.op("pe", lambda e, o=o, l=l, rr=rr, i=i, n=n: e.matmul(o, lhsT=l, rhs=rr, start=(i == 0), stop=(i == n - 1)),
                             reads=rb, writes=[bsbk], mark=(i == n - 1))
                    pb_, bpb = pbuf[pbi[0] % 3], B_pbuf[pbi[0] % 3]
                    if moba:
                        eb_, beb = pb_, bpb
                    else:
                        eb_, beb = ebuf[pbi[0] % 2], B_ebuf[pbi[0] % 2]
                    pbi[0] += 1
                    col = hg + (16 if kt >= 8 * m + 4 else 0)
                    T.op("act", lambda e: e.activation(out=eb_[:, q0:SL], in_=sbk[:, q0:SL], func=AF.Exp, bias=cb[:, col:col + 1], scale=1.0),
                         reads=[bsbk, B_const], writes=[beb])
                    if not moba:
                        T.op("dve", lambda e: e.scalar_tensor_tensor(out=pb_[:, q0:SL], in0=eb_[:, q0:SL], scalar=1.0,
                                                                     in1=maskT[:, kt, q0:SL], op0=ALU.mult, op1=ALU.mult),
                             reads=[beb] + B_maskT, writes=[bpb])
                    return (kt, q0, pb_, bpb)

                def stageB(st_):
                    kt, q0, pb_, bpb = st_
                    T.op("pe", lambda e: e.matmul(po[0:65, q0:SL], lhsT=vbuf[:, kt, 0:65], rhs=pb_[:, q0:SL],
                                                  start=(kt == 0), stop=(kt == nkt - 1)),
                         reads=[B_vbuf, bpb], writes=[bpo], mark=True)

                prev_st = None
                for kt in range(nkt):
                    cur = stageA(kt)
                    if prev_st is not None:
                        stageB(prev_st)
                    prev_st = cur
                stageB(prev_st)
                chk(8.4 if hg == 0 else (8.7 if hg == 8 else 8.99))
                T.op("dve", lambda e, po=po: e.reciprocal(out=rd[64:65, :], in_=po[64:65, :]), reads=[bpo], writes=[B_rd])
                if hg == 0:
                    chk(8.41)
                T.op("pe", lambda e: e.matmul(pmisc[:], lhsT=ones32[64:65, :], rhs=rd[64:65, :], start=True, stop=True),
                     reads=[B_rd, B_const], writes=[B_misc])
                if hg == 0:
                    chk(8.42)
                tmpo, btmpo = nextscr()
                T.op("act", lambda e, po=po, tmpo=tmpo, p0=p0: e.activation(out=tmpo[p0:p0 + 64, :], in_=po[0:64, :], func=AF.Copy),
                     reads=[bpo], writes=[btmpo])
                ap_ = pr + (4 if moba else 0)
                if hg == 0:
                    chk(8.43)
                T.op("dve", lambda e, tmpo=tmpo, p0=p0, ap_=ap_: e.tensor_tensor(out=AT[p0:p0 + 64, ap_, :], in0=pmisc[p0:p0 + 64, :],
                                                                                 in1=tmpo[p0:p0 + 64, :], op=ALU.mult),
                     reads=[btmpo, B_misc], writes=[B_AT[ap_]])
                chk(8.5 if hg == 0 else (8.6 if hg == 7 else (8.8 if hg == 8 else 8.99)))

        def mix(m):
            wba_v = wbf["wba"].rearrange("(kc p) n -> p kc n", p=128)
            wbb_v = wbf["wbb"].rearrange("(kc p) n -> p kc n", p=128)
            wo_v = wbf["wo"].rearrange("(kc p) n -> p kc n", p=128)
            for cc in range(8):
                wa, bwa = winb[wctr[0] % 3], B_win[wctr[0] % 3]
                wctr[0] += 1
                T.dma("sp", wa[:, 0:4, :], wba_v[:, :, cc * 128:(cc + 1) * 128], writes=[bwa])
                T.dma("sp", wa[:, 4:8, :], wbb_v[:, :, cc * 128:(cc + 1) * 128], writes=[bwa])
                wga, bwga = load_win(3396 + cc * 128)
                wgb, bwgb = load_win(3396 + 1024 + cc * 128)
                bA, bbA = nextbank()
                bB, bbB = nextbank()
                bGa, bbGa = nextbank()
                bGb, bbGb = nextbank()
                mmgroup(bA[:], bbA, [(wa[:, kc, :], AT[:, kc, :], [bwa, B_AT[kc]]) for kc in range(4)])
                mmgroup(bB[:], bbB, [(wa[:, 4 + kc, :], AT[:, 4 + kc, :], [bwa, B_AT[4 + kc]]) for kc in range(4)])
                mmgroup(bGa[:], bbGa, [(wga[:, kc, :], xnT[:, kc, :], [bwga, B_xnT[kc]]) for kc in range(8)])
                mmgroup(bGb[:], bbGb, [(wgb[:, kc, :], xnT[:, kc, :], [bwgb, B_xnT[kc]]) for kc in range(8)])
                ga, bga = nextscr()
                gb2, bgb2 = nextscr()
                T.op("act", lambda e, ga=ga, bGa=bGa, cc=cc: e.activation(out=ga[:], in_=bGa[:], func=AF.Sigmoid, bias=bg[:, cc:cc + 1], scale=1.0),
                     reads=[bbGa, B_const], writes=[bga])
                T.op("act", lambda e, gb2=gb2, bGb=bGb, cc=cc: e.activation(out=gb2[:], in_=bGb[:], func=AF.Sigmoid, bias=bg[:, 8 + cc:9 + cc], scale=1.0),
                     reads=[bbGb, B_const], writes=[bgb2])
                T.op("dve", lambda e, ga=ga, bA=bA: e.tensor_tensor(out=ga[:], in0=bA[:], in1=ga[:], op=ALU.mult), reads=[bbA, bga], writes=[bga])
                T.op("dve", lambda e, gb2=gb2, bB=bB: e.tensor_tensor(out=gb2[:], in0=bB[:], in1=gb2[:], op=ALU.mult), reads=[bbB, bgb2], writes=[bgb2])
                T.op("dve", lambda e, ga=ga, gb2=gb2, cc=cc: e.tensor_tensor(out=mixT[:, cc, :], in0=ga[:], in1=gb2[:], op=ALU.add),
                     reads=[bga, bgb2], writes=[B_mixT[cc]])
            for cc in range(8):
                w, bw = winb[wctr[0] % 3], B_win[wctr[0] % 3]
                wctr[0] += 1
                T.dma("sp", w[:], wo_v[:, :, cc * 128:(cc + 1) * 128], writes=[bw])
                bk, bbk = nextbank()
                mmgroup(bk[:], bbk, [(w[:, kc, :], mixT[:, kc, :], [bw, B_mixT[kc]]) for kc in range(8)])
                T.op("dve", lambda e, bk=bk, cc=cc: e.tensor_tensor(out=xT[:, cc, :], in0=bk[:], in1=xT[:, cc, :], op=ALU.add),
                     reads=[bbk, B_xT[cc]], writes=[B_xT[cc]])

        def output(m):
            for tt in range(4):
                for half in range(2):
                    bk, bbk = nextbank()
                    for i in range(4):
                        kc = half * 4 + i
                        T.op("pe", lambda e, bk=bk, i=i, kc=kc, tt=tt: e.transpose(out=bk[:, i * 128:(i + 1) * 128],
                                                                                  in_=xT[:, kc, tt * 128:(tt + 1) * 128], identity=ident32),
                             reads=[B_xT[kc], B_const], writes=[bbk], mark=(i == 3))
                    if half == 0:
                        T.op("act", lambda e, bk=bk, tt=tt: e.activation(out=xtok[:, tt, 0:512], in_=bk[:], func=AF.Copy), reads=[bbk], writes=[B_xtok])
                    else:
                        T.op("dve", lambda e, bk=bk, tt=tt: e.tensor_copy(out=xtok[:, tt, 512:1024], in_=bk[:]), reads=[bbk], writes=[B_xtok])
            T.dma("sp", y_d[m * SL:(m + 1) * SL, :].rearrange("(tt p) f -> p tt f", p=128), xtok, reads=[B_xtok], writes=[B_y])

        T.barrier()
        try:
            chk(0)
            for m in range(4):
                phase1(2 * m + 1, False, m)
                chk(4 if m == 0 else 12.2)
                phase1(2 * m, True, m)
                chk(5 if m == 0 else 12.3)
                T.barrier()
                attention(m)
                chk(9 if m == 0 else 12.5)
                T.barrier()
                mix(m)
                chk(10 if m == 0 else 12.6)
                rmsnorm(16)
                ffn(wbf["w2gu"], wbf["w2d"])
                chk(11)
                output(m)
                chk(12 + m)
        except StopBuild:
            pass
        T.barrier()
    return nc


def _t5_bucket(d):
    d = np.maximum(d, 0)
    df = np.maximum(d, 1).astype(np.float64)
    large = 16 + (np.log(df / 16.0) / math.log(128 / 16) * 16).astype(np.int64)
    large = np.minimum(large, 31)
    return np.where(d < 16, d, large)


def _consts(p, rel_bias):
    c = {}
    s = np.arange(128)[:, None]
    cc = np.arange(384)[None, :]
    bucket = _t5_bucket(cc - s)
    c["dtab"] = np.ascontiguousarray(np.transpose(rel_bias[bucket], (0, 2, 1))).astype(np.float32)
    c["cb"] = np.ascontiguousarray(np.broadcast_to(rel_bias[31][None, :], (128, 16))).astype(np.float32)
    c["cmask"] = np.where(cc >= s, 0.0, -BIG).astype(np.float32)
    c2 = np.arange(896)[None, :]
    c["mtab"] = np.where(c2 <= s + 384, 0.0, -BIG).astype(np.float32)
    pb = 0.0 if p == 1 else -BIG
    c["pbv"] = np.full((128, 1), pb, np.float32)
    eye = np.eye(128, dtype=np.float32)
    c["idab"] = np.ascontiguousarray(np.stack([eye, eye * (1.0 if p == 0 else 0.0), eye * (1.0 if p == 1 else 0.0)], axis=1))
    pp = np.arange(128)
    es = np.zeros((128, 16, 128), np.float32)
    for b in range(16):
        es[pp == b, b, :] = 1.0
    c["esel"] = es
    gm = np.zeros((2, 4), np.float32)
    oh = np.zeros((2, 4), np.float32)
    gm[0] = [-BIG, -BIG, pb, pb]
    oh[0] = [1, 0, 0, 0]
    gm[1] = [0.0, -BIG, pb, pb]
    oh[1] = [0, 1, 0, 0]
    c["gmask"] = np.ascontiguousarray(np.broadcast_to(gm[None, :, None, :], (128, 2, 8, 4))).astype(np.float32)
    c["ownhot"] = np.ascontiguousarray(np.broadcast_to(oh[None, :, None, :], (128, 2, 8, 4))).astype(np.float32)
    bo = np.zeros((128, 128), np.float32)
    bo[0:64, 0:64] = 1.0
    bo[64:128, 64:128] = 1.0
    c["c32"] = np.ascontiguousarray(np.stack([eye, np.ones((128, 128), np.float32), bo], axis=1))
    c["pow2"] = np.ascontiguousarray(np.broadcast_to((0.5 ** (np.arange(NSTEP) + 1))[None, :], (128, NSTEP))).astype(np.float32)
    return c


_NC_CACHE = {}


def make_in_maps(**inp):
    f = lambda k: np.asarray(inp[k], dtype=np.float32)
    x = f("x")
    fm = lambda v: np.ascontiguousarray(v.reshape(8, 128).T)
    gains = np.concatenate([fm(f("ffn1_norm")[0]), fm(f("mix_norm")[0]), fm(f("ffn2_norm")[0])], axis=1).astype(np.float32)
    bgv = np.ascontiguousarray(f("b_gate")[0].reshape(16, 128).T)
    gqk = np.stack([np.tile(f("q_norm_dsa")[0], 2), np.tile(f("q_norm_moba")[0], 2),
                    np.tile(f("k_norm_dsa")[0], 2), np.tile(f("k_norm_moba")[0], 2)], axis=1).astype(np.float32)
    rel_bias = f("rel_bias")
    shared = {
        "w1gu": np.ascontiguousarray(f("ffn1_w_gu")[0]), "w1d": np.ascontiguousarray(f("ffn1_w_down")[0]),
        "win": np.ascontiguousarray(f("w_in")[0]), "wba": np.ascontiguousarray(f("w_branch_dsa")[0]),
        "wbb": np.ascontiguousarray(f("w_branch_moba")[0]), "wo": np.ascontiguousarray(f("w_out")[0]),
        "w2gu": np.ascontiguousarray(f("ffn2_w_gu")[0]), "w2d": np.ascontiguousarray(f("ffn2_w_down")[0]),
        "gains": np.ascontiguousarray(gains), "bg": bgv, "gqk": np.ascontiguousarray(gqk),
    }
    cons = [_consts(0, rel_bias), _consts(1, rel_bias)]
    in_maps = []
    for core in range(8):
        b, p = core // 2, core % 2
        xb = x[b].reshape(8, SL, D)
        order = list(range(8)) if p == 0 else [1, 0, 3, 2, 5, 4, 7, 6]
        d = dict(shared)
        d.update(cons[p])
        d["x"] = np.ascontiguousarray(xb[order].reshape(S, D))
        in_maps.append(d)
    return in_maps


def kernel(**inp):
    in_maps = make_in_maps(**inp)
    if "nc" not in _NC_CACHE:
        _NC_CACHE["nc"] = build_program()
    res = run_bass_kernel_spmd(_NC_CACHE["nc"], in_maps, core_ids=list(range(8)))
    out = np.empty((4, S, D), np.float32)
    for core in range(8):
        b, p = core // 2, core % 2
        y = np.asarray(res.results[core]["y"]).reshape(4, SL, D)
        for m in range(4):
            out[b, (2 * m + p) * SL:(2 * m + p + 1) * SL] = y[m]
    return out
```

```python
import math
from contextlib import ExitStack

import numpy as np
import concourse.bass as bass
import concourse.mybir as mybir
from concourse.bass_utils import run_bass_kernel_spmd

F32 = mybir.dt.float32
BF16 = mybir.dt.bfloat16
AF = mybir.ActivationFunctionType
ALU = mybir.AluOpType
AX = mybir.AxisListType

S = 4096
D = 1024
DFF = 2816
NFC = 22
SL = 512
BIG = 30000.0
EPS = 1e-6
NSTEP = 14
NDS = 20
DEBUG = False
import os
KSTOP = float(os.environ.get('KSTOP', '1000'))


class StopBuild(Exception):
    pass


def chk(n):
    if KSTOP <= n:
        raise StopBuild()


class Buf:
    __slots__ = ("w", "rs")

    def __init__(self):
        self.w = None
        self.rs = {}


class Tr:
    def __init__(self, nc, st):
        self.nc = nc
        self.E = {}
        for name, h in [("pe", nc.tensor), ("act", nc.scalar), ("dve", nc.vector),
                        ("pool", nc.gpsimd), ("sp", nc.sync)]:
            sem = st.enter_context(nc.semaphore("s_" + name))
            self.E[name] = dict(h=h, sem=sem, cnt=0, waited={})
        self.ds = {}
        for q, nq in (("pool", 4), ("sp", NDS)):
            sems = [st.enter_context(nc.semaphore("d%s%d" % (q, i))) for i in range(nq)]
            self.ds[q] = dict(sems=sems, val=[0] * nq, nxt=0, n=nq)

    def wait(self, eng, ev):
        if ev is None:
            return
        sem, val, key = ev
        e = self.E[eng]
        if key == "pe" and eng == "pe":
            return
        if key in self.E:
            assert self.E[key]["cnt"] >= val, "wait on unmarked event %s" % key
        if e["waited"].get(key, 0) >= val:
            return
        e["h"].wait_ge(sem, val)
        e["waited"][key] = val

    def _deps(self, eng, reads, writes):
        for b in reads:
            self.wait(eng, b.w)
        for b in writes:
            self.wait(eng, b.w)
            for k, r in b.rs.items():
                self.wait(eng, r)

    def _record(self, ev, reads, writes):
        for b in writes:
            b.w = ev
            b.rs = {}
        for b in reads:
            b.rs[ev[2]] = ev

    def op(self, eng, fn, reads=(), writes=(), mark=True):
        self._deps(eng, reads, writes)
        e = self.E[eng]
        inst = fn(e["h"])
        if mark:
            inst.then_inc(e["sem"], 1)
            e["cnt"] += 1
            ev = (e["sem"], e["cnt"], eng)
        else:
            ev = (e["sem"], e["cnt"] + 1, eng)
        self._record(ev, reads, writes)
        return ev

    def dma(self, q, out, in_, reads=(), writes=()):
        self._deps(q, reads, writes)
        d = self.ds[q]
        i = d["nxt"]
        d["nxt"] = (i + 1) % d["n"]
        key = ("d", q, i)
        if d["val"][i] > 0:
            self.wait(q, (d["sems"][i], d["val"][i], key))
        self.E[q]["h"].dma_start(out=out, in_=in_).then_inc(d["sems"][i], 16)
        d["val"][i] += 16
        ev = (d["sems"][i], d["val"][i], key)
        self._record(ev, reads, writes)
        return ev

    def barrier(self):
        evs = []
        for k, e in self.E.items():
            if e["cnt"] > 0:
                evs.append((e["sem"], e["cnt"], k))
        for q, d in self.ds.items():
            for i in range(d["n"]):
                if d["val"][i] > 0:
                    evs.append((d["sems"][i], d["val"][i], ("d", q, i)))
        for k in self.E:
            for ev in evs:
                if ev[2] == k:
                    continue
                self.wait(k, ev)


def build_program(debug=False):
    nc = bass.Bass("TRN2", target_bir_lowering=False)

    def din(name, shape, dt=F32):
        return nc.dram_tensor(name, shape, dt, kind="ExternalInput").ap()

    x_d = din("x", [S, D])
    w1gu_d = din("w1gu", [D, 2 * DFF])
    w1d_d = din("w1d", [DFF, D])
    win_d = din("win", [D, 5444])
    wba_d = din("wba", [512, D])
    wbb_d = din("wbb", [512, D])
    wo_d = din("wo", [D, D])
    w2gu_d = din("w2gu", [D, 2 * DFF])
    w2d_d = din("w2d", [DFF, D])
    gains_d = din("gains", [128, 24])
    bg_d = din("bg", [128, 16])
    gqk_d = din("gqk", [128, 4])
    dtab_d = din("dtab", [128, 16, 384])
    cb_d = din("cb", [128, 16])
    cmask_d = din("cmask", [128, 384])
    mtab_d = din("mtab", [128, 896])
    pbv_d = din("pbv", [128, 1])
    idab_d = din("idab", [128, 3, 128])
    esel_d = din("esel", [128, 16, 128])
    gmask_d = din("gmask", [128, 2, 8, 4])
    ownhot_d = din("ownhot", [128, 2, 8, 4])
    c32_d = din("c32", [128, 3, 128])
    pow2_d = din("pow2", [128, NSTEP])
    y_d = nc.dram_tensor("y", [4 * SL, D], F32, kind="ExternalOutput").ap()
    Kc_d = nc.dram_tensor("Kc", [8, 128, S], BF16, kind="Internal").ap()
    Vc_d = nc.dram_tensor("Vc", [16, 128, 32, 66], BF16, kind="Internal").ap()
    kic_d = nc.dram_tensor("kic", [64, S], BF16, kind="Internal").ap()
    wb_shapes = {"w1gu": (D, 2 * DFF), "w1d": (DFF, D), "win": (D, 5444), "wba": (512, D), "wbb": (512, D),
                 "wo": (D, D), "w2gu": (D, 2 * DFF), "w2d": (DFF, D)}
    wsrc = {"w1gu": w1gu_d, "w1d": w1d_d, "win": win_d, "wba": wba_d, "wbb": wbb_d, "wo": wo_d, "w2gu": w2gu_d, "w2d": w2d_d}
    wbf = {k: nc.dram_tensor(k + "_bf", list(v), BF16, kind="Internal").ap() for k, v in wb_shapes.items()}

    with ExitStack() as st:
        T = Tr(nc, st)

        def sb(name, shape, dt):
            return st.enter_context(nc.sbuf_tensor("sb_" + name, shape, dt))

        def ps(name, shape, dt):
            return st.enter_context(nc.psum_tensor("ps_" + name, shape, dt))

        R1 = sb("R1", [128, 57344], mybir.dt.uint8)

        def r1view(off, shape, dt):
            n = int(np.prod(shape[1:]))
            esz = 2 if dt == BF16 else 4
            v = R1[:, off:off + n * esz].bitcast(dt)
            if len(shape) == 3:
                v = v.rearrange("p (a b) -> p a b", a=shape[1])
            return v
        actT = r1view(0, [128, NFC, SL], BF16)
        xtok = r1view(22528, [128, 4, D], F32)
        wdb = [r1view(38912, [128, NFC, 128], BF16), r1view(44544, [128, NFC, 128], BF16)]
        sqb = [r1view(50176, [128, SL], F32), r1view(52224, [128, SL], F32)]
        maskT = r1view(0, [128, 32, SL], BF16)
        Isc = r1view(32768, [128, S], F32)
        msk = r1view(49152, [128, S], BF16)
        B_R1 = Buf()

        xT = sb("xT", [128, 8, SL], F32)
        xnT = sb("xnT", [128, 8, SL], BF16)
        wgub = [sb("wgu%d" % i, [128, 8, 2, 128], BF16) for i in range(3)]
        winb = [sb("winb%d" % i, [128, 8, 128], BF16) for i in range(3)]
        wvb = sb("wvb", [128, 8, 256], BF16)
        wkib = sb("wkib", [128, 8, 64], BF16)
        wwib = sb("wwib", [128, 8, 4], BF16)
        scr = [sb("scr%d" % i, [128, SL], F32) for i in range(4)]
        rstd = sb("rstd", [128, SL], F32)
        rstd2 = sb("rstd2", [128, SL], F32)
        epst = sb("epst", [128, 1], F32)
        qaT = sb("qaT", [128, 4, SL], BF16)
        qbT = sb("qbT", [128, 4, SL], BF16)
        qiT = sb("qiT", [128, 2, SL], BF16)
        wS = sb("wS", [128, 4, 4], F32)
        kiT2 = sb("kiT2", [128, S], BF16)
        kbuf = sb("kbuf", [128, S], BF16)
        vbuf = sb("vbuf", [128, 32, 66], BF16)
        pbuf = [sb("pbuf%d" % i, [128, SL], BF16) for i in range(3)]
        ebuf = [sb("ebuf%d" % i, [128, SL], BF16) for i in range(2)]
        AT = sb("AT", [128, 8, SL], BF16)
        mixT = sb("mixT", [128, 8, SL], BF16)
        Dt = sb("Dt", [128, 16, 384], BF16)
        esel = sb("esel", [128, 16, 128], BF16)
        idab = sb("idab", [128, 3, 128], BF16)
        c32 = sb("c32", [128, 3, 128], F32)
        gains = sb("gains", [128, 24], F32)
        bg = sb("bg", [128, 16], F32)
        gqk = sb("gqk", [128, 4], F32)
        gq8 = sb("gq8", [128, 2], F32)
        cb = sb("cb", [128, 32], F32)
        mtab = sb("mtab", [128, 896], BF16)
        pbv = sb("pbv", [128, 1], F32)
        gmask = sb("gmask", [128, 2, 8, 4], F32)
        ownhot = sb("ownhot", [128, 2, 8, 4], F32)
        pow2 = sb("pow2", [128, NSTEP], F32)
        kst = [sb("kst%d" % i, [128, SL], BF16) for i in range(2)]
        vst = sb("vst", [128, 4, 8, 66], BF16)
        kist = sb("kist", [64, SL], BF16)
        kms = sb("kms", [128, 4, 16], F32)
        kmb = sb("kmb", [128, 4, 16], BF16)
        G = sb("G", [128, 8, 16], F32)
        sel = sb("sel", [128, 8, 16], F32)
        m8 = sb("m8", [128, 8, 8], F32)
        thr = sb("thr", [128, 8], F32)
        negm = sb("negm", [128, 8, 32], BF16)
        negmT = sb("negmT", [128, 8, SL], BF16)
        small = sb("small", [128, 8], F32)
        steps = sb("steps", [128, NSTEP], F32)
        rd = rstd
        dtmp = scr[0][:, 0:384]
        cmask = scr[1][:, 0:384]

        ident_b = idab[:, 0, :]
        identA = idab[:, 1, :]
        identB = idab[:, 2, :]
        ident32 = c32[:, 0, :]
        ones32 = c32[:, 1, :]
        bones32 = c32[:, 2, :]

        pbank = [ps("pb%d" % i, [128, SL], F32) for i in range(4)]
        B_pbank = [Buf() for _ in range(4)]
        pobank = [ps("po%d" % i, [128, SL], F32) for i in range(2)]
        B_po = [Buf(), Buf()]
        pmisc = ps("pmisc", [128, SL], F32)
        B_misc = Buf()
        psb = ps("psb", [128, 8, 128], BF16)
        B_psb = Buf()
        pctr = [0]

        def nextbank():
            i = pctr[0] % 4
            pctr[0] += 1
            return pbank[i], B_pbank[i]

        B_xtok = Buf()
        B_xT = [Buf() for _ in range(8)]
        B_xnT = [Buf() for _ in range(8)]
        B_actT = [Buf() for _ in range(NFC)]
        B_wgu = [[Buf(), Buf()] for _ in range(3)]
        B_wd = [Buf(), Buf()]
        B_win = [Buf() for _ in range(3)]
        B_wv, B_wki, B_wwi = Buf(), Buf(), Buf()
        B_sq = [Buf(), Buf()]
        B_scr = [Buf() for _ in range(4)]
        B_rstd, B_rstd2 = Buf(), Buf()
        B_const = Buf()
        B_qaT = [Buf() for _ in range(4)]
        B_qbT = [Buf() for _ in range(4)]
        B_qiT = [Buf(), Buf()]
        B_wS = Buf()
        B_kiT2, B_kbuf, B_vbuf = Buf(), Buf(), Buf()
        B_pbuf = [Buf() for _ in range(3)]
        B_ebuf = [Buf(), Buf()]
        B_AT = [Buf() for _ in range(8)]
        B_mixT = [Buf() for _ in range(8)]
        B_kst = [Buf(), Buf()]
        B_vst, B_kist = Buf(), Buf()
        B_km = Buf()
        B_G, B_sel, B_m8, B_thr, B_negm, B_negmT = Buf(), Buf(), Buf(), Buf(), Buf(), Buf()
        B_small, B_steps, B_rd = Buf(), Buf(), Buf()
        B_Isc, B_msk = Buf(), Buf()
        B_maskT = [Buf() for _ in range(4)]
        B_Kc = [[Buf() for _ in range(8)] for _ in range(8)]
        B_Vc = [[[Buf() for _ in range(4)] for _ in range(8)] for _ in range(2)]
        B_kic = [Buf() for _ in range(8)]
        B_y = Buf()
        sctr = [0]

        def nextscr():
            i = sctr[0] % 4
            sctr[0] += 1
            return scr[i], B_scr[i]

        stg = [r1view(i * 11264, [128, 5632], BF16) for i in range(4)]
        B_stg = [Buf() for _ in range(4)]
        B_wbf = Buf()
        si = 0
        for k in ("w1gu", "w1d", "win", "wba", "wbb", "wo", "w2gu", "w2d"):
            R_, C_ = wb_shapes[k]
            for rb in range(R_ // 128):
                i = si % 4
                si += 1
                T.dma("pool", stg[i][:, 0:C_], wsrc[k][rb * 128:(rb + 1) * 128, :], writes=[B_stg[i]])
                T.dma("sp", wbf[k][rb * 128:(rb + 1) * 128, :], stg[i][:, 0:C_], reads=[B_stg[i]], writes=[B_wbf])
        T.dma("pool", idab[:], idab_d, writes=[B_const])
        T.dma("pool", esel[:], esel_d, writes=[B_const])
        T.dma("pool", mtab[:], mtab_d, writes=[B_const])
        for dst, src in [(c32, c32_d), (gains, gains_d), (bg, bg_d), (gqk, gqk_d), (pbv, pbv_d),
                         (gmask, gmask_d), (ownhot, ownhot_d), (pow2, pow2_d)]:
            T.dma("sp", dst[:], src, writes=[B_const])
        T.dma("sp", cmask, cmask_d, writes=[B_scr[1]])
        T.dma("sp", cb[:, 0:16], cb_d, writes=[B_const])
        T.op("dve", lambda e: e.memset(epst[:], EPS), writes=[B_const])
        T.op("dve", lambda e: e.memset(vst[:], 1.0), writes=[B_vst])
        T.op("dve", lambda e: e.memset(negm[:], 0.0), writes=[B_negm])
        T.op("dve", lambda e: e.memset(negmT[:], 0.0), writes=[B_negmT])
        T.op("dve", lambda e: e.tensor_scalar(out=gq8[:], in0=gqk[:, 0:2], scalar1=0.125, scalar2=None, op0=ALU.mult),
             reads=[B_const], writes=[B_const])
        T.op("dve", lambda e: e.tensor_scalar(out=cb[:, 16:32], in0=cb[:, 0:16], scalar1=pbv[:, 0:1], scalar2=None, op0=ALU.add),
             reads=[B_const], writes=[B_const])
        for h in range(16):
            T.dma("sp", dtmp, dtab_d[:, h, :], writes=[B_scr[0]])
            T.op("dve", lambda e, h=h: e.scalar_tensor_tensor(out=Dt[:, h, :], in0=dtmp, scalar=cb[:, h:h + 1], in1=cmask,
                                                             op0=ALU.subtract, op1=ALU.add),
                 reads=[B_scr[0], B_scr[1], B_const], writes=[B_const])

        def mmgroup(out_ap, obuf, items, extra_reads=()):
            n = len(items)
            for i, (l, r, rb) in enumerate(items):
                T.op("pe", lambda e, l=l, r=r, i=i: e.matmul(out_ap, lhsT=l, rhs=r, start=(i == 0), stop=(i == n - 1)),
                     reads=list(rb) + list(extra_reads), writes=[obuf], mark=(i == n - 1))

        win_v = wbf["win"].rearrange("(kc p) n -> p kc n", p=128)

        def rmsnorm(gcol0):
            for kc in range(8):
                sq, bsq = sqb[kc % 2], B_sq[kc % 2]
                T.op("act", lambda e, kc=kc, sq=sq: e.activation(out=sq, in_=xT[:, kc, :], func=AF.Square),
                     reads=[B_xT[kc]], writes=[bsq])
                T.op("pe", lambda e, kc=kc, sq=sq: e.matmul(pmisc[:], lhsT=ones32, rhs=sq, start=(kc == 0), stop=(kc == 7)),
                     reads=[bsq, B_const], writes=[B_misc], mark=True)
            T.op("act", lambda e: e.activation(out=rstd[:], in_=pmisc[:], func=AF.Sqrt, bias=epst[:, 0:1], scale=1.0 / D),
                 reads=[B_misc, B_const], writes=[B_rstd])
            T.op("dve", lambda e: e.reciprocal(out=rstd2[:], in_=rstd[:]), reads=[B_rstd], writes=[B_rstd2])
            for kc in range(8):
                T.op("dve", lambda e, kc=kc: e.scalar_tensor_tensor(out=xnT[:, kc, :], in0=xT[:, kc, :],
                                                                   scalar=gains[:, gcol0 + kc:gcol0 + kc + 1], in1=rstd2[:],
                                                                   op0=ALU.mult, op1=ALU.mult),
                     reads=[B_xT[kc], B_rstd2, B_const], writes=[B_xnT[kc]])

        def ffn(wgu_d, wd_d):
            wgu_v = wgu_d.rearrange("(kc p) (two f) -> p kc two f", p=128, two=2)
            wd_v = wd_d.rearrange("(c p) n -> p c n", p=128)

            def load_gu(c):
                T.dma("sp", wgub[c % 3][:, :, 0, :], wgu_v[:, :, 0, c * 128:(c + 1) * 128], writes=[B_wgu[c % 3][0]])
                T.dma("sp", wgub[c % 3][:, :, 1, :], wgu_v[:, :, 1, c * 128:(c + 1) * 128], writes=[B_wgu[c % 3][1]])

            def load_d(cc):
                T.dma("sp", wdb[cc % 2], wd_v[:, :, cc * 128:(cc + 1) * 128], writes=[B_wd[cc % 2]])
            load_gu(0)
            load_gu(1)
            for c in range(NFC):
                if c + 2 < NFC:
                    load_gu(c + 2)
                w = wgub[c % 3]
                gb_, bgb = nextbank()
                ub_, bub = nextbank()
                mmgroup(gb_[:], bgb, [(w[:, kc, 0, :], xnT[:, kc, :], [B_wgu[c % 3][0], B_xnT[kc]]) for kc in range(8)])
                mmgroup(ub_[:], bub, [(w[:, kc, 1, :], xnT[:, kc, :], [B_wgu[c % 3][1], B_xnT[kc]]) for kc in range(8)])
                sg, bsg = nextscr()
                T.op("act", lambda e, sg=sg, gb_=gb_: e.activation(out=sg[:], in_=gb_[:], func=AF.Silu), reads=[bgb], writes=[bsg])
                T.op("dve", lambda e, sg=sg, ub_=ub_, c=c: e.tensor_tensor(out=actT[:, c, :], in0=ub_[:], in1=sg[:], op=ALU.mult),
                     reads=[bub, bsg], writes=[B_actT[c]])
                if c == NFC - 3:
                    load_d(0)
                if c == NFC - 2:
                    load_d(1)
            for cc in range(8):
                w = wdb[cc % 2]
                bk, bbk = nextbank()
                mmgroup(bk[:], bbk, [(w[:, c, :], actT[:, c, :], [B_wd[cc % 2], B_actT[c]]) for c in range(NFC)])
                T.op("dve", lambda e, bk=bk, cc=cc: e.scalar_tensor_tensor(out=xT[:, cc, :], in0=bk[:], scalar=0.5, in1=xT[:, cc, :],
                                                                          op0=ALU.mult, op1=ALU.add),
                     reads=[bbk, B_xT[cc]], writes=[B_xT[cc]])
                if cc + 2 < 8:
                    load_d(cc + 2)

        wctr = [0]

        def load_win(col0):
            i = wctr[0] % 3
            wctr[0] += 1
            T.dma("sp", winb[i][:], win_v[:, :, col0:col0 + 128], writes=[B_win[i]])
            return winb[i], B_win[i]

        def proj_pair_normed(col0, gain_ap, out_ap, obufs):
            w, bw = load_win(col0)
            bk, bbk = nextbank()
            mmgroup(bk[:], bbk, [(w[:, kc, :], xnT[:, kc, :], [bw, B_xnT[kc]]) for kc in range(8)])
            raw, braw = nextscr()
            sq, bsq = nextscr()
            T.op("act", lambda e: e.activation(out=raw[:], in_=bk[:], func=AF.Copy), reads=[bbk], writes=[braw])
            T.op("act", lambda e: e.activation(out=sq[:], in_=bk[:], func=AF.Square), reads=[bbk], writes=[bsq])
            T.op("pe", lambda e: e.matmul(pmisc[:], lhsT=bones32, rhs=sq[:], start=True, stop=True),
                 reads=[bsq, B_const], writes=[B_misc])
            T.op("act", lambda e: e.activation(out=rstd[:], in_=pmisc[:], func=AF.Sqrt, bias=epst[:, 0:1], scale=1.0 / 64),
                 reads=[B_misc, B_const], writes=[B_rstd])
            T.op("dve", lambda e: e.reciprocal(out=rstd2[:], in_=rstd[:]), reads=[B_rstd], writes=[B_rstd2])
            T.op("dve", lambda e: e.scalar_tensor_tensor(out=out_ap, in0=raw[:], scalar=gain_ap, in1=rstd2[:], op0=ALU.mult, op1=ALU.mult),
                 reads=[braw, B_rstd2, B_const], writes=obufs)

        def phase1(slot, own, m):
            T.dma("sp", xtok, x_d[slot * SL:(slot + 1) * SL, :].rearrange("(tt p) f -> p tt f", p=128), writes=[B_xtok])
            for kc in range(8):
                bk, bbk = nextbank()
                for tt in range(4):
                    T.op("pe", lambda e, bk=bk, tt=tt, kc=kc: e.transpose(out=bk[:, tt * 128:(tt + 1) * 128],
                                                                         in_=xtok[:, tt, kc * 128:(kc + 1) * 128], identity=ident32),
                         reads=[B_xtok, B_const], writes=[bbk], mark=(tt == 3))
                if kc % 2 == 0:
                    T.op("act", lambda e, bk=bk, kc=kc: e.activation(out=xT[:, kc, :], in_=bk[:], func=AF.Copy), reads=[bbk], writes=[B_xT[kc]])
                else:
                    T.op("dve", lambda e, bk=bk, kc=kc: e.tensor_copy(out=xT[:, kc, :], in_=bk[:]), reads=[bbk], writes=[B_xT[kc]])
            chk(1)
            rmsnorm(0)
            chk(2)
            ffn(wbf["w1gu"], wbf["w1d"])
            chk(3)
            rmsnorm(8)
            for grp, col_base, gcol in ((0, 512, 2), (1, 2048, 3)):
                for pr in range(4):
                    ks, bks = kst[pr % 2], B_kst[pr % 2]
                    proj_pair_normed(col_base + pr * 128, gqk[:, gcol:gcol + 1], ks[:], [bks])
                    if grp == 1:
                        T.op("dve", lambda e, ks=ks, pr=pr: e.tensor_reduce(out=kms[:, pr, 2 * slot:2 * slot + 2],
                                                                           in_=ks[:].rearrange("p (a b) -> p a b", a=2),
                                                                           axis=AX.X, op=ALU.add),
                             reads=[bks], writes=[B_km])
                        T.op("dve", lambda e, pr=pr: e.tensor_scalar(out=kmb[:, pr, 2 * slot:2 * slot + 2], in0=kms[:, pr, 2 * slot:2 * slot + 2],
                                                                    scalar1=1.0 / 256, scalar2=None, op0=ALU.mult),
                             reads=[B_km], writes=[B_km])
                    T.dma("sp", Kc_d[grp * 4 + pr, :, slot * SL:(slot + 1) * SL], ks[:], reads=[bks], writes=[B_Kc[grp * 4 + pr][slot]])
            for grp, col_base in ((0, 1024), (1, 2560)):
                for half in range(2):
                    T.dma("sp", wvb[:], win_v[:, :, col_base + half * 256:col_base + (half + 1) * 256], writes=[B_wv])
                    for tt in range(4):
                        bk, bbk = nextbank()
                        mmgroup(bk[:, 0:256], bbk, [(xnT[:, kc, tt * 128:(tt + 1) * 128], wvb[:, kc, :], [B_wv, B_xnT[kc]]) for kc in range(8)])
                        T.op("act", lambda e, bk=bk, tt=tt, half=half: e.activation(
                            out=vst[:, tt, half * 4:(half + 1) * 4, 0:64],
                            in_=bk[:, 0:256].rearrange("p (h d) -> p h d", h=4), func=AF.Copy),
                            reads=[bbk], writes=[B_vst])
                for tt in range(4):
                    T.dma("sp", Vc_d[grp * 8:(grp + 1) * 8].rearrange("h p t d -> p t h d")[:, slot * 4 + tt, :, :], vst[:, tt, :, :],
                          reads=[B_vst], writes=[B_Vc[grp][slot][tt]])
            T.dma("sp", wkib[:], win_v[:, :, 3328:3392], writes=[B_wki])
            bk, bbk = nextbank()
            mmgroup(bk[0:64, :], bbk, [(wkib[:, kc, :], xnT[:, kc, :], [B_wki, B_xnT[kc]]) for kc in range(8)])
            T.op("act", lambda e: e.activation(out=kist[:], in_=bk[0:64, :], func=AF.Copy), reads=[bbk], writes=[B_kist])
            T.dma("sp", kic_d[:, slot * SL:(slot + 1) * SL], kist[:], reads=[B_kist], writes=[B_kic[slot]])
            if not own:
                return
            for pr in range(4):
                proj_pair_normed(pr * 128, gq8[:, 0:1], qaT[:, pr, :], [B_qaT[pr]])
            for pr in range(4):
                proj_pair_normed(1536 + pr * 128, gq8[:, 1:2], qbT[:, pr, :], [B_qbT[pr]])
            for pr in range(2):
                w, bw = load_win(3072 + pr * 128)
                bk, bbk = nextbank()
                mmgroup(bk[:], bbk, [(w[:, kc, :], xnT[:, kc, :], [bw, B_xnT[kc]]) for kc in range(8)])
                T.op("act", lambda e, bk=bk, pr=pr: e.activation(out=qiT[:, pr, :], in_=bk[:], func=AF.Copy), reads=[bbk], writes=[B_qiT[pr]])
            T.dma("sp", wwib[:], win_v[:, :, 3392:3396], writes=[B_wwi])
            for tt in range(4):
                bk, bbk = nextbank()
                mmgroup(bk[:, 0:4], bbk, [(xnT[:, kc, tt * 128:(tt + 1) * 128], wwib[:, kc, :], [B_wwi, B_xnT[kc]]) for kc in range(8)])
                T.op("dve", lambda e, bk=bk, tt=tt: e.tensor_scalar(out=wS[:, tt, :], in0=bk[:, 0:4], scalar1=0.0625, scalar2=None, op0=ALU.mult),
                     reads=[bbk], writes=[B_wS])

        def attention(m):
            nsl = 2 * m + 2
            nk = nsl * SL
            nkt = nsl * 4
            nb = nsl * 2
            k0 = 1024 * m
            for hf in range(2):
                T.dma("sp", kiT2[hf * 64:(hf + 1) * 64, 0:nk], kic_d[:, 0:nk], reads=B_kic[0:nsl], writes=[B_kiT2])
            def gating(r):
                chk(7 if m == 0 else (12.32 if r == 0 else 12.39))
                half = r // 2
                T.op("dve", lambda e: e.memset(G[:], -BIG), writes=[B_G])
                gbo, bgbo = nextbank()
                for par, (gbk, bgbk) in enumerate(((pmisc, B_misc), (gbo, bgbo))):
                    p0 = 64 * par
                    for pr_ in range(4):
                        h = 2 * pr_ + par
                        T.op("pe", lambda e, h=h, p0=p0, pr_=pr_, gbk=gbk: e.matmul(gbk[:, h * 16:h * 16 + nb], lhsT=qbT[p0:p0 + 64, pr_, r * 128:(r + 1) * 128],
                                                                                  rhs=kmb[p0:p0 + 64, pr_, 0:nb], start=True, stop=True),
                             reads=[B_qbT[pr_], B_km], writes=[bgbk], mark=(pr_ == 3))
                    T.op("dve", lambda e, par=par, gbk=gbk: e.tensor_copy(
                        out=G[:].rearrange("p (hp two) b -> p hp two b", two=2)[:, :, par, 0:nb],
                        in_=gbk[:, 0:128].rearrange("p (hp two b) -> p hp two b", two=2, b=16)[:, :, par, 0:nb]),
                        reads=[bgbk], writes=[B_G])
                T.op("dve", lambda e: e.tensor_tensor(out=G[:, :, 4 * m:4 * m + 4], in0=G[:, :, 4 * m:4 * m + 4], in1=gmask[:, half, :, :], op=ALU.add),
                     reads=[B_G, B_const], writes=[B_G])
                for h in range(8):
                    T.op("dve", lambda e, h=h: e.max(out=m8[:, h, :], in_=G[:, h, :]), reads=[B_G], writes=[B_m8])
                T.op("dve", lambda e: e.tensor_scalar(out=thr[:], in0=m8[:, :, 2], scalar1=-BIG / 2, scalar2=None, op0=ALU.max),
                     reads=[B_m8], writes=[B_thr])
                for h in range(8):
                    T.op("dve", lambda e, h=h: e.tensor_scalar(out=sel[:, h, :], in0=G[:, h, :], scalar1=thr[:, h:h + 1], scalar2=None, op0=ALU.is_ge),
                         reads=[B_G, B_thr], writes=[B_sel])
                T.op("dve", lambda e: e.tensor_tensor(out=sel[:, :, 4 * m:4 * m + 4], in0=sel[:, :, 4 * m:4 * m + 4], in1=ownhot[:, half, :, :], op=ALU.add),
                     reads=[B_sel, B_const], writes=[B_sel])
                T.op("dve", lambda e: e.tensor_scalar(out=negm[:, :, 0:16], in0=sel[:], scalar1=-1.0, scalar2=BIG, op0=ALU.add, op1=ALU.mult),
                     reads=[B_sel], writes=[B_negm])
                for h in range(8):
                    T.op("pe", lambda e, h=h: e.transpose(out=psb[0:32, h, :], in_=negm[:, h, :], identity=ident_b),
                         reads=[B_negm, B_const], writes=[B_psb], mark=(h == 7))
                T.op("act", lambda e: e.activation(out=negmT[0:32, :, r * 128:(r + 1) * 128], in_=psb[0:32, :, :], func=AF.Copy),
                     reads=[B_psb], writes=[B_negmT])
                if m == 1 and r == 0:
                    chk(12.33)


            for r_ in range(4):
                gating(r_)
            pbi = [0]

            def attend(hg):
                moba = hg >= 8
                h = hg % 8
                pr = h // 2
                p0 = 64 * (h % 2)
                grp = 1 if moba else 0
                if h % 2 == 0:
                    T.dma("sp", kbuf[:, 0:nk], Kc_d[grp * 4 + pr, :, 0:nk], reads=B_Kc[grp * 4 + pr][0:nsl], writes=[B_kbuf])
                T.dma("sp", vbuf[:, 0:nkt, :], Vc_d[hg, :, 0:nkt, :], reads=[b for sl_ in B_Vc[grp][0:nsl] for b in sl_], writes=[B_vbuf])
                qT = qbT if moba else qaT
                bq = (B_qbT if moba else B_qaT)[pr]
                po, bpo = pobank[hg % 2], B_po[hg % 2]
                g, jj = h // 3, h % 3
                def stageA(kt):
                    j = kt - 8 * m
                    q0 = 128 * j if 0 <= j < 4 else 0
                    sbk, bsbk = nextbank()
                    items = [(sbk[:, q0:SL], kbuf[p0:p0 + 64, kt * 128:(kt + 1) * 128], qT[p0:p0 + 64, pr, q0:SL], [B_kbuf, bq])]
                    if 0 <= j < 4:
                        W = min(384, SL - q0)
                        items.append((sbk[:, q0:q0 + W], ident_b, Dt[:, hg, 0:W], [B_const]))
                    if m >= 1 and kt == 8 * m - 1:
                        items.append((sbk[:, 0:256], identA, Dt[:, hg, 128:384], [B_const]))
                    if kt == 8 * m + 7:
                        items.append((sbk[:, 0:256], identB, Dt[:, hg, 128:384], [B_const]))
                    if moba:
                        items.append((sbk[:, q0:SL], esel[:, kt // 2, :], negmT[:, h, q0:SL], [B_const, B_negmT]))
                    n = len(items)
                    for i, (o, l, rr, rb) in enumerate(items):
                        T.op("pe", lambda e, o=o, l=l, rr=rr, i=i, n=n: e.matmul(o, lhsT=l, rhs=rr, start=(i == 0), stop=(i == n - 1)),
                             reads=rb, writes=[bsbk], mark=(i == n - 1))
                    pb_, bpb = pbuf[pbi[0] % 3], B_pbuf[pbi[0] % 3]
                    if moba:
                        eb_, beb = pb_, bpb
                    else:
                        eb_, beb = ebuf[pbi[0] % 2], B_ebuf[pbi[0] % 2]
                    pbi[0] += 1
                    col = hg + (16 if kt >= 8 * m + 4 else 0)
                    T.op("act", lambda e: e.activation(out=eb_[:, q0:SL], in_=sbk[:, q0:SL], func=AF.Exp, bias=cb[:, col:col + 1], scale=1.0),
                         reads=[bsbk, B_const], writes=[beb])
                    if not moba:
                        T.op("dve", lambda e: e.scalar_tensor_tensor(out=pb_[:, q0:SL], in0=eb_[:, q0:SL], scalar=1.0,
                                                                     in1=maskT[:, kt, q0:SL], op0=ALU.mult, op1=ALU.mult),
                             reads=[beb] + B_maskT, writes=[bpb])
                    return (kt, q0, pb_, bpb)

                def stageB(st_):
                    kt, q0, pb_, bpb = st_
                    T.op("pe", lambda e: e.matmul(po[0:65, q0:SL], lhsT=vbuf[:, kt, 0:65], rhs=pb_[:, q0:SL],
                                                  start=(kt == 0), stop=(kt == nkt - 1)),
                         reads=[B_vbuf, bpb], writes=[bpo], mark=True)

                prev_st = None
                for kt in range(nkt):
                    cur = stageA(kt)
                    if prev_st is not None:
                        stageB(prev_st)
                    prev_st = cur
                stageB(prev_st)
                chk(8.4 if hg == 0 else (8.7 if hg == 8 else 8.99))
                T.op("dve", lambda e, po=po: e.reciprocal(out=rd[64:65, :], in_=po[64:65, :]), reads=[bpo], writes=[B_rd])
                if hg == 0:
                    chk(8.41)
                T.op("pe", lambda e: e.matmul(pmisc[:], lhsT=ones32[64:65, :], rhs=rd[64:65, :], start=True, stop=True),
                     reads=[B_rd, B_const], writes=[B_misc])
                if hg == 0:
                    chk(8.42)
                tmpo, btmpo = nextscr()
                T.op("act", lambda e, po=po, tmpo=tmpo, p0=p0: e.activation(out=tmpo[p0:p0 + 64, :], in_=po[0:64, :], func=AF.Copy),
                     reads=[bpo], writes=[btmpo])
                ap_ = pr + (4 if moba else 0)
                if hg == 0:
                    chk(8.43)
                T.op("dve", lambda e, tmpo=tmpo, p0=p0, ap_=ap_: e.tensor_tensor(out=AT[p0:p0 + 64, ap_, :], in0=pmisc[p0:p0 + 64, :],
                                                                                 in1=tmpo[p0:p0 + 64, :], op=ALU.mult),
                     reads=[btmpo, B_misc], writes=[B_AT[ap_]])
                chk(8.5 if hg == 0 else (8.6 if hg == 7 else (8.8 if hg == 8 else 8.99)))


            def finish_mask(r):
                if m == 1 and r == 0:
                    chk(12.314)
                for g0 in range(0, nkt, 8):
                    if m == 1 and r == 0 and g0 == 8:
                        chk(12.315)
                    for i in range(8):
                        kt = g0 + i
                        T.op("pe", lambda e, i=i, kt=kt: e.transpose(out=psb[:, i, :], in_=msk[:, kt * 128:(kt + 1) * 128], identity=ident_b),
                             reads=[B_msk, B_const], writes=[B_psb], mark=(i == 7))
                    T.op("act", lambda e, g0=g0: e.activation(out=maskT[:, g0:g0 + 8, r * 128:(r + 1) * 128], in_=psb[:], func=AF.Copy),
                         reads=[B_psb], writes=[B_maskT[r]])

            for r in range(4):
                for j in range(nsl):
                    banks = [nextbank() for _ in range(4)]
                    for h in range(4):
                        p0 = 64 * (h % 2)
                        bk, bbk = banks[h]
                        mmgroup(bk[:], bbk, [(qiT[p0:p0 + 64, h // 2, r * 128:(r + 1) * 128], kiT2[p0:p0 + 64, j * SL:(j + 1) * SL],
                                              [B_qiT[h // 2], B_kiT2])])
                    for h in range(4):
                        bk, bbk = banks[h]
                        rl, brl = nextscr()
                        T.op("act", lambda e, bk=bk, rl=rl: e.activation(out=rl[:], in_=bk[:], func=AF.Relu), reads=[bbk], writes=[brl])
                        dst = Isc[:, j * SL:(j + 1) * SL]
                        if h == 0:
                            T.op("dve", lambda e, rl=rl, dst=dst: e.tensor_scalar(out=dst, in0=rl[:], scalar1=wS[:, r, 0:1], scalar2=None, op0=ALU.mult),
                                 reads=[brl, B_wS], writes=[B_Isc])
                        else:
                            T.op("dve", lambda e, rl=rl, dst=dst, h=h: e.scalar_tensor_tensor(out=dst, in0=rl[:], scalar=wS[:, r, h:h + 1], in1=dst,
                                                                                            op0=ALU.mult, op1=ALU.add),
                                 reads=[brl, B_wS, B_Isc], writes=[B_Isc])
                chk(6 if m == 0 else (12.31 if r == 0 else 12.39))
                Bv, lo, mid, cnt, tt_ = (small[:, i:i + 1] for i in range(5))
                T.op("dve", lambda e: e.tensor_reduce(out=Bv, in_=Isc[:, 0:nk], axis=AX.X, op=ALU.max, apply_absolute_value=True),
                     reads=[B_Isc], writes=[B_small])
                if m == 1 and r == 0:
                    chk(12.311)
                T.op("dve", lambda e: e.tensor_scalar(out=lo, in0=Bv, scalar1=1.0, scalar2=-1.0, op0=ALU.add, op1=ALU.mult),
                     reads=[B_small], writes=[B_small])
                T.op("dve", lambda e: e.tensor_scalar(out=steps[:], in0=pow2[:], scalar1=lo, scalar2=-2.0, op0=ALU.mult, op1=ALU.mult),
                     reads=[B_small, B_const], writes=[B_steps])
                T.op("dve", lambda e: e.tensor_tensor(out=Isc[:, k0:k0 + SL], in0=Isc[:, k0:k0 + SL],
                                                      in1=mtab[:, 384 - 128 * r:896 - 128 * r], op=ALU.add),
                     reads=[B_Isc, B_const], writes=[B_Isc])
                T.op("dve", lambda e: e.tensor_scalar(out=Isc[:, k0 + SL:k0 + 2 * SL], in0=Isc[:, k0 + SL:k0 + 2 * SL], scalar1=pbv[:, 0:1],
                                                      scalar2=None, op0=ALU.add),
                     reads=[B_Isc, B_const], writes=[B_Isc])
                if m == 1 and r == 0:
                    chk(12.312)
                for s_ in range(NSTEP):
                    if m == 1 and r == 0 and s_ == 1:
                        chk(12.313)
                    T.op("dve", lambda e, s_=s_: e.tensor_tensor(out=mid, in0=lo, in1=steps[:, s_:s_ + 1], op=ALU.add),
                         reads=[B_small, B_steps], writes=[B_small])
                    T.op("dve", lambda e: e.tensor_scalar(out=msk[:, 0:nk], in0=Isc[:, 0:nk], scalar1=mid, scalar2=0.0, op0=ALU.is_ge, op1=ALU.add,
                                                          accum_out=cnt),
                         reads=[B_Isc, B_small], writes=[B_msk, B_small])
                    T.op("dve", lambda e, s_=s_: e.tensor_scalar(out=tt_, in0=cnt, scalar1=256.0, scalar2=steps[:, s_:s_ + 1], op0=ALU.is_ge, op1=ALU.mult),
                         reads=[B_small, B_steps], writes=[B_small])
                    T.op("dve", lambda e: e.tensor_tensor(out=lo, in0=lo, in1=tt_, op=ALU.add), reads=[B_small], writes=[B_small])
                T.op("dve", lambda e: e.tensor_scalar(out=msk[:, 0:nk], in0=Isc[:, 0:nk], scalar1=lo, scalar2=None, op0=ALU.is_ge),
                     reads=[B_Isc, B_small], writes=[B_msk])
                for hg_ in (8 + 2 * r, 9 + 2 * r):
                    attend(hg_)
                finish_mask(r)
            for hg_ in range(8):
                attend(hg_)

        def mix(m):
            wba_v = wbf["wba"].rearrange("(kc p) n -> p kc n", p=128)
            wbb_v = wbf["wbb"].rearrange("(kc p) n -> p kc n", p=128)
            wo_v = wbf["wo"].rearrange("(kc p) n -> p kc n", p=128)
            for cc in range(8):
                wa, bwa = winb[wctr[0] % 3], B_win[wctr[0] % 3]
                wctr[0] += 1
                T.dma("sp", wa[:, 0:4, :], wba_v[:, :, cc * 128:(cc + 1) * 128], writes=[bwa])
                T.dma("sp", wa[:, 4:8, :], wbb_v[:, :, cc * 128:(cc + 1) * 128], writes=[bwa])
                wga, bwga = load_win(3396 + cc * 128)
                wgb, bwgb = load_win(3396 + 1024 + cc * 128)
                bA, bbA = nextbank()
                bB, bbB = nextbank()
                bGa, bbGa = nextbank()
                bGb, bbGb = nextbank()
                mmgroup(bA[:], bbA, [(wa[:, kc, :], AT[:, kc, :], [bwa, B_AT[kc]]) for kc in range(4)])
                mmgroup(bB[:], bbB, [(wa[:, 4 + kc, :], AT[:, 4 + kc, :], [bwa, B_AT[4 + kc]]) for kc in range(4)])
                mmgroup(bGa[:], bbGa, [(wga[:, kc, :], xnT[:, kc, :], [bwga, B_xnT[kc]]) for kc in range(8)])
                mmgroup(bGb[:], bbGb, [(wgb[:, kc, :], xnT[:, kc, :], [bwgb, B_xnT[kc]]) for kc in range(8)])
                ga, bga = nextscr()
                gb2, bgb2 = nextscr()
                T.op("act", lambda e, ga=ga, bGa=bGa, cc=cc: e.activation(out=ga[:], in_=bGa[:], func=AF.Sigmoid, bias=bg[:, cc:cc + 1], scale=1.0),
                     reads=[bbGa, B_const], writes=[bga])
                T.op("act", lambda e, gb2=gb2, bGb=bGb, cc=cc: e.activation(out=gb2[:], in_=bGb[:], func=AF.Sigmoid, bias=bg[:, 8 + cc:9 + cc], scale=1.0),
                     reads=[bbGb, B_const], writes=[bgb2])
                T.op("dve", lambda e, ga=ga, bA=bA: e.tensor_tensor(out=ga[:], in0=bA[:], in1=ga[:], op=ALU.mult), reads=[bbA, bga], writes=[bga])
                T.op("dve", lambda e, gb2=gb2, bB=bB: e.tensor_tensor(out=gb2[:], in0=bB[:], in1=gb2[:], op=ALU.mult), reads=[bbB, bgb2], writes=[bgb2])
                T.op("dve", lambda e, ga=ga, gb2=gb2, cc=cc: e.tensor_tensor(out=mixT[:, cc, :], in0=ga[:], in1=gb2[:], op=ALU.add),
                     reads=[bga, bgb2], writes=[B_mixT[cc]])
            for cc in range(8):
                w, bw = winb[wctr[0] % 3], B_win[wctr[0] % 3]
                wctr[0] += 1
                T.dma("sp", w[:], wo_v[:, :, cc * 128:(cc + 1) * 128], writes=[bw])
                bk, bbk = nextbank()
                mmgroup(bk[:], bbk, [(w[:, kc, :], mixT[:, kc, :], [bw, B_mixT[kc]]) for kc in range(8)])
                T.op("dve", lambda e, bk=bk, cc=cc: e.tensor_tensor(out=xT[:, cc, :], in0=bk[:], in1=xT[:, cc, :], op=ALU.add),
                     reads=[bbk, B_xT[cc]], writes=[B_xT[cc]])

        def output(m):
            for tt in range(4):
                for half in range(2):
                    bk, bbk = nextbank()
                    for i in range(4):
                        kc = half * 4 + i
                        T.op("pe", lambda e, bk=bk, i=i, kc=kc, tt=tt: e.transpose(out=bk[:, i * 128:(i + 1) * 128],
                                                                                  in_=xT[:, kc, tt * 128:(tt + 1) * 128], identity=ident32),
                             reads=[B_xT[kc], B_const], writes=[bbk], mark=(i == 3))
                    if half == 0:
                        T.op("act", lambda e, bk=bk, tt=tt: e.activation(out=xtok[:, tt, 0:512], in_=bk[:], func=AF.Copy), reads=[bbk], writes=[B_xtok])
                    else:
                        T.op("dve", lambda e, bk=bk, tt=tt: e.tensor_copy(out=xtok[:, tt, 512:1024], in_=bk[:]), reads=[bbk], writes=[B_xtok])
            T.dma("sp", y_d[m * SL:(m + 1) * SL, :].rearrange("(tt p) f -> p tt f", p=128), xtok, reads=[B_xtok], writes=[B_y])

        T.barrier()
        try:
            chk(0)
            for m in range(4):
                phase1(2 * m + 1, False, m)
                chk(4 if m == 0 else 12.2)
                phase1(2 * m, True, m)
                chk(5 if m == 0 else 12.3)
                T.barrier()
                attention(m)
                chk(9 if m == 0 else 12.5)
                T.barrier()
                mix(m)
                chk(10 if m == 0 else 12.6)
                rmsnorm(16)
                ffn(wbf["w2gu"], wbf["w2d"])
                chk(11)
                output(m)
                chk(12 + m)
        except StopBuild:
            pass
        T.barrier()
    return nc


def _t5_bucket(d):
    d = np.maximum(d, 0)
    df = np.maximum(d, 1).astype(np.float64)
    large = 16 + (np.log(df / 16.0) / math.log(128 / 16) * 16).astype(np.int64)
    large = np.minimum(large, 31)
    return np.where(d < 16, d, large)


def _consts(p, rel_bias):
    c = {}
    s = np.arange(128)[:, None]
    cc = np.arange(384)[None, :]
    bucket = _t5_bucket(cc - s)
    c["dtab"] = np.ascontiguousarray(np.transpose(rel_bias[bucket], (0, 2, 1))).astype(np.float32)
    c["cb"] = np.ascontiguousarray(np.broadcast_to(rel_bias[31][None, :], (128, 16))).astype(np.float32)
    c["cmask"] = np.where(cc >= s, 0.0, -BIG).astype(np.float32)
    c2 = np.arange(896)[None, :]
    c["mtab"] = np.where(c2 <= s + 384, 0.0, -BIG).astype(np.float32)
    pb = 0.0 if p == 1 else -BIG
    c["pbv"] = np.full((128, 1), pb, np.float32)
    eye = np.eye(128, dtype=np.float32)
    c["idab"] = np.ascontiguousarray(np.stack([eye, eye * (1.0 if p == 0 else 0.0), eye * (1.0 if p == 1 else 0.0)], axis=1))
    pp = np.arange(128)
    es = np.zeros((128, 16, 128), np.float32)
    for b in range(16):
        es[pp == b, b, :] = 1.0
    c["esel"] = es
    gm = np.zeros((2, 4), np.float32)
    oh = np.zeros((2, 4), np.float32)
    gm[0] = [-BIG, -BIG, pb, pb]
    oh[0] = [1, 0, 0, 0]
    gm[1] = [0.0, -BIG, pb, pb]
    oh[1] = [0, 1, 0, 0]
    c["gmask"] = np.ascontiguousarray(np.broadcast_to(gm[None, :, None, :], (128, 2, 8, 4))).astype(np.float32)
    c["ownhot"] = np.ascontiguousarray(np.broadcast_to(oh[None, :, None, :], (128, 2, 8, 4))).astype(np.float32)
    bo = np.zeros((128, 128), np.float32)
    bo[0:64, 0:64] = 1.0
    bo[64:128, 64:128] = 1.0
    c["c32"] = np.ascontiguousarray(np.stack([eye, np.ones((128, 128), np.float32), bo], axis=1))
    c["pow2"] = np.ascontiguousarray(np.broadcast_to((0.5 ** (np.arange(NSTEP) + 1))[None, :], (128, NSTEP))).astype(np.float32)
    return c


_NC_CACHE = {}


def make_in_maps(**inp):
    f = lambda k: np.asarray(inp[k], dtype=np.float32)
    x = f("x")
    fm = lambda v: np.ascontiguousarray(v.reshape(8, 128).T)
    gains = np.concatenate([fm(f("ffn1_norm")[0]), fm(f("mix_norm")[0]), fm(f("ffn2_norm")[0])], axis=1).astype(np.float32)
    bgv = np.ascontiguousarray(f("b_gate")[0].reshape(16, 128).T)
    gqk = np.stack([np.tile(f("q_norm_dsa")[0], 2), np.tile(f("q_norm_moba")[0], 2),
                    np.tile(f("k_norm_dsa")[0], 2), np.tile(f("k_norm_moba")[0], 2)], axis=1).astype(np.float32)
    rel_bias = f("rel_bias")
    shared = {
        "w1gu": np.ascontiguousarray(f("ffn1_w_gu")[0]), "w1d": np.ascontiguousarray(f("ffn1_w_down")[0]),
        "win": np.ascontiguousarray(f("w_in")[0]), "wba": np.ascontiguousarray(f("w_branch_dsa")[0]),
        "wbb": np.ascontiguousarray(f("w_branch_moba")[0]), "wo": np.ascontiguousarray(f("w_out")[0]),
        "w2gu": np.ascontiguousarray(f("ffn2_w_gu")[0]), "w2d": np.ascontiguousarray(f("ffn2_w_down")[0]),
        "gains": np.ascontiguousarray(gains), "bg": bgv, "gqk": np.ascontiguousarray(gqk),
    }
    cons = [_consts(0, rel_bias), _consts(1, rel_bias)]
    in_maps = []
    for core in range(8):
        b, p = core // 2, core % 2
        xb = x[b].reshape(8, SL, D)
        order = list(range(8)) if p == 0 else [1, 0, 3, 2, 5, 4, 7, 6]
        d = dict(shared)
        d.update(cons[p])
        d["x"] = np.ascontiguousarray(xb[order].reshape(S, D))
        in_maps.append(d)
    return in_maps


def kernel(**inp):
    in_maps = make_in_maps(**inp)
    if "nc" not in _NC_CACHE:
        _NC_CACHE["nc"] = build_program()
    res = run_bass_kernel_spmd(_NC_CACHE["nc"], in_maps, core_ids=list(range(8)))
    out = np.empty((4, S, D), np.float32)
    for core in range(8):
        b, p = core // 2, core % 2
        y = np.asarray(res.results[core]["y"]).reshape(4, SL, D)
        for m in range(4):
            out[b, (2 * m + p) * SL:(2 * m + p + 1) * SL] = y[m]
    return out
```

```python
import math
from contextlib import ExitStack

import numpy as np
import concourse.bass as bass
import concourse.mybir as mybir
from concourse.bass_utils import run_bass_kernel_spmd

F32 = mybir.dt.float32
BF16 = mybir.dt.bfloat16
AF = mybir.ActivationFunctionType
ALU = mybir.AluOpType
AX = mybir.AxisListType

S = 4096
D = 1024
DFF = 2816
NFC = 22
SL = 512
BIG = 30000.0
EPS = 1e-6
NSTEP = 14
NDS = 20
DEBUG = False
import os
KSTOP = float(os.environ.get('KSTOP', '1000'))


class StopBuild(Exception):
    pass


def chk(n):
    if KSTOP <= n:
        raise StopBuild()


class Buf:
    __slots__ = ("w", "rs")

    def __init__(self):
        self.w = None
        self.rs = {}


class Tr:
    def __init__(self, nc, st):
        self.nc = nc
        self.E = {}
        for name, h in [("pe", nc.tensor), ("act", nc.scalar), ("dve", nc.vector),
                        ("pool", nc.gpsimd), ("sp", nc.sync)]:
            sem = st.enter_context(nc.semaphore("s_" + name))
            self.E[name] = dict(h=h, sem=sem, cnt=0, waited={})
        self.ds = {}
        for q, nq in (("pool", 4), ("sp", NDS)):
            sems = [st.enter_context(nc.semaphore("d%s%d" % (q, i))) for i in range(nq)]
            self.ds[q] = dict(sems=sems, val=[0] * nq, nxt=0, n=nq)

    def wait(self, eng, ev):
        if ev is None:
            return
        sem, val, key = ev
        e = self.E[eng]
        if key == "pe" and eng == "pe":
            return
        if key in self.E:
            assert self.E[key]["cnt"] >= val, "wait on unmarked event %s" % key
        if e["waited"].get(key, 0) >= val:
            return
        e["h"].wait_ge(sem, val)
        e["waited"][key] = val

    def _deps(self, eng, reads, writes):
        for b in reads:
            self.wait(eng, b.w)
        for b in writes:
            self.wait(eng, b.w)
            for k, r in b.rs.items():
                self.wait(eng, r)

    def _record(self, ev, reads, writes):
        for b in writes:
            b.w = ev
            b.rs = {}
        for b in reads:
            b.rs[ev[2]] = ev

    def op(self, eng, fn, reads=(), writes=(), mark=True):
        self._deps(eng, reads, writes)
        e = self.E[eng]
        inst = fn(e["h"])
        if mark:
            inst.then_inc(e["sem"], 1)
            e["cnt"] += 1
            ev = (e["sem"], e["cnt"], eng)
        else:
            ev = (e["sem"], e["cnt"] + 1, eng)
        self._record(ev, reads, writes)
        return ev

    def dma(self, q, out, in_, reads=(), writes=()):
        self._deps(q, reads, writes)
        d = self.ds[q]
        i = d["nxt"]
        d["nxt"] = (i + 1) % d["n"]
        key = ("d", q, i)
        if d["val"][i] > 0:
            self.wait(q, (d["sems"][i], d["val"][i], key))
        self.E[q]["h"].dma_start(out=out, in_=in_).then_inc(d["sems"][i], 16)
        d["val"][i] += 16
        ev = (d["sems"][i], d["val"][i], key)
        self._record(ev, reads, writes)
        return ev

    def barrier(self):
        evs = []
        for k, e in self.E.items():
            if e["cnt"] > 0:
                evs.append((e["sem"], e["cnt"], k))
        for q, d in self.ds.items():
            for i in range(d["n"]):
                if d["val"][i] > 0:
                    evs.append((d["sems"][i], d["val"][i], ("d", q, i)))
        for k in self.E:
            for ev in evs:
                if ev[2] == k:
                    continue
                self.wait(k, ev)


def build_program(debug=False):
    nc = bass.Bass("TRN2", target_bir_lowering=False)

    def din(name, shape, dt=F32):
        return nc.dram_tensor(name, shape, dt, kind="ExternalInput").ap()

    x_d = din("x", [S, D])
    w1gu_d = din("w1gu", [D, 2 * DFF])
    w1d_d = din("w1d", [DFF, D])
    win_d = din("win", [D, 5444])
    wba_d = din("wba", [512, D])
    wbb_d = din("wbb", [512, D])
    wo_d = din("wo", [D, D])
    w2gu_d = din("w2gu", [D, 2 * DFF])
    w2d_d = din("w2d", [DFF, D])
    gains_d = din("gains", [128, 24])
    bg_d = din("bg", [128, 16])
    gqk_d = din("gqk", [128, 4])
    dtab_d = din("dtab", [128, 16, 384])
    cb_d = din("cb", [128, 16])
    cmask_d = din("cmask", [128, 384])
    mtab_d = din("mtab", [128, 896])
    pbv_d = din("pbv", [128, 1])
    idab_d = din("idab", [128, 3, 128])
    esel_d = din("esel", [128, 16, 128])
    gmask_d = din("gmask", [128, 2, 8, 4])
    ownhot_d = din("ownhot", [128, 2, 8, 4])
    c32_d = din("c32", [128, 3, 128])
    pow2_d = din("pow2", [128, NSTEP])
    y_d = nc.dram_tensor("y", [4 * SL, D], F32, kind="ExternalOutput").ap()
    Kc_d = nc.dram_tensor("Kc", [8, 128, S], BF16, kind="Internal").ap()
    Vc_d = nc.dram_tensor("Vc", [16, 128, 32, 66], BF16, kind="Internal").ap()
    kic_d = nc.dram_tensor("kic", [64, S], BF16, kind="Internal").ap()
    wb_shapes = {"w1gu": (D, 2 * DFF), "w1d": (DFF, D), "win": (D, 5444), "wba": (512, D), "wbb": (512, D),
                 "wo": (D, D), "w2gu": (D, 2 * DFF), "w2d": (DFF, D)}
    wsrc = {"w1gu": w1gu_d, "w1d": w1d_d, "win": win_d, "wba": wba_d, "wbb": wbb_d, "wo": wo_d, "w2gu": w2gu_d, "w2d": w2d_d}
    wbf = {k: nc.dram_tensor(k + "_bf", list(v), BF16, kind="Internal").ap() for k, v in wb_shapes.items()}

    with ExitStack() as st:
        T = Tr(nc, st)

        def sb(name, shape, dt):
            return st.enter_context(nc.sbuf_tensor("sb_" + name, shape, dt))

        def ps(name, shape, dt):
            return st.enter_context(nc.psum_tensor("ps_" + name, shape, dt))

        R1 = sb("R1", [128, 57344], mybir.dt.uint8)

        def r1view(off, shape, dt):
            n = int(np.prod(shape[1:]))
            esz = 2 if dt == BF16 else 4
            v = R1[:, off:off + n * esz].bitcast(dt)
            if len(shape) == 3:
                v = v.rearrange("p (a b) -> p a b", a=shape[1])
            return v
        actT = r1view(0, [128, NFC, SL], BF16)
        xtok = r1view(22528, [128, 4, D], F32)
        wdb = [r1view(38912, [128, NFC, 128], BF16), r1view(44544, [128, NFC, 128], BF16)]
        sqb = [r1view(50176, [128, SL], F32), r1view(52224, [128, SL], F32)]
        maskT = r1view(0, [128, 32, SL], BF16)
        Isc = r1view(32768, [128, S], F32)
        msk = r1view(49152, [128, S], BF16)
        B_R1 = Buf()

        xT = sb("xT", [128, 8, SL], F32)
        xnT = sb("xnT", [128, 8, SL], BF16)
        wgub = [sb("wgu%d" % i, [128, 8, 2, 128], BF16) for i in range(3)]
        winb = [sb("winb%d" % i, [128, 8, 128], BF16) for i in range(3)]
        wvb = sb("wvb", [128, 8, 256], BF16)
        wkib = sb("wkib", [128, 8, 64], BF16)
        wwib = sb("wwib", [128, 8, 4], BF16)
        scr = [sb("scr%d" % i, [128, SL], F32) for i in range(4)]
        rstd = sb("rstd", [128, SL], F32)
        rstd2 = sb("rstd2", [128, SL], F32)
        epst = sb("epst", [128, 1], F32)
        qaT = sb("qaT", [128, 4, SL], BF16)
        qbT = sb("qbT", [128, 4, SL], BF16)
        qiT = sb("qiT", [128, 2, SL], BF16)
        wS = sb("wS", [128, 4, 4], F32)
        kiT2 = sb("kiT2", [128, S], BF16)
        kbuf = sb("kbuf", [128, S], BF16)
        vbuf = sb("vbuf", [128, 32, 66], BF16)
        pbuf = [sb("pbuf%d" % i, [128, SL], BF16) for i in range(4)]
        ebuf = [sb("ebuf%d" % i, [128, SL], BF16) for i in range(3)]
        AT = sb("AT", [128, 8, SL], BF16)
        mixT = sb("mixT", [128, 8, SL], BF16)
        Dt = sb("Dt", [128, 16, 384], BF16)
        esel = sb("esel", [128, 16, 128], BF16)
        idab = sb("idab", [128, 3, 128], BF16)
        c32 = sb("c32", [128, 3, 128], F32)
        gains = sb("gains", [128, 24], F32)
        bg = sb("bg", [128, 16], F32)
        gqk = sb("gqk", [128, 4], F32)
        gq8 = sb("gq8", [128, 2], F32)
        cb = sb("cb", [128, 32], F32)
        mtab = sb("mtab", [128, 896], BF16)
        pbv = sb("pbv", [128, 1], F32)
        gmask = sb("gmask", [128, 2, 8, 4], F32)
        ownhot = sb("ownhot", [128, 2, 8, 4], F32)
        pow2 = sb("pow2", [128, NSTEP], F32)
        kst = [sb("kst%d" % i, [128, SL], BF16) for i in range(2)]
        vst = sb("vst", [128, 4, 8, 66], BF16)
        kist = sb("kist", [64, SL], BF16)
        kms = sb("kms", [128, 4, 16], F32)
        kmb = sb("kmb", [128, 4, 16], BF16)
        G = sb("G", [128, 8, 16], F32)
        sel = sb("sel", [128, 8, 16], F32)
        m8 = sb("m8", [128, 8, 8], F32)
        thr = sb("thr", [128, 8], F32)
        negm = sb("negm", [128, 8, 32], BF16)
        negmT = sb("negmT", [128, 8, SL], BF16)
        small = sb("small", [128, 8], F32)
        steps = sb("steps", [128, NSTEP], F32)
        rd = rstd
        dtmp = scr[0][:, 0:384]
        cmask = scr[1][:, 0:384]

        ident_b = idab[:, 0, :]
        identA = idab[:, 1, :]
        identB = idab[:, 2, :]
        ident32 = c32[:, 0, :]
        ones32 = c32[:, 1, :]
        bones32 = c32[:, 2, :]

        pbank = [ps("pb%d" % i, [128, SL], F32) for i in range(4)]
        B_pbank = [Buf() for _ in range(4)]
        pobank = [ps("po%d" % i, [128, SL], F32) for i in range(2)]
        B_po = [Buf(), Buf()]
        pmisc = ps("pmisc", [128, SL], F32)
        B_misc = Buf()
        psb = ps("psb", [128, 8, 128], BF16)
        B_psb = Buf()
        pctr = [0]

        def nextbank():
            i = pctr[0] % 4
            pctr[0] += 1
            return pbank[i], B_pbank[i]

        B_xtok = Buf()
        B_xT = [Buf() for _ in range(8)]
        B_xnT = [Buf() for _ in range(8)]
        B_actT = [Buf() for _ in range(NFC)]
        B_wgu = [[Buf(), Buf()] for _ in range(3)]
        B_wd = [Buf(), Buf()]
        B_win = [Buf() for _ in range(3)]
        B_wv, B_wki, B_wwi = Buf(), Buf(), Buf()
        B_sq = [Buf(), Buf()]
        B_scr = [Buf() for _ in range(4)]
        B_rstd, B_rstd2 = Buf(), Buf()
        B_const = Buf()
        B_qaT = [Buf() for _ in range(4)]
        B_qbT = [Buf() for _ in range(4)]
        B_qiT = [Buf(), Buf()]
        B_wS = Buf()
        B_kiT2, B_kbuf, B_vbuf = Buf(), Buf(), Buf()
        B_pbuf = [Buf() for _ in range(4)]
        B_ebuf = [Buf(), Buf(), Buf()]
        B_AT = [Buf() for _ in range(8)]
        B_mixT = [Buf() for _ in range(8)]
        B_kst = [Buf(), Buf()]
        B_vst, B_kist = Buf(), Buf()
        B_km = Buf()
        B_G, B_sel, B_m8, B_thr, B_negm, B_negmT = Buf(), Buf(), Buf(), Buf(), Buf(), Buf()
        B_small, B_steps, B_rd = Buf(), Buf(), Buf()
        B_Isc, B_msk = Buf(), Buf()
        B_maskT = [Buf() for _ in range(4)]
        B_Kc = [[Buf() for _ in range(8)] for _ in range(8)]
        B_Vc = [[[Buf() for _ in range(4)] for _ in range(8)] for _ in range(2)]
        B_kic = [Buf() for _ in range(8)]
        B_y = Buf()
        sctr = [0]

        def nextscr():
            i = sctr[0] % 4
            sctr[0] += 1
            return scr[i], B_scr[i]

        stg = [r1view(i * 11264, [128, 5632], BF16) for i in range(4)]
        B_stg = [Buf() for _ in range(4)]
        B_wbf = Buf()
        si = 0
        for k in ("w1gu", "w1d", "win", "wba", "wbb", "wo", "w2gu", "w2d"):
            R_, C_ = wb_shapes[k]
            for rb in range(R_ // 128):
                i = si % 4
                si += 1
                T.dma("pool", stg[i][:, 0:C_], wsrc[k][rb * 128:(rb + 1) * 128, :], writes=[B_stg[i]])
                T.dma("sp", wbf[k][rb * 128:(rb + 1) * 128, :], stg[i][:, 0:C_], reads=[B_stg[i]], writes=[B_wbf])
        T.dma("pool", idab[:], idab_d, writes=[B_const])
        T.dma("pool", esel[:], esel_d, writes=[B_const])
        T.dma("pool", mtab[:], mtab_d, writes=[B_const])
        for dst, src in [(c32, c32_d), (gains, gains_d), (bg, bg_d), (gqk, gqk_d), (pbv, pbv_d),
                         (gmask, gmask_d), (ownhot, ownhot_d), (pow2, pow2_d)]:
            T.dma("sp", dst[:], src, writes=[B_const])
        T.dma("sp", cmask, cmask_d, writes=[B_scr[1]])
        T.dma("sp", cb[:, 0:16], cb_d, writes=[B_const])
        T.op("dve", lambda e: e.memset(epst[:], EPS), writes=[B_const])
        T.op("dve", lambda e: e.memset(vst[:], 1.0), writes=[B_vst])
        T.op("dve", lambda e: e.memset(negm[:], 0.0), writes=[B_negm])
        T.op("dve", lambda e: e.memset(negmT[:], 0.0), writes=[B_negmT])
        T.op("dve", lambda e: e.tensor_scalar(out=gq8[:], in0=gqk[:, 0:2], scalar1=0.125, scalar2=None, op0=ALU.mult),
             reads=[B_const], writes=[B_const])
        T.op("dve", lambda e: e.tensor_scalar(out=cb[:, 16:32], in0=cb[:, 0:16], scalar1=pbv[:, 0:1], scalar2=None, op0=ALU.add),
             reads=[B_const], writes=[B_const])
        for h in range(16):
            T.dma("sp", dtmp, dtab_d[:, h, :], writes=[B_scr[0]])
            T.op("dve", lambda e, h=h: e.scalar_tensor_tensor(out=Dt[:, h, :], in0=dtmp, scalar=cb[:, h:h + 1], in1=cmask,
                                                             op0=ALU.subtract, op1=ALU.add),
                 reads=[B_scr[0], B_scr[1], B_const], writes=[B_const])

        def mmgroup(out_ap, obuf, items, extra_reads=()):
            n = len(items)
            for i, (l, r, rb) in enumerate(items):
                T.op("pe", lambda e, l=l, r=r, i=i: e.matmul(out_ap, lhsT=l, rhs=r, start=(i == 0), stop=(i == n - 1)),
                     reads=list(rb) + list(extra_reads), writes=[obuf], mark=(i == n - 1))

        win_v = wbf["win"].rearrange("(kc p) n -> p kc n", p=128)

        def rmsnorm(gcol0):
            for kc in range(8):
                sq, bsq = sqb[kc % 2], B_sq[kc % 2]
                T.op("act", lambda e, kc=kc, sq=sq: e.activation(out=sq, in_=xT[:, kc, :], func=AF.Square),
                     reads=[B_xT[kc]], writes=[bsq])
                T.op("pe", lambda e, kc=kc, sq=sq: e.matmul(pmisc[:], lhsT=ones32, rhs=sq, start=(kc == 0), stop=(kc == 7)),
                     reads=[bsq, B_const], writes=[B_misc], mark=True)
            T.op("act", lambda e: e.activation(out=rstd[:], in_=pmisc[:], func=AF.Sqrt, bias=epst[:, 0:1], scale=1.0 / D),
                 reads=[B_misc, B_const], writes=[B_rstd])
            T.op("dve", lambda e: e.reciprocal(out=rstd2[:], in_=rstd[:]), reads=[B_rstd], writes=[B_rstd2])
            for kc in range(8):
                T.op("dve", lambda e, kc=kc: e.scalar_tensor_tensor(out=xnT[:, kc, :], in0=xT[:, kc, :],
                                                                   scalar=gains[:, gcol0 + kc:gcol0 + kc + 1], in1=rstd2[:],
                                                                   op0=ALU.mult, op1=ALU.mult),
                     reads=[B_xT[kc], B_rstd2, B_const], writes=[B_xnT[kc]])

        def ffn(wgu_d, wd_d):
            wgu_v = wgu_d.rearrange("(kc p) (two f) -> p kc two f", p=128, two=2)
            wd_v = wd_d.rearrange("(c p) n -> p c n", p=128)

            def load_gu(c):
                T.dma("sp", wgub[c % 3][:, :, 0, :], wgu_v[:, :, 0, c * 128:(c + 1) * 128], writes=[B_wgu[c % 3][0]])
                T.dma("sp", wgub[c % 3][:, :, 1, :], wgu_v[:, :, 1, c * 128:(c + 1) * 128], writes=[B_wgu[c % 3][1]])

            def load_d(cc):
                T.dma("sp", wdb[cc % 2], wd_v[:, :, cc * 128:(cc + 1) * 128], writes=[B_wd[cc % 2]])
            load_gu(0)
            load_gu(1)
            for c in range(NFC):
                if c + 2 < NFC:
                    load_gu(c + 2)
                w = wgub[c % 3]
                gb_, bgb = nextbank()
                ub_, bub = nextbank()
                mmgroup(gb_[:], bgb, [(w[:, kc, 0, :], xnT[:, kc, :], [B_wgu[c % 3][0], B_xnT[kc]]) for kc in range(8)])
                mmgroup(ub_[:], bub, [(w[:, kc, 1, :], xnT[:, kc, :], [B_wgu[c % 3][1], B_xnT[kc]]) for kc in range(8)])
                sg, bsg = nextscr()
                T.op("act", lambda e, sg=sg, gb_=gb_: e.activation(out=sg[:], in_=gb_[:], func=AF.Silu), reads=[bgb], writes=[bsg])
                T.op("dve", lambda e, sg=sg, ub_=ub_, c=c: e.tensor_tensor(out=actT[:, c, :], in0=ub_[:], in1=sg[:], op=ALU.mult),
                     reads=[bub, bsg], writes=[B_actT[c]])
                if c == NFC - 3:
                    load_d(0)
                if c == NFC - 2:
                    load_d(1)
            for cc in range(8):
                w = wdb[cc % 2]
                bk, bbk = nextbank()
                mmgroup(bk[:], bbk, [(w[:, c, :], actT[:, c, :], [B_wd[cc % 2], B_actT[c]]) for c in range(NFC)])
                T.op("dve", lambda e, bk=bk, cc=cc: e.scalar_tensor_tensor(out=xT[:, cc, :], in0=bk[:], scalar=0.5, in1=xT[:, cc, :],
                                                                          op0=ALU.mult, op1=ALU.add),
                     reads=[bbk, B_xT[cc]], writes=[B_xT[cc]])
                if cc + 2 < 8:
                    load_d(cc + 2)

        wctr = [0]

        def load_win(col0):
            i = wctr[0] % 3
            wctr[0] += 1
            T.dma("sp", winb[i][:], win_v[:, :, col0:col0 + 128], writes=[B_win[i]])
            return winb[i], B_win[i]

        def proj_pair_normed(col0, gain_ap, out_ap, obufs):
            w, bw = load_win(col0)
            bk, bbk = nextbank()
            mmgroup(bk[:], bbk, [(w[:, kc, :], xnT[:, kc, :], [bw, B_xnT[kc]]) for kc in range(8)])
            raw, braw = nextscr()
            sq, bsq = nextscr()
            T.op("act", lambda e: e.activation(out=raw[:], in_=bk[:], func=AF.Copy), reads=[bbk], writes=[braw])
            T.op("act", lambda e: e.activation(out=sq[:], in_=bk[:], func=AF.Square), reads=[bbk], writes=[bsq])
            pending.append(lambda: proj_stage_b(raw, braw, sq, bsq, gain_ap, out_ap, obufs))
            if len(pending) > 1:
                pending.pop(0)()

        pending = []

        def flush_proj():
            while pending:
                pending.pop(0)()

        def proj_stage_b(raw, braw, sq, bsq, gain_ap, out_ap, obufs):
            T.op("pe", lambda e: e.matmul(pmisc[:], lhsT=bones32, rhs=sq[:], start=True, stop=True),
                 reads=[bsq, B_const], writes=[B_misc])
            T.op("act", lambda e: e.activation(out=rstd[:], in_=pmisc[:], func=AF.Sqrt, bias=epst[:, 0:1], scale=1.0 / 64),
                 reads=[B_misc, B_const], writes=[B_rstd])
            T.op("dve", lambda e: e.reciprocal(out=rstd2[:], in_=rstd[:]), reads=[B_rstd], writes=[B_rstd2])
            T.op("dve", lambda e: e.scalar_tensor_tensor(out=out_ap, in0=raw[:], scalar=gain_ap, in1=rstd2[:], op0=ALU.mult, op1=ALU.mult),
                 reads=[braw, B_rstd2, B_const], writes=obufs)

        def phase1(slot, own, m):
            T.dma("sp", xtok, x_d[slot * SL:(slot + 1) * SL, :].rearrange("(tt p) f -> p tt f", p=128), writes=[B_xtok])
            for kc in range(8):
                bk, bbk = nextbank()
                for tt in range(4):
                    T.op("pe", lambda e, bk=bk, tt=tt, kc=kc: e.transpose(out=bk[:, tt * 128:(tt + 1) * 128],
                                                                         in_=xtok[:, tt, kc * 128:(kc + 1) * 128], identity=ident32),
                         reads=[B_xtok, B_const], writes=[bbk], mark=(tt == 3))
                if kc % 2 == 0:
                    T.op("act", lambda e, bk=bk, kc=kc: e.activation(out=xT[:, kc, :], in_=bk[:], func=AF.Copy), reads=[bbk], writes=[B_xT[kc]])
                else:
                    T.op("dve", lambda e, bk=bk, kc=kc: e.tensor_copy(out=xT[:, kc, :], in_=bk[:]), reads=[bbk], writes=[B_xT[kc]])
            chk(1)
            rmsnorm(0)
            chk(2)
            ffn(wbf["w1gu"], wbf["w1d"])
            chk(3)
            rmsnorm(8)
            for grp, col_base, gcol in ((0, 512, 2), (1, 2048, 3)):
                for pr in range(4):
                    ks, bks = kst[pr % 2], B_kst[pr % 2]
                    proj_pair_normed(col_base + pr * 128, gqk[:, gcol:gcol + 1], ks[:], [bks])
                    flush_proj()
                    if grp == 1:
                        T.op("dve", lambda e, ks=ks, pr=pr: e.tensor_reduce(out=kms[:, pr, 2 * slot:2 * slot + 2],
                                                                           in_=ks[:].rearrange("p (a b) -> p a b", a=2),
                                                                           axis=AX.X, op=ALU.add),
                             reads=[bks], writes=[B_km])
                        T.op("dve", lambda e, pr=pr: e.tensor_scalar(out=kmb[:, pr, 2 * slot:2 * slot + 2], in0=kms[:, pr, 2 * slot:2 * slot + 2],
                                                                    scalar1=1.0 / 256, scalar2=None, op0=ALU.mult),
                             reads=[B_km], writes=[B_km])
                    T.dma("sp", Kc_d[grp * 4 + pr, :, slot * SL:(slot + 1) * SL], ks[:], reads=[bks], writes=[B_Kc[grp * 4 + pr][slot]])
            for grp, col_base in ((0, 1024), (1, 2560)):
                for half in range(2):
                    T.dma("sp", wvb[:], win_v[:, :, col_base + half * 256:col_base + (half + 1) * 256], writes=[B_wv])
                    for tt in range(4):
                        bk, bbk = nextbank()
                        mmgroup(bk[:, 0:256], bbk, [(xnT[:, kc, tt * 128:(tt + 1) * 128], wvb[:, kc, :], [B_wv, B_xnT[kc]]) for kc in range(8)])
                        T.op("act", lambda e, bk=bk, tt=tt, half=half: e.activation(
                            out=vst[:, tt, half * 4:(half + 1) * 4, 0:64],
                            in_=bk[:, 0:256].rearrange("p (h d) -> p h d", h=4), func=AF.Copy),
                            reads=[bbk], writes=[B_vst])
                for tt in range(4):
                    T.dma("sp", Vc_d[grp * 8:(grp + 1) * 8].rearrange("h p t d -> p t h d")[:, slot * 4 + tt, :, :], vst[:, tt, :, :],
                          reads=[B_vst], writes=[B_Vc[grp][slot][tt]])
            T.dma("sp", wkib[:], win_v[:, :, 3328:3392], writes=[B_wki])
            bk, bbk = nextbank()
            mmgroup(bk[0:64, :], bbk, [(wkib[:, kc, :], xnT[:, kc, :], [B_wki, B_xnT[kc]]) for kc in range(8)])
            T.op("act", lambda e: e.activation(out=kist[:], in_=bk[0:64, :], func=AF.Copy), reads=[bbk], writes=[B_kist])
            T.dma("sp", kic_d[:, slot * SL:(slot + 1) * SL], kist[:], reads=[B_kist], writes=[B_kic[slot]])
            if not own:
                return
            for pr in range(4):
                proj_pair_normed(pr * 128, gq8[:, 0:1], qaT[:, pr, :], [B_qaT[pr]])
            for pr in range(4):
                proj_pair_normed(1536 + pr * 128, gq8[:, 1:2], qbT[:, pr, :], [B_qbT[pr]])
            flush_proj()
            for pr in range(2):
                w, bw = load_win(3072 + pr * 128)
                bk, bbk = nextbank()
                mmgroup(bk[:], bbk, [(w[:, kc, :], xnT[:, kc, :], [bw, B_xnT[kc]]) for kc in range(8)])
                T.op("act", lambda e, bk=bk, pr=pr: e.activation(out=qiT[:, pr, :], in_=bk[:], func=AF.Copy), reads=[bbk], writes=[B_qiT[pr]])
            T.dma("sp", wwib[:], win_v[:, :, 3392:3396], writes=[B_wwi])
            for tt in range(4):
                bk, bbk = nextbank()
                mmgroup(bk[:, 0:4], bbk, [(xnT[:, kc, tt * 128:(tt + 1) * 128], wwib[:, kc, :], [B_wwi, B_xnT[kc]]) for kc in range(8)])
                T.op("dve", lambda e, bk=bk, tt=tt: e.tensor_scalar(out=wS[:, tt, :], in0=bk[:, 0:4], scalar1=0.0625, scalar2=None, op0=ALU.mult),
                     reads=[bbk], writes=[B_wS])

        def attention(m):
            nsl = 2 * m + 2
            nk = nsl * SL
            nkt = nsl * 4
            nb = nsl * 2
            k0 = 1024 * m
            for hf in range(2):
                T.dma("sp", kiT2[hf * 64:(hf + 1) * 64, 0:nk], kic_d[:, 0:nk], reads=B_kic[0:nsl], writes=[B_kiT2])
            def gating(r):
                chk(7 if m == 0 else (12.32 if r == 0 else 12.39))
                half = r // 2
                T.op("dve", lambda e: e.memset(G[:], -BIG), writes=[B_G])
                gbo, bgbo = nextbank()
                for par, (gbk, bgbk) in enumerate(((pmisc, B_misc), (gbo, bgbo))):
                    p0 = 64 * par
                    for pr_ in range(4):
                        h = 2 * pr_ + par
                        T.op("pe", lambda e, h=h, p0=p0, pr_=pr_, gbk=gbk: e.matmul(gbk[:, h * 16:h * 16 + nb], lhsT=qbT[p0:p0 + 64, pr_, r * 128:(r + 1) * 128],
                                                                                  rhs=kmb[p0:p0 + 64, pr_, 0:nb], start=True, stop=True),
                             reads=[B_qbT[pr_], B_km], writes=[bgbk], mark=(pr_ == 3))
                    T.op("dve", lambda e, par=par, gbk=gbk: e.tensor_copy(
                        out=G[:].rearrange("p (hp two) b -> p hp two b", two=2)[:, :, par, 0:nb],
                        in_=gbk[:, 0:128].rearrange("p (hp two b) -> p hp two b", two=2, b=16)[:, :, par, 0:nb]),
                        reads=[bgbk], writes=[B_G])
                T.op("dve", lambda e: e.tensor_tensor(out=G[:, :, 4 * m:4 * m + 4], in0=G[:, :, 4 * m:4 * m + 4], in1=gmask[:, half, :, :], op=ALU.add),
                     reads=[B_G, B_const], writes=[B_G])
                for h in range(8):
                    T.op("dve", lambda e, h=h: e.max(out=m8[:, h, :], in_=G[:, h, :]), reads=[B_G], writes=[B_m8])
                T.op("dve", lambda e: e.tensor_scalar(out=thr[:], in0=m8[:, :, 2], scalar1=-BIG / 2, scalar2=None, op0=ALU.max),
                     reads=[B_m8], writes=[B_thr])
                for h in range(8):
                    T.op("dve", lambda e, h=h: e.tensor_scalar(out=sel[:, h, :], in0=G[:, h, :], scalar1=thr[:, h:h + 1], scalar2=None, op0=ALU.is_ge),
                         reads=[B_G, B_thr], writes=[B_sel])
                T.op("dve", lambda e: e.tensor_tensor(out=sel[:, :, 4 * m:4 * m + 4], in0=sel[:, :, 4 * m:4 * m + 4], in1=ownhot[:, half, :, :], op=ALU.add),
                     reads=[B_sel, B_const], writes=[B_sel])
                T.op("dve", lambda e: e.tensor_scalar(out=negm[:, :, 0:16], in0=sel[:], scalar1=-1.0, scalar2=BIG, op0=ALU.add, op1=ALU.mult),
                     reads=[B_sel], writes=[B_negm])
                for h in range(8):
                    T.op("pe", lambda e, h=h: e.transpose(out=psb[0:32, h, :], in_=negm[:, h, :], identity=ident_b),
                         reads=[B_negm, B_const], writes=[B_psb], mark=(h == 7))
                T.op("act", lambda e: e.activation(out=negmT[0:32, :, r * 128:(r + 1) * 128], in_=psb[0:32, :, :], func=AF.Copy),
                     reads=[B_psb], writes=[B_negmT])
                if m == 1 and r == 0:
                    chk(12.33)


            for r_ in range(4):
                gating(r_)
            pbi = [0]

            def attend(hg):
                moba = hg >= 8
                h = hg % 8
                pr = h // 2
                p0 = 64 * (h % 2)
                grp = 1 if moba else 0
                if h % 2 == 0:
                    T.dma("sp", kbuf[:, 0:nk], Kc_d[grp * 4 + pr, :, 0:nk], reads=B_Kc[grp * 4 + pr][0:nsl], writes=[B_kbuf])
                T.dma("sp", vbuf[:, 0:nkt, :], Vc_d[hg, :, 0:nkt, :], reads=[b for sl_ in B_Vc[grp][0:nsl] for b in sl_], writes=[B_vbuf])
                qT = qbT if moba else qaT
                bq = (B_qbT if moba else B_qaT)[pr]
                po, bpo = pobank[hg % 2], B_po[hg % 2]
                g, jj = h // 3, h % 3
                def stageA(kt):
                    j = kt - 8 * m
                    q0 = 128 * j if 0 <= j < 4 else 0
                    sbk, bsbk = nextbank()
                    items = [(sbk[:, q0:SL], kbuf[p0:p0 + 64, kt * 128:(kt + 1) * 128], qT[p0:p0 + 64, pr, q0:SL], [B_kbuf, bq])]
                    if 0 <= j < 4:
                        W = min(384, SL - q0)
                        items.append((sbk[:, q0:q0 + W], ident_b, Dt[:, hg, 0:W], [B_const]))
                    if m >= 1 and kt == 8 * m - 1:
                        items.append((sbk[:, 0:256], identA, Dt[:, hg, 128:384], [B_const]))
                    if kt == 8 * m + 7:
                        items.append((sbk[:, 0:256], identB, Dt[:, hg, 128:384], [B_const]))
                    if moba:
                        items.append((sbk[:, q0:SL], esel[:, kt // 2, :], negmT[:, h, q0:SL], [B_const, B_negmT]))
                    n = len(items)
                    for i, (o, l, rr, rb) in enumerate(items):
                        T.op("pe", lambda e, o=o, l=l, rr=rr, i=i, n=n: e.matmul(o, lhsT=l, rhs=rr, start=(i == 0), stop=(i == n - 1)),
                             reads=rb, writes=[bsbk], mark=(i == n - 1))
                    pb_, bpb = pbuf[pbi[0] % 4], B_pbuf[pbi[0] % 4]
                    if moba:
                        eb_, beb = pb_, bpb
                    else:
                        eb_, beb = ebuf[pbi[0] % 3], B_ebuf[pbi[0] % 3]
                    pbi[0] += 1
                    col = hg + (16 if kt >= 8 * m + 4 else 0)
                    T.op("act", lambda e: e.activation(out=eb_[:, q0:SL], in_=sbk[:, q0:SL], func=AF.Exp, bias=cb[:, col:col + 1], scale=1.0),
                         reads=[bsbk, B_const], writes=[beb])
                    if not moba:
                        T.op("dve", lambda e: e.scalar_tensor_tensor(out=pb_[:, q0:SL], in0=eb_[:, q0:SL], scalar=1.0,
                                                                     in1=maskT[:, kt, q0:SL], op0=ALU.mult, op1=ALU.mult),
                             reads=[beb] + B_maskT, writes=[bpb])
                    return (kt, q0, pb_, bpb)

                def stageB(st_):
                    kt, q0, pb_, bpb = st_
                    T.op("pe", lambda e: e.matmul(po[0:65, q0:SL], lhsT=vbuf[:, kt, 0:65], rhs=pb_[:, q0:SL],
                                                  start=(kt == 0), stop=(kt == nkt - 1)),
                         reads=[B_vbuf, bpb], writes=[bpo], mark=True)

                pend = []
                for kt in range(nkt):
                    pend.append(stageA(kt))
                    if len(pend) > 2:
                        stageB(pend.pop(0))
                while pend:
                    stageB(pend.pop(0))
                chk(8.4 if hg == 0 else (8.7 if hg == 8 else 8.99))
                T.op("dve", lambda e, po=po: e.reciprocal(out=rd[64:65, :], in_=po[64:65, :]), reads=[bpo], writes=[B_rd])
                if hg == 0:
                    chk(8.41)
                T.op("pe", lambda e: e.matmul(pmisc[:], lhsT=ones32[64:65, :], rhs=rd[64:65, :], start=True, stop=True),
                     reads=[B_rd, B_const], writes=[B_misc])
                if hg == 0:
                    chk(8.42)
                tmpo, btmpo = nextscr()
                T.op("act", lambda e, po=po, tmpo=tmpo, p0=p0: e.activation(out=tmpo[p0:p0 + 64, :], in_=po[0:64, :], func=AF.Copy),
                     reads=[bpo], writes=[btmpo])
                ap_ = pr + (4 if moba else 0)
                if hg == 0:
                    chk(8.43)
                T.op("dve", lambda e, tmpo=tmpo, p0=p0, ap_=ap_: e.tensor_tensor(out=AT[p0:p0 + 64, ap_, :], in0=pmisc[p0:p0 + 64, :],
                                                                                 in1=tmpo[p0:p0 + 64, :], op=ALU.mult),
                     reads=[btmpo, B_misc], writes=[B_AT[ap_]])
                chk(8.5 if hg == 0 else (8.6 if hg == 7 else (8.8 if hg == 8 else 8.99)))


            def finish_mask(r):
                if m == 1 and r == 0:
                    chk(12.314)
                for g0 in range(0, nkt, 8):
                    if m == 1 and r == 0 and g0 == 8:
                        chk(12.315)
                    for i in range(8):
                        kt = g0 + i
                        T.op("pe", lambda e, i=i, kt=kt: e.transpose(out=psb[:, i, :], in_=msk[:, kt * 128:(kt + 1) * 128], identity=ident_b),
                             reads=[B_msk, B_const], writes=[B_psb], mark=(i == 7))
                    T.op("act", lambda e, g0=g0: e.activation(out=maskT[:, g0:g0 + 8, r * 128:(r + 1) * 128], in_=psb[:], func=AF.Copy),
                         reads=[B_psb], writes=[B_maskT[r]])

            for r in range(4):
                for j in range(nsl):
                    banks = [nextbank() for _ in range(4)]
                    for h in range(4):
                        p0 = 64 * (h % 2)
                        bk, bbk = banks[h]
                        mmgroup(bk[:], bbk, [(qiT[p0:p0 + 64, h // 2, r * 128:(r + 1) * 128], kiT2[p0:p0 + 64, j * SL:(j + 1) * SL],
                                              [B_qiT[h // 2], B_kiT2])])
                    for h in range(4):
                        bk, bbk = banks[h]
                        rl, brl = nextscr()
                        T.op("act", lambda e, bk=bk, rl=rl: e.activation(out=rl[:], in_=bk[:], func=AF.Relu), reads=[bbk], writes=[brl])
                        dst = Isc[:, j * SL:(j + 1) * SL]
                        if h == 0:
                            T.op("dve", lambda e, rl=rl, dst=dst: e.tensor_scalar(out=dst, in0=rl[:], scalar1=wS[:, r, 0:1], scalar2=None, op0=ALU.mult),
                                 reads=[brl, B_wS], writes=[B_Isc])
                        else:
                            T.op("dve", lambda e, rl=rl, dst=dst, h=h: e.scalar_tensor_tensor(out=dst, in0=rl[:], scalar=wS[:, r, h:h + 1], in1=dst,
                                                                                            op0=ALU.mult, op1=ALU.add),
                                 reads=[brl, B_wS, B_Isc], writes=[B_Isc])
                chk(6 if m == 0 else (12.31 if r == 0 else 12.39))
                Bv, lo, mid, cnt, tt_ = (small[:, i:i + 1] for i in range(5))
                T.op("dve", lambda e: e.tensor_reduce(out=Bv, in_=Isc[:, 0:nk], axis=AX.X, op=ALU.max, apply_absolute_value=True),
                     reads=[B_Isc], writes=[B_small])
                if m == 1 and r == 0:
                    chk(12.311)
                T.op("dve", lambda e: e.tensor_scalar(out=lo, in0=Bv, scalar1=1.0, scalar2=-1.0, op0=ALU.add, op1=ALU.mult),
                     reads=[B_small], writes=[B_small])
                T.op("dve", lambda e: e.tensor_scalar(out=steps[:], in0=pow2[:], scalar1=lo, scalar2=-2.0, op0=ALU.mult, op1=ALU.mult),
                     reads=[B_small, B_const], writes=[B_steps])
                T.op("dve", lambda e: e.tensor_tensor(out=Isc[:, k0:k0 + SL], in0=Isc[:, k0:k0 + SL],
                                                      in1=mtab[:, 384 - 128 * r:896 - 128 * r], op=ALU.add),
                     reads=[B_Isc, B_const], writes=[B_Isc])
                T.op("dve", lambda e: e.tensor_scalar(out=Isc[:, k0 + SL:k0 + 2 * SL], in0=Isc[:, k0 + SL:k0 + 2 * SL], scalar1=pbv[:, 0:1],
                                                      scalar2=None, op0=ALU.add),
                     reads=[B_Isc, B_const], writes=[B_Isc])
                if m == 1 and r == 0:
                    chk(12.312)
                for s_ in range(NSTEP):
                    if m == 1 and r == 0 and s_ == 1:
                        chk(12.313)
                    T.op("dve", lambda e, s_=s_: e.tensor_tensor(out=mid, in0=lo, in1=steps[:, s_:s_ + 1], op=ALU.add),
                         reads=[B_small, B_steps], writes=[B_small])
                    T.op("dve", lambda e: e.tensor_scalar(out=msk[:, 0:nk], in0=Isc[:, 0:nk], scalar1=mid, scalar2=0.0, op0=ALU.is_ge, op1=ALU.add,
                                                          accum_out=cnt),
                         reads=[B_Isc, B_small], writes=[B_msk, B_small])
                    T.op("dve", lambda e, s_=s_: e.tensor_scalar(out=tt_, in0=cnt, scalar1=256.0, scalar2=steps[:, s_:s_ + 1], op0=ALU.is_ge, op1=ALU.mult),
                         reads=[B_small, B_steps], writes=[B_small])
                    T.op("dve", lambda e: e.tensor_tensor(out=lo, in0=lo, in1=tt_, op=ALU.add), reads=[B_small], writes=[B_small])
                T.op("dve", lambda e: e.tensor_scalar(out=msk[:, 0:nk], in0=Isc[:, 0:nk], scalar1=lo, scalar2=None, op0=ALU.is_ge),
                     reads=[B_Isc, B_small], writes=[B_msk])
                for hg_ in (8 + 2 * r, 9 + 2 * r):
                    attend(hg_)
                finish_mask(r)
            for hg_ in range(8):
                attend(hg_)

        def mix(m):
            wba_v = wbf["wba"].rearrange("(kc p) n -> p kc n", p=128)
            wbb_v = wbf["wbb"].rearrange("(kc p) n -> p kc n", p=128)
            wo_v = wbf["wo"].rearrange("(kc p) n -> p kc n", p=128)
            for cc in range(8):
                wa, bwa = winb[wctr[0] % 3], B_win[wctr[0] % 3]
                wctr[0] += 1
                T.dma("sp", wa[:, 0:4, :], wba_v[:, :, cc * 128:(cc + 1) * 128], writes=[bwa])
                T.dma("sp", wa[:, 4:8, :], wbb_v[:, :, cc * 128:(cc + 1) * 128], writes=[bwa])
                wga, bwga = load_win(3396 + cc * 128)
                wgb, bwgb = load_win(3396 + 1024 + cc * 128)
                bA, bbA = nextbank()
                bB, bbB = nextbank()
                bGa, bbGa = nextbank()
                bGb, bbGb = nextbank()
                mmgroup(bA[:], bbA, [(wa[:, kc, :], AT[:, kc, :], [bwa, B_AT[kc]]) for kc in range(4)])
                mmgroup(bB[:], bbB, [(wa[:, 4 + kc, :], AT[:, 4 + kc, :], [bwa, B_AT[4 + kc]]) for kc in range(4)])
                mmgroup(bGa[:], bbGa, [(wga[:, kc, :], xnT[:, kc, :], [bwga, B_xnT[kc]]) for kc in range(8)])
                mmgroup(bGb[:], bbGb, [(wgb[:, kc, :], xnT[:, kc, :], [bwgb, B_xnT[kc]]) for kc in range(8)])
                ga, bga = nextscr()
                gb2, bgb2 = nextscr()
                T.op("act", lambda e, ga=ga, bGa=bGa, cc=cc: e.activation(out=ga[:], in_=bGa[:], func=AF.Sigmoid, bias=bg[:, cc:cc + 1], scale=1.0),
                     reads=[bbGa, B_const], writes=[bga])
                T.op("act", lambda e, gb2=gb2, bGb=bGb, cc=cc: e.activation(out=gb2[:], in_=bGb[:], func=AF.Sigmoid, bias=bg[:, 8 + cc:9 + cc], scale=1.0),
                     reads=[bbGb, B_const], writes=[bgb2])
                T.op("dve", lambda e, ga=ga, bA=bA: e.tensor_tensor(out=ga[:], in0=bA[:], in1=ga[:], op=ALU.mult), reads=[bbA, bga], writes=[bga])
                T.op("dve", lambda e, gb2=gb2, bB=bB: e.tensor_tensor(out=gb2[:], in0=bB[:], in1=gb2[:], op=ALU.mult), reads=[bbB, bgb2], writes=[bgb2])
                T.op("dve", lambda e, ga=ga, gb2=gb2, cc=cc: e.tensor_tensor(out=mixT[:, cc, :], in0=ga[:], in1=gb2[:], op=ALU.add),
                     reads=[bga, bgb2], writes=[B_mixT[cc]])
            for cc in range(8):
                w, bw = winb[wctr[0] % 3], B_win[wctr[0] % 3]
                wctr[0] += 1
                T.dma("sp", w[:], wo_v[:, :, cc * 128:(cc + 1) * 128], writes=[bw])
                bk, bbk = nextbank()
                mmgroup(bk[:], bbk, [(w[:, kc, :], mixT[:, kc, :], [bw, B_mixT[kc]]) for kc in range(8)])
                T.op("dve", lambda e, bk=bk, cc=cc: e.tensor_tensor(out=xT[:, cc, :], in0=bk[:], in1=xT[:, cc, :], op=ALU.add),
                     reads=[bbk, B_xT[cc]], writes=[B_xT[cc]])

        def output(m):
            for tt in range(4):
                for half in range(2):
                    bk, bbk = nextbank()
                    for i in range(4):
                        kc = half * 4 + i
                        T.op("pe", lambda e, bk=bk, i=i, kc=kc, tt=tt: e.transpose(out=bk[:, i * 128:(i + 1) * 128],
                                                                                  in_=xT[:, kc, tt * 128:(tt + 1) * 128], identity=ident32),
                             reads=[B_xT[kc], B_const], writes=[bbk], mark=(i == 3))
                    if half == 0:
                        T.op("act", lambda e, bk=bk, tt=tt: e.activation(out=xtok[:, tt, 0:512], in_=bk[:], func=AF.Copy), reads=[bbk], writes=[B_xtok])
                    else:
                        T.op("dve", lambda e, bk=bk, tt=tt: e.tensor_copy(out=xtok[:, tt, 512:1024], in_=bk[:]), reads=[bbk], writes=[B_xtok])
            T.dma("sp", y_d[m * SL:(m + 1) * SL, :].rearrange("(tt p) f -> p tt f", p=128), xtok, reads=[B_xtok], writes=[B_y])

        T.barrier()
        try:
            chk(0)
            for m in range(4):
                phase1(2 * m + 1, False, m)
                chk(4 if m == 0 else 12.2)
                phase1(2 * m, True, m)
                chk(5 if m == 0 else 12.3)
                T.barrier()
                attention(m)
                chk(9 if m == 0 else 12.5)
                T.barrier()
                mix(m)
                chk(10 if m == 0 else 12.6)
                rmsnorm(16)
                ffn(wbf["w2gu"], wbf["w2d"])
                chk(11)
                output(m)
                chk(12 + m)
        except StopBuild:
            pass
        T.barrier()
    return nc


def _t5_bucket(d):
    d = np.maximum(d, 0)
    df = np.maximum(d, 1).astype(np.float64)
    large = 16 + (np.log(df / 16.0) / math.log(128 / 16) * 16).astype(np.int64)
    large = np.minimum(large, 31)
    return np.where(d < 16, d, large)


def _consts(p, rel_bias):
    c = {}
    s = np.arange(128)[:, None]
    cc = np.arange(384)[None, :]
    bucket = _t5_bucket(cc - s)
    c["dtab"] = np.ascontiguousarray(np.transpose(rel_bias[bucket], (0, 2, 1))).astype(np.float32)
    c["cb"] = np.ascontiguousarray(np.broadcast_to(rel_bias[31][None, :], (128, 16))).astype(np.float32)
    c["cmask"] = np.where(cc >= s, 0.0, -BIG).astype(np.float32)
    c2 = np.arange(896)[None, :]
    c["mtab"] = np.where(c2 <= s + 384, 0.0, -BIG).astype(np.float32)
    pb = 0.0 if p == 1 else -BIG
    c["pbv"] = np.full((128, 1), pb, np.float32)
    eye = np.eye(128, dtype=np.float32)
    c["idab"] = np.ascontiguousarray(np.stack([eye, eye * (1.0 if p == 0 else 0.0), eye * (1.0 if p == 1 else 0.0)], axis=1))
    pp = np.arange(128)
    es = np.zeros((128, 16, 128), np.float32)
    for b in range(16):
        es[pp == b, b, :] = 1.0
    c["esel"] = es
    gm = np.zeros((2, 4), np.float32)
    oh = np.zeros((2, 4), np.float32)
    gm[0] = [-BIG, -BIG, pb, pb]
    oh[0] = [1, 0, 0, 0]
    gm[1] = [0.0, -BIG, pb, pb]
    oh[1] = [0, 1, 0, 0]
    c["gmask"] = np.ascontiguousarray(np.broadcast_to(gm[None, :, None, :], (128, 2, 8, 4))).astype(np.float32)
    c["ownhot"] = np.ascontiguousarray(np.broadcast_to(oh[None, :, None, :], (128, 2, 8, 4))).astype(np.float32)
    bo = np.zeros((128, 128), np.float32)
    bo[0:64, 0:64] = 1.0
    bo[64:128, 64:128] = 1.0
    c["c32"] = np.ascontiguousarray(np.stack([eye, np.ones((128, 128), np.float32), bo], axis=1))
    c["pow2"] = np.ascontiguousarray(np.broadcast_to((0.5 ** (np.arange(NSTEP) + 1))[None, :], (128, NSTEP))).astype(np.float32)
    return c


_NC_CACHE = {}


def make_in_maps(**inp):
    f = lambda k: np.asarray(inp[k], dtype=np.float32)
    x = f("x")
    fm = lambda v: np.ascontiguousarray(v.reshape(8, 128).T)
    gains = np.concatenate([fm(f("ffn1_norm")[0]), fm(f("mix_norm")[0]), fm(f("ffn2_norm")[0])], axis=1).astype(np.float32)
    bgv = np.ascontiguousarray(f("b_gate")[0].reshape(16, 128).T)
    gqk = np.stack([np.tile(f("q_norm_dsa")[0], 2), np.tile(f("q_norm_moba")[0], 2),
                    np.tile(f("k_norm_dsa")[0], 2), np.tile(f("k_norm_moba")[0], 2)], axis=1).astype(np.float32)
    rel_bias = f("rel_bias")
    shared = {
        "w1gu": np.ascontiguousarray(f("ffn1_w_gu")[0]), "w1d": np.ascontiguousarray(f("ffn1_w_down")[0]),
        "win": np.ascontiguousarray(f("w_in")[0]), "wba": np.ascontiguousarray(f("w_branch_dsa")[0]),
        "wbb": np.ascontiguousarray(f("w_branch_moba")[0]), "wo": np.ascontiguousarray(f("w_out")[0]),
        "w2gu": np.ascontiguousarray(f("ffn2_w_gu")[0]), "w2d": np.ascontiguousarray(f("ffn2_w_down")[0]),
        "gains": np.ascontiguousarray(gains), "bg": bgv, "gqk": np.ascontiguousarray(gqk),
    }
    cons = [_consts(0, rel_bias), _consts(1, rel_bias)]
    in_maps = []
    for core in range(8):
        b, p = core // 2, core % 2
        xb = x[b].reshape(8, SL, D)
        order = list(range(8)) if p == 0 else [1, 0, 3, 2, 5, 4, 7, 6]
        d = dict(shared)
        d.update(cons[p])
        d["x"] = np.ascontiguousarray(xb[order].reshape(S, D))
        in_maps.append(d)
    return in_maps


def kernel(**inp):
    in_maps = make_in_maps(**inp)
    if "nc" not in _NC_CACHE:
        _NC_CACHE["nc"] = build_program()
    res = run_bass_kernel_spmd(_NC_CACHE["nc"], in_maps, core_ids=list(range(8)))
    out = np.empty((4, S, D), np.float32)
    for core in range(8):
        b, p = core // 2, core % 2
        y = np.asarray(res.results[core]["y"]).reshape(4, SL, D)
        for m in range(4):
            out[b, (2 * m + p) * SL:(2 * m + p + 1) * SL] = y[m]
    return out
```
